# Optimizing a Trainium2 kernel written in Bass

```python
import math
import jax
import jax.numpy as jnp
from jax import lax
import numpy as np

D_MODEL = 1024
BATCH = 4
SEQ = 8192
DEPTH = 2

GRID_W = 64
CTX_LEN = 256
EPS = 1e-6
ROPE_BASE = 10000.0

A_HEADS = 8
A_DK = 64
A_DV = 64
A_CHUNK = 64
SHORT_CONV = 3
B_HEADS = 4
B_DH = 64
Q_BLOCK = 128
C_GROUPS = 4
PEER_HEADS = 8
PEER_NKEYS = 128
PEER_EXPERTS = PEER_NKEYS * PEER_NKEYS
PEER_DKEY = 256
PEER_TOPK = 16
PEER_BLOCK = 128

N_EVEN = (DEPTH + 1) // 2
N_ODD = DEPTH // 2

A_QK_W = A_HEADS * A_DK
A_V_W = A_HEADS * A_DV
B_QK_W = B_HEADS * 2 * B_DH
B_V_W = B_HEADS * 2 * B_DH
IN_SPLITS = (A_QK_W, A_QK_W, A_V_W, A_V_W, 2 * A_HEADS, 2 * A_HEADS, B_QK_W, B_QK_W, B_V_W)
IN_W = sum(IN_SPLITS)
MIX_W = A_V_W + B_V_W
CONV_CH = 2 * A_QK_W + A_V_W

kernel_name = 'hybrid_gdn_diffattn_fnet_peer_dit'


def rmsnorm(x, w):
    xf = x.astype(jnp.float32)
    y = xf * lax.rsqrt(jnp.mean(xf * xf, axis=-1, keepdims=True) + EPS)
    return (y * w.astype(jnp.float32)).astype(x.dtype)


def l2norm(x):
    return x * lax.rsqrt(jnp.sum(x * x, axis=-1, keepdims=True) + EPS)


def split_cols(p):
    idx, acc = [], 0
    for w in IN_SPLITS[:-1]:
        acc += w
        idx.append(acc)
    return jnp.split(p, idx, axis=-1)


def short_conv(x, w):
    pad = SHORT_CONV // 2
    return lax.conv_general_dilated(x, w[:, None, :].astype(x.dtype), (1,), [(pad, pad)],
                                    dimension_numbers=('NWC', 'WIO', 'NWC'),
                                    feature_group_count=x.shape[-1])


def axial_rope(n_tokens):
    rows = n_tokens // GRID_W
    row = jnp.repeat(jnp.arange(rows, dtype=jnp.float32), GRID_W)
    col = jnp.tile(jnp.arange(GRID_W, dtype=jnp.float32), rows)
    axis_dim = B_DH // 2
    inv = ROPE_BASE ** (-jnp.arange(0, axis_dim, 2, dtype=jnp.float32) / axis_dim)
    ang = jnp.concatenate([row[:, None] * inv, col[:, None] * inv], axis=-1)
    return jnp.cos(ang), jnp.sin(ang)


def apply_rope(x, cos, sin):
    x1, x2 = jnp.split(x, 2, axis=-1)
    c = cos[None, :, None, None, :]
    s = sin[None, :, None, None, :]
    return jnp.concatenate([x1 * c - x2 * s, x1 * s + x2 * c], axis=-1).astype(x.dtype)


def _to_chunks(t, n):
    t = t.reshape((t.shape[0], n, A_CHUNK) + t.shape[2:])
    return jnp.moveaxis(t, (1, 3), (0, 2))


def gated_delta_chunked(q, k, v, beta, g, s0):
    bsz, L, H, _ = k.shape
    n = L // A_CHUNK
    kc, vc, bc, gc = (_to_chunks(t, n) for t in (k, v, beta, g))
    gcum = jnp.cumsum(gc, axis=-1)
    lower = jnp.tril(jnp.ones((A_CHUNK, A_CHUNK), dtype=bool))
    strict = jnp.tril(jnp.ones((A_CHUNK, A_CHUNK), dtype=bool), -1)
    decay = jnp.exp(jnp.where(lower, gcum[..., :, None] - gcum[..., None, :], -jnp.inf))
    kb = kc * bc[..., None]
    m = jnp.where(strict, jnp.einsum('nbhid,nbhjd->nbhij', kb, kc) * decay, 0.0)
    eye = jnp.eye(A_CHUNK, dtype=jnp.float32)
    t_inv = lax.linalg.triangular_solve(eye + m, jnp.broadcast_to(eye, m.shape), left_side=True,
                                        lower=True, unit_diagonal=True)
    u = jnp.einsum('nbhij,nbhjd->nbhid', t_inv, vc * bc[..., None])
    w = jnp.einsum('nbhij,nbhjd->nbhid', t_inv, kb * jnp.exp(gcum)[..., None])
    k_tail = kc * jnp.exp(gcum[..., -1:] - gcum)[..., None]
    g_tot = jnp.exp(gcum[..., -1])

    def advance(S, u_i, w_i, kt_i, gt_i):
        v_new = u_i - jnp.einsum('bhcd,bhde->bhce', w_i, S)
        S_next = S * gt_i[..., None, None] + jnp.einsum('bhcd,bhce->bhde', kt_i, v_new)
        return S_next, v_new

    if q is None:
        def step_state(S, xs):
            S_next, _ = advance(S, *xs)
            return S_next, None
        S_fin, _ = lax.scan(step_state, s0, (u, w, k_tail, g_tot))
        return S_fin, None

    qc = _to_chunks(q, n)
    q_dec = qc * jnp.exp(gcum)[..., None]
    attn = jnp.einsum('nbhid,nbhjd->nbhij', qc, kc) * decay

    def step(S, xs):
        u_i, w_i, kt_i, gt_i, qd_i, at_i = xs
        S_next, v_new = advance(S, u_i, w_i, kt_i, gt_i)
        o = jnp.einsum('bhcd,bhde->bhce', qd_i, S) + jnp.einsum('bhij,bhje->bhie', at_i, v_new)
        return S_next, o

    S_fin, o = lax.scan(step, s0, (u, w, k_tail, g_tot, q_dec, attn))
    o = jnp.moveaxis(o, (0, 2), (1, 3)).reshape(bsz, L, H, v.shape[-1])
    return S_fin, o


def flip_if(t, rev):
    return jnp.flip(t, axis=1) if rev else t


def diff_softmax_mix(q, keys, vals, lam):
    s = jnp.einsum('bqhmd,bkhmd->bhmqk', q.astype(jnp.float32), keys.astype(jnp.float32)) * (B_DH ** -0.5)
    p = jax.nn.softmax(s, axis=-1)
    a = p[:, :, 0] - lam * p[:, :, 1]
    return jnp.einsum('bhqk,bkhe->bqhe', a, vals.astype(jnp.float32))


def mixer_ab(h, h_c, layer, w_in, conv_w, a_log, dt_bias, gdn_norm_w, lam_q1, lam_k1, lam_q2, lam_k2,
             subln_w, w_out, cos, sin, with_ctx_out):
    f32 = jnp.float32
    lam_init = 0.8 - 0.6 * math.exp(-0.3 * layer)
    lam = (jnp.exp(jnp.sum(lam_q1.astype(f32) * lam_k1.astype(f32)))
           - jnp.exp(jnp.sum(lam_q2.astype(f32) * lam_k2.astype(f32))) + lam_init)

    def project(t):
        bsz, n, _ = t.shape
        aq, ak, av, ag, a_beta, a_alpha, bq, bk, bv = split_cols(t @ w_in)
        qkv = jax.nn.silu(short_conv(jnp.concatenate([aq, ak, av], axis=-1), conv_w)).astype(f32)
        q, k, v = jnp.split(qkv, [A_QK_W, 2 * A_QK_W], axis=-1)
        q = l2norm(q.reshape(bsz, n, A_HEADS, A_DK)) * (A_DK ** -0.5)
        k = l2norm(k.reshape(bsz, n, A_HEADS, A_DK))
        v = v.reshape(bsz, n, A_HEADS, A_DV)
        beta = jax.nn.sigmoid(a_beta.astype(f32).reshape(bsz, n, 2, A_HEADS))
        g = -jnp.exp(a_log.astype(f32)) * jax.nn.softplus(
            a_alpha.astype(f32).reshape(bsz, n, 2, A_HEADS) + dt_bias.astype(f32))
        return (q, k, v, beta, g, ag,
                bq.reshape(bsz, n, B_HEADS, 2, B_DH), bk.reshape(bsz, n, B_HEADS, 2, B_DH),
                bv.reshape(bsz, n, B_HEADS, 2 * B_DH))

    ql, kl, vl, betal, gl, gate_l, bql, bkl, bvl = project(h)
    qc, kc, vc, betac, gc, gate_c, bqc, bkc, bvc = project(h_c)
    bsz, L = h.shape[0], h.shape[1]

    s0 = jnp.zeros((bsz, A_HEADS, A_DK, A_DV), f32)
    o_lat_dirs, o_ctx_dirs = [], []
    for d in range(2):
        rev = d == 1
        s_ctx, oc = gated_delta_chunked(flip_if(qc, rev) if with_ctx_out else None, flip_if(kc, rev),
                                        flip_if(vc, rev), flip_if(betac[:, :, d], rev),
                                        flip_if(gc[:, :, d], rev), s0)
        _, ol = gated_delta_chunked(flip_if(ql, rev), flip_if(kl, rev), flip_if(vl, rev),
                                    flip_if(betal[:, :, d], rev), flip_if(gl[:, :, d], rev), s_ctx)
        o_lat_dirs.append(flip_if(ol, rev))
        if with_ctx_out:
            o_ctx_dirs.append(flip_if(oc, rev))

    def gdn_out(o, gate):
        y = rmsnorm(o, gdn_norm_w) * jax.nn.silu(gate.astype(f32)).reshape(o.shape)
        return y.reshape(o.shape[0], o.shape[1], A_V_W)

    def diff_out(o):
        return (rmsnorm(o, subln_w) * (1.0 - lam_init)).reshape(o.shape[0], o.shape[1], B_V_W)

    bql_r = apply_rope(bql, cos, sin)
    bkl_r = apply_rope(bkl, cos, sin)
    keys = jnp.concatenate([bkl_r, bkc], axis=1)
    vals = jnp.concatenate([bvl, bvc], axis=1)
    nb = L // Q_BLOCK
    qb = jnp.moveaxis(bql_r.reshape(bsz, nb, Q_BLOCK, B_HEADS, 2, B_DH), 1, 0)
    ob = lax.map(lambda blk: diff_softmax_mix(blk, keys, vals, lam), qb)
    d_lat = jnp.moveaxis(ob, 0, 1).reshape(bsz, L, B_HEADS, 2 * B_DH)

    y_lat = jnp.concatenate([gdn_out(o_lat_dirs[0] + o_lat_dirs[1], gate_l), diff_out(d_lat)],
                            axis=-1).astype(h.dtype) @ w_out
    y_ctx = None
    if with_ctx_out:
        d_ctx = diff_softmax_mix(bqc, bkc, bvc, lam)
        y_ctx = jnp.concatenate([gdn_out(o_ctx_dirs[0] + o_ctx_dirs[1], gate_c), diff_out(d_ctx)],
                                axis=-1).astype(h_c.dtype) @ w_out
    return y_lat, y_ctx


def fourier_mix(h):
    bsz, L, D = h.shape
    hg = h.astype(jnp.float32).reshape(bsz, L, C_GROUPS, D // C_GROUPS)
    return jnp.fft.fft2(hg, axes=(1, 3), norm='ortho').real.reshape(bsz, L, D).astype(h.dtype)


def peer(h, w_q, sub_keys, u_tab, v_tab):
    bsz, L, D = h.shape
    blocks = h.reshape(-1, PEER_BLOCK, D)

    def block(hb):
        q = (hb @ w_q).astype(jnp.float32).reshape(PEER_BLOCK, PEER_HEADS, 2, PEER_DKEY // 2)
        s = jnp.einsum('thpd,hpnd->thpn', q, sub_keys.astype(jnp.float32))
        s1, i1 = lax.top_k(s[:, :, 0], PEER_TOPK)
        s2, i2 = lax.top_k(s[:, :, 1], PEER_TOPK)
        cand_s = (s1[..., :, None] + s2[..., None, :]).reshape(PEER_BLOCK, PEER_HEADS, PEER_TOPK * PEER_TOPK)
        cand_i = (i1[..., :, None] * PEER_NKEYS + i2[..., None, :]).reshape(PEER_BLOCK, PEER_HEADS, PEER_TOPK * PEER_TOPK)
        top_s, pos = lax.top_k(cand_s, PEER_TOPK)
        idx = jnp.take_along_axis(cand_i, pos, axis=-1)
        gate = jax.nn.softmax(top_s, axis=-1)
        act = jax.nn.gelu(jnp.einsum('td,thkd->thk', hb.astype(jnp.float32), u_tab[idx].astype(jnp.float32)))
        return jnp.einsum('thk,thkd->td', (gate * act).astype(v_tab.dtype), v_tab[idx])

    return lax.map(block, blocks).reshape(bsz, L, D).astype(h.dtype)


def setup_inputs(seed: int = 0) -> dict:
    key = jax.random.key(seed)
    ks = jax.random.split(key, 25)

    def nrm(k, shape, s):
        return jax.random.normal(k, shape, jnp.float32) * s

    dt = jnp.exp(jax.random.uniform(ks[11], (N_EVEN, 2, A_HEADS), jnp.float32,
                                    minval=math.log(1e-3), maxval=math.log(1e-1)))
    return {
        'x': nrm(ks[0], (BATCH, SEQ, D_MODEL), 1.0),
        'c': nrm(ks[1], (BATCH, D_MODEL), 1.0),
        'ctx': nrm(ks[2], (BATCH, CTX_LEN, D_MODEL), 1.0),
        'c_ctx': nrm(ks[3], (D_MODEL,), 1.0),
        'ada_w': nrm(ks[4], (DEPTH, D_MODEL, 6 * D_MODEL), 0.3 * D_MODEL ** -0.5),
        'ada_b': nrm(ks[5], (DEPTH, 6 * D_MODEL), 0.02),
        'norm1_w': 1.0 + nrm(ks[6], (DEPTH, D_MODEL), 0.02),
        'norm2_w': 1.0 + nrm(ks[7], (DEPTH, D_MODEL), 0.02),
        'w_in': nrm(ks[8], (N_EVEN, D_MODEL, IN_W), D_MODEL ** -0.5),
        'conv_w': nrm(ks[9], (N_EVEN, SHORT_CONV, CONV_CH), SHORT_CONV ** -0.5),
        'a_log': jnp.log(jax.random.uniform(ks[10], (N_EVEN, 2, A_HEADS), jnp.float32, minval=1.0, maxval=16.0)),
        'dt_bias': dt + jnp.log(-jnp.expm1(-dt)),
        'gdn_norm_w': 1.0 + nrm(ks[12], (N_EVEN, A_DV), 0.02),
        'lam_q1': nrm(ks[13], (N_EVEN, B_DH), 0.1),
        'lam_k1': nrm(ks[14], (N_EVEN, B_DH), 0.1),
        'lam_q2': nrm(ks[15], (N_EVEN, B_DH), 0.1),
        'lam_k2': nrm(ks[16], (N_EVEN, B_DH), 0.1),
        'subln_w': 1.0 + nrm(ks[17], (N_EVEN, 2 * B_DH), 0.02),
        'w_out_ab': nrm(ks[18], (N_EVEN, MIX_W, D_MODEL), MIX_W ** -0.5),
        'w_out_f': nrm(ks[19], (N_ODD, D_MODEL, D_MODEL), D_MODEL ** -0.5),
        'peer_wq': nrm(ks[20], (DEPTH, D_MODEL, PEER_HEADS * PEER_DKEY), D_MODEL ** -0.5),
        'peer_keys': nrm(ks[21], (DEPTH, PEER_HEADS, 2, PEER_NKEYS, PEER_DKEY // 2), (PEER_DKEY // 2) ** -0.5),
        'peer_u': nrm(ks[22], (DEPTH, PEER_EXPERTS, D_MODEL), D_MODEL ** -0.5),
        'peer_v': nrm(ks[23], (DEPTH, PEER_EXPERTS, D_MODEL), 1.0),
        'final_norm_w': 1.0 + nrm(ks[24], (D_MODEL,), 0.02),
    }


def reference(x, c, ctx, c_ctx, ada_w, ada_b, norm1_w, norm2_w, w_in, conv_w, a_log, dt_bias,
              gdn_norm_w, lam_q1, lam_k1, lam_q2, lam_k2, subln_w, w_out_ab, w_out_f, peer_wq,
              peer_keys, peer_u, peer_v, final_norm_w):
    bsz, n_lat = x.shape[0], x.shape[1]
    cos, sin = axial_rope(n_lat)
    last_ctx_reader = 2 * ((DEPTH - 1) // 2)
    c_act = jax.nn.silu(c)
    cc_act = jax.nn.silu(c_ctx)
    for i in range(DEPTH):
        ctx_in = i <= last_ctx_reader
        ctx_adv = i < last_ctx_reader
        mod = (c_act @ ada_w[i] + ada_b[i]).reshape(bsz, 6, 1, D_MODEL)
        sh1, sc1, g1, sh2, sc2, g2 = (mod[:, j] for j in range(6))
        h = rmsnorm(x, norm1_w[i]) * (1.0 + sc1) + sh1
        if ctx_in:
            mc = (cc_act @ ada_w[i] + ada_b[i]).reshape(6, 1, 1, D_MODEL)
            h_c = rmsnorm(ctx, norm1_w[i]) * (1.0 + mc[1]) + mc[0]
        j = i // 2
        if i % 2 == 0:
            y, y_c = mixer_ab(h, h_c, i, w_in[j], conv_w[j], a_log[j], dt_bias[j], gdn_norm_w[j],
                              lam_q1[j], lam_k1[j], lam_q2[j], lam_k2[j], subln_w[j], w_out_ab[j],
                              cos, sin, ctx_adv)
        else:
            y = fourier_mix(h) @ w_out_f[j]
            y_c = fourier_mix(h_c) @ w_out_f[j] if ctx_adv else None
        x = x + g1 * y
        x = x + g2 * peer(rmsnorm(x, norm2_w[i]) * (1.0 + sc2) + sh2,
                          peer_wq[i], peer_keys[i], peer_u[i], peer_v[i])
        if ctx_adv:
            ctx = ctx + mc[2] * y_c
            ctx = ctx + mc[5] * peer(rmsnorm(ctx, norm2_w[i]) * (1.0 + mc[4]) + mc[3],
                                     peer_wq[i], peer_keys[i], peer_u[i], peer_v[i])
    return rmsnorm(x, final_norm_w)
```

```python
import math
import numpy as np
import concourse.bass as bass
import concourse.mybir as mybir
from concourse.bass_utils import run_bass_kernel_spmd

F32 = mybir.dt.float32
I32 = mybir.dt.int32
U32 = mybir.dt.uint32
AF = mybir.ActivationFunctionType
ALU = mybir.AluOpType
AX = mybir.AxisListType

NCORES = 8
D = 1024
EPS = 1e-6


class Sched:
    LIMIT = 20000

    def __init__(self, nc):
        self.nc = nc
        self.eng = {'pe': nc.tensor, 'act': nc.scalar, 'dve': nc.vector, 'pool': nc.gpsimd, 'sp': nc.sync}
        self.epoch = {k: 0 for k in self.eng}
        self.sem = {(k, 0): nc.alloc_semaphore('s_%s_0' % k) for k in self.eng}
        self.cnt = {k: 0 for k in self.eng}
        self.seen = {k: {} for k in self.eng}
        self.ndsem = 24
        self.dsem = [nc.alloc_semaphore('d_%d' % i) for i in range(self.ndsem)]
        self.dcnt = [0] * self.ndsem
        self.dnext = 0
        self.lastw = {}
        self.readers = {}
        self.ninst = 0

    def _wait(self, e, tok, kindw):
        kind, key, val = tok
        if kind == 'e':
            src = key[0]
            if src == e and (e == 'pe' or kindw != 'raw'):
                return
        seen = self.seen[e]
        k = (kind, key)
        if seen.get(k, 0) >= val:
            return
        sem = self.sem[key] if kind == 'e' else self.dsem[key]
        self.eng[e].wait_ge(sem, val)
        seen[k] = val

    def _deps(self, e, reads, writes):
        for b in reads:
            t = self.lastw.get(b)
            if t is not None:
                self._wait(e, t, 'raw')
        for b in writes:
            t = self.lastw.get(b)
            if t is not None:
                self._wait(e, t, 'waw')
            for t in self.readers.get(b, ()):
                self._wait(e, t, 'war')

    def _commit(self, tok, reads, writes):
        for b in writes:
            self.lastw[b] = tok
            self.readers[b] = []
        for b in reads:
            if b in writes:
                continue
            self.readers.setdefault(b, []).append(tok)

    def op(self, e, fn, reads=(), writes=()):
        self._deps(e, reads, writes)
        ins = fn(self.eng[e])
        if self.cnt[e] >= self.LIMIT:
            self.epoch[e] += 1
            self.cnt[e] = 0
            self.sem[(e, self.epoch[e])] = self.nc.alloc_semaphore('s_%s_%d' % (e, self.epoch[e]))
        self.cnt[e] += 1
        key = (e, self.epoch[e])
        ins.then_inc(self.sem[key], 1)
        tok = ('e', key, self.cnt[e])
        self._commit(tok, reads, writes)
        self.ninst += 1
        return tok

    def dma(self, e, out, in_, reads=(), writes=(), **kw):
        return self.dmaf(e, lambda g: g.dma_start(out=out, in_=in_, **kw), reads, writes)

    def dmaf(self, e, fn, reads=(), writes=()):
        i = self.dnext
        self.dnext = (self.dnext + 1) % self.ndsem
        if self.dcnt[i] > 0:
            self._wait(e, ('d', i, self.dcnt[i]), 'raw')
        self._deps(e, reads, writes)
        ins = fn(self.eng[e])
        self.dcnt[i] += 16
        ins.then_inc(self.dsem[i], 16)
        tok = ('d', i, self.dcnt[i])
        self._commit(tok, reads, writes)
        self.ninst += 1
        return tok

    def finish(self, bufs, e='sp'):
        for b in bufs:
            t = self.lastw.get(b)
            if t is not None:
                self._wait(e, t, 'raw')


class Prog:
    def __init__(self):
        self.nc = bass.Bass("TRN2", target_bir_lowering=False)
        self.S = Sched(self.nc)
        self.outs = []
        self._n = 0

    def din(self, name, shape, dt=F32):
        return self.nc.dram_tensor(name, list(shape), dt, kind="ExternalInput").ap()

    def dout(self, name, shape, dt=F32):
        self.outs.append(name)
        return self.nc.dram_tensor(name, list(shape), dt, kind="ExternalOutput").ap()

    def sb(self, name, shape, dt=F32):
        return self.nc.alloc_sbuf_tensor(name, list(shape), dt)

    def ps(self, name, shape):
        return self.nc.alloc_psum_tensor(name, list(shape), F32)

    def ident(self):
        idf = self.sb("ident", [128, 128])
        S = self.S
        S.op('pool', lambda g: g.memset(idf[:], 1.0), writes=['ident'])
        S.op('pool', lambda g: g.affine_select(out=idf[:], in_=idf[:], pattern=[[-1, 128]], compare_op=ALU.is_equal,
                                               fill=0.0, base=0, channel_multiplier=1), reads=['ident'], writes=['ident'])
        return idf

    def run(self, in_maps):
        self.S.finish(self.outs)
        res = run_bass_kernel_spmd(self.nc, in_maps, core_ids=list(range(len(in_maps))))
        return res.results


def emit_mod(P, csil, v, adaw, adab, col0, ncols, outt, key, ones, tmpname, BW=256, pmx=None):
    S = P.S
    if not hasattr(P, '_modtmp'):
        P._modtmp = {}
    if tmpname not in P._modtmp:
        P._modtmp[tmpname] = (P.sb(tmpname + "_lt", [128, 8, 128]), [P.sb(tmpname + "_w%d" % i, [128, 8, BW]) for i in range(2)],
                              [P.ps(tmpname + "_pm%d" % i, [128, BW]) for i in range(2)] if pmx is None else None, P.sb(tmpname + "_b", [128, getattr(P, "mod_bw", 2048)]))
    lt, wts, pm, bt = P._modtmp[tmpname]
    pmk = [tmpname + '_pm0', tmpname + '_pm1']
    if pmx is not None:
        pm, pmk = pmx
    for k in range(8):
        S.op('dve', lambda e: e.tensor_scalar(out=lt[:, k, :], in0=ones[:, 0:128], scalar1=csil[:, v, k:k + 1], scalar2=None,
                                              op0=ALU.mult), reads=['csil', 'ones'], writes=[tmpname + '_lt%d' % k])
    wv = adaw.rearrange("(k p) n -> p k n", p=128)
    nb = (ncols + BW - 1) // BW
    S.dma('sp', bt[:, 0:ncols], adab[:, col0:col0 + ncols], writes=[tmpname + '_b'])
    for j in range(nb):
        c0 = col0 + j * BW
        w = min(BW, col0 + ncols - c0)
        wt = wts[j % 2]
        wk = tmpname + '_w%d' % (j % 2)
        S.dma('sp', wt[:, :, 0:w], wv[:, :, c0:c0 + w], writes=[wk])
        pk = pmk[j % 2]
        for k in range(8):
            S.op('pe', lambda e: e.matmul(pm[j % 2][:, 0:w], lhsT=lt[:, k, :], rhs=wt[:, k, 0:w], start=(k == 0), stop=(k == 7)),
                 reads=[wk, tmpname + '_lt%d' % k], writes=[pk])
        S.op('dve', lambda e: e.tensor_tensor(out=outt[:, j * BW:j * BW + w], in0=pm[j % 2][:, 0:w], in1=bt[:, j * BW:j * BW + w],
                                              op=ALU.add), reads=[pk, tmpname + '_b'], writes=[key])


def emit_rmsnorm_mod(P, xt, xkey, wmod, shb, modkeys, outt, okey, tmp):
    S = P.S
    junk, ss = tmp
    S.op('act', lambda e: e.activation(out=junk[:], in_=xt[:], func=AF.Square, accum_out=ss[:, 0:1]), reads=[xkey], writes=[okey, 'ss'])
    S.op('act', lambda e: e.activation(out=ss[:, 1:2], in_=ss[:, 0:1], func=AF.Sqrt, bias=P.epsb[:, 0:1], scale=1.0 / D), reads=['ss', 'epsb'], writes=['ss1'])
    S.op('dve', lambda e: e.reciprocal(out=ss[:, 2:3], in_=ss[:, 1:2]), reads=['ss1'], writes=['ss2'])
    S.op('dve', lambda e: e.scalar_tensor_tensor(out=outt[:], in0=xt[:], scalar=ss[:, 2:3], in1=wmod[:], op0=ALU.mult, op1=ALU.mult),
         reads=[xkey, 'ss2'] + modkeys, writes=[okey])
    if shb is not None:
        S.op('pool', lambda e: e.tensor_tensor(out=outt[:], in0=outt[:], in1=shb[:], op=ALU.add), reads=[okey] + modkeys, writes=[okey])


def consts(P):
    S = P.S
    P.ones = P.sb("ones", [128, 512])
    S.op('pool', lambda g: g.memset(P.ones[:], 1.0), writes=['ones'])
    P.epsb = P.sb("epsb", [128, 1])
    S.op('pool', lambda g: g.memset(P.epsb[:], EPS), writes=['epsb'])
    P.idf = P.ident()


L1_TILES = 33
IN_W = 3616


def build_l1():
    P = Prog()
    S = P.S
    X = P.din("X", [L1_TILES * 128, D])
    cT = P.din("cT", [128, 2, 8])
    adaw = P.din("adaw", [D, 2048])
    adab = P.din("adab", [128, 2048])
    n1w = P.din("n1w", [128, D])
    win = P.din("win", [D, IN_W])
    Pout = P.dout("P", [L1_TILES * 128, IN_W])
    consts(P)
    csil = P.sb("csil", [128, 2, 8])
    S.dma('sp', csil[:], cT, writes=['csil'])
    S.op('act', lambda e: e.activation(out=csil[:], in_=csil[:], func=AF.Silu), reads=['csil'], writes=['csil'])
    n1wt = P.sb("n1wt", [128, D])
    S.dma('sp', n1wt[:], n1w, writes=['n1w'])
    wint = P.sb("wint", [128, 8, IN_W])
    winv = win.rearrange("(k p) n -> p k n", p=128)
    for k in range(8):
        S.dma('sp', wint[:, k, :], winv[:, k, :], writes=['win%d' % k])
    mods = []
    for v in range(2):
        mb = P.sb("modb%d" % v, [128, 2048])
        emit_mod(P, csil, v, adaw, adab, 0, 2048, mb, 'modb%d' % v, P.ones, "m")
        wm = P.sb("wmod%d" % v, [128, D])
        S.op('dve', lambda e: e.scalar_tensor_tensor(out=wm[:], in0=mb[:, 1024:2048], scalar=1.0, in1=n1wt[:], op0=ALU.add, op1=ALU.mult),
             reads=['modb%d' % v, 'n1w'], writes=['wmod%d' % v])
        mods.append((wm, mb))
    xts = [P.sb("xt%d" % i, [128, D]) for i in range(2)]
    ss = P.sb("ss", [128, 4])
    ht = P.sb("ht", [128, D])
    junk = ht
    hT = P.sb("hT", [128, D])
    pT = P.ps("pT", [128, D])
    pp = [P.ps("pp%d" % i, [128, 512]) for i in range(4)]
    pts = [P.sb("pt0", [128, IN_W])] * 2
    for t in range(L1_TILES):
        v = 1 if t == L1_TILES - 1 else 0
        xt = xts[t % 2]
        xk = 'xt%d' % (t % 2)
        S.dma('sp', xt[:], X[t * 128:(t + 1) * 128, :], writes=[xk])
        wm, mb = mods[v]
        emit_rmsnorm_mod(P, xt, xk, wm, mb[:, 0:1024], ['wmod%d' % v, 'modb%d' % v], ht, 'ht', (junk, ss))
        for k in range(8):
            S.op('pe', lambda e: e.transpose(pT[:, k * 128:(k + 1) * 128], ht[:, k * 128:(k + 1) * 128], P.idf[:]),
                 reads=['ht', 'ident'], writes=['pT%d' % k])
        S.op('act', lambda e: e.activation(out=hT[:, 0:512], in_=pT[:, 0:512], func=AF.Copy), reads=['pT%d' % k for k in range(4)], writes=['hT0'])
        S.op('dve', lambda e: e.tensor_copy(out=hT[:, 512:1024], in_=pT[:, 512:1024]), reads=['pT%d' % k for k in range(4, 8)], writes=['hT1'])
        pt = pts[t % 2]
        ptk = 'pt0'
        ncb = (IN_W + 511) // 512
        for cb in range(ncb):
            c0 = cb * 512
            w = min(512, IN_W - c0)
            pq = pp[cb % 4]
            for k in range(8):
                S.op('pe', lambda e: e.matmul(pq[:, 0:w], lhsT=hT[:, k * 128:(k + 1) * 128], rhs=wint[:, k, c0:c0 + w], start=(k == 0), stop=(k == 7)),
                     reads=['hT%d' % (k // 4), 'win%d' % k], writes=['pp%d' % (cb % 4)])
            if cb % 2 == 0:
                S.op('act', lambda e: e.activation(out=pt[:, c0:c0 + w], in_=pq[:, 0:w], func=AF.Copy), reads=['pp%d' % (cb % 4)], writes=[ptk + '_%d' % cb])
            else:
                S.op('dve', lambda e: e.tensor_copy(out=pt[:, c0:c0 + w], in_=pq[:, 0:w]), reads=['pp%d' % (cb % 4)], writes=[ptk + '_%d' % cb])
        S.dma('pool', Pout[t * 128:(t + 1) * 128, :], pt[:], reads=[ptk + '_%d' % cb for cb in range(ncb)], writes=['P'])
    return P


def rep128(v):
    v = np.asarray(v, np.float32).reshape(1, -1)
    return np.ascontiguousarray(np.broadcast_to(v, (128, v.shape[1])))


def cT_layout(vecs):
    vecs = np.asarray(vecs, np.float32)
    return np.ascontiguousarray(vecs.reshape(vecs.shape[0], 8, 128).transpose(2, 0, 1))


def run_l1(x, c, ctx, c_ctx, ada_w0, ada_b0, norm1_w0, w_in0):
    P = build_l1()
    in_maps = []
    for core in range(NCORES):
        b, hf = core // 2, core % 2
        X = np.concatenate([x[b, hf * 4096:(hf + 1) * 4096], ctx[b, hf * 128:(hf + 1) * 128]], axis=0)
        in_maps.append(dict(X=np.ascontiguousarray(X), cT=cT_layout(np.stack([c[b], c_ctx])),
                            adaw=np.ascontiguousarray(ada_w0[:, :2048]), adab=rep128(ada_b0[:2048]),
                            n1w=rep128(norm1_w0), win=np.ascontiguousarray(w_in0)))
    res = P.run(in_maps)
    Plat = np.empty((4, 8192, IN_W), np.float32)
    Pctx = np.empty((4, 256, IN_W), np.float32)
    for core in range(NCORES):
        b, hf = core // 2, core % 2
        r = res[core]["P"]
        Plat[b, hf * 4096:(hf + 1) * 4096] = r[:4096]
        Pctx[b, hf * 128:(hf + 1) * 128] = r[4096:]
    return Plat, Pctx


def bc(ap, axis, n):
    a = ap.unsqueeze(axis)
    shp = list(a.shape)
    shp[axis] = n
    return a.broadcast_to(shp)


def build_l2():
    P = Prog()
    S = P.S
    NT = L1_TILES
    R = NT * 128
    Pp = P.din("Pp", [R, 1536]); Pc = P.din("Pc", [R, 1536]); Pn = P.din("Pn", [R, 1536])
    Pab = P.din("Pab", [R, 32]); Pqk = P.din("Pqk", [R, 1024])
    cosT = P.din("cosT", [R, 32]); sinT = P.din("sinT", [R, 32])
    convw = P.din("convw", [128, 3, 1536]); alog = P.din("alog", [128, 16]); dtb = P.din("dtb", [128, 16])
    QKV = P.dout("QKV", [R, 1536]); BG = P.dout("BG", [R, 32]); QKr = P.dout("QKr", [R, 1024])
    consts(P)
    cw = P.sb("cw", [128, 3, 1536]); S.dma('sp', cw[:], convw, writes=['cw'])
    negA = P.sb("negA", [128, 16]); S.dma('sp', negA[:], alog, writes=['negA'])
    S.op('act', lambda e: e.activation(out=negA[:], in_=negA[:], func=AF.Exp), reads=['negA'], writes=['negA'])
    S.op('dve', lambda e: e.tensor_scalar(out=negA[:], in0=negA[:], scalar1=-1.0, scalar2=None, op0=ALU.mult), reads=['negA'], writes=['negA'])
    dtbt = P.sb("dtbt", [128, 16]); S.dma('sp', dtbt[:], dtb, writes=['dtbt'])
    a0 = P.sb("a0", [128, 1536]); a1 = P.sb("a1", [128, 1536]); a2 = P.sb("a2", [128, 1536])
    qkv = P.sb("qkv", [128, 1536]); sq = P.sb("sq", [128, 1024]); st = P.sb("st", [128, 3, 16])
    ab = P.sb("ab", [128, 32]); bg = P.sb("bg", [128, 32]); tt = P.sb("tt", [128, 16])
    qk = P.sb("qk", [128, 1024]); qo = P.sb("qo", [128, 1024]); cs = P.sb("cs", [128, 2, 32])
    r0 = P.sb("r0", [128, 512]); r1 = P.sb("r1", [128, 512])
    for t in range(NT):
        rs = slice(t * 128, (t + 1) * 128)
        S.dma('sp', a0[:], Pp[rs, :], writes=['a0']); S.dma('sp', a1[:], Pc[rs, :], writes=['a1']); S.dma('sp', a2[:], Pn[rs, :], writes=['a2'])
        S.dma('sp', ab[:], Pab[rs, :], writes=['ab']); S.dma('sp', qk[:], Pqk[rs, :], writes=['qk'])
        S.dma('sp', cs[:, 0, :], cosT[rs, :], writes=['cs0']); S.dma('sp', cs[:, 1, :], sinT[rs, :], writes=['cs1'])
        S.op('dve', lambda e: e.tensor_tensor(out=a0[:], in0=a0[:], in1=cw[:, 0, :], op=ALU.mult), reads=['a0', 'cw'], writes=['a0'])
        S.op('pool', lambda e: e.tensor_tensor(out=a1[:], in0=a1[:], in1=cw[:, 1, :], op=ALU.mult), reads=['a1', 'cw'], writes=['a1'])
        S.op('dve', lambda e: e.tensor_tensor(out=a2[:], in0=a2[:], in1=cw[:, 2, :], op=ALU.mult), reads=['a2', 'cw'], writes=['a2'])
        S.op('pool', lambda e: e.tensor_tensor(out=a1[:], in0=a1[:], in1=a0[:], op=ALU.add), reads=['a1', 'a0'], writes=['a1'])
        S.op('dve', lambda e: e.tensor_tensor(out=a1[:], in0=a1[:], in1=a2[:], op=ALU.add), reads=['a1', 'a2'], writes=['a1'])
        S.op('act', lambda e: e.activation(out=qkv[:], in_=a1[:], func=AF.Silu), reads=['a1'], writes=['qkv'])
        S.op('act', lambda e: e.activation(out=sq[:], in_=qkv[:, 0:1024], func=AF.Square), reads=['qkv'], writes=['sq'])
        S.op('dve', lambda e: e.tensor_reduce(out=st[:, 0, :], in_=sq[:].rearrange("p (g d) -> p g d", d=64), axis=AX.X, op=ALU.add), reads=['sq'], writes=['st0'])
        S.op('act', lambda e: e.activation(out=st[:, 1, :], in_=st[:, 0, :], func=AF.Sqrt, bias=P.epsb[:, 0:1], scale=1.0), reads=['st0', 'epsb'], writes=['st1'])
        S.op('dve', lambda e: e.reciprocal(out=st[:, 2, :], in_=st[:, 1, :]), reads=['st1'], writes=['st2'])
        S.op('dve', lambda e: e.tensor_scalar(out=st[:, 2, 0:8], in0=st[:, 2, 0:8], scalar1=0.125, scalar2=None, op0=ALU.mult), reads=['st2'], writes=['st2'])
        S.op('dve', lambda e: e.tensor_tensor(out=qkv[:, 0:1024].rearrange("p (g d) -> p g d", d=64), in0=qkv[:, 0:1024].rearrange("p (g d) -> p g d", d=64),
                                              in1=bc(st[:, 2, :], 2, 64), op=ALU.mult), reads=['qkv', 'st2'], writes=['qkv'])
        S.dma('pool', QKV[rs, :], qkv[:], reads=['qkv'], writes=['QKV'])
        S.op('act', lambda e: e.activation(out=bg[:, 0:16], in_=ab[:, 0:16], func=AF.Sigmoid), reads=['ab'], writes=['bg0'])
        S.op('dve', lambda e: e.tensor_tensor(out=tt[:], in0=ab[:, 16:32], in1=dtbt[:], op=ALU.add), reads=['ab', 'dtbt'], writes=['tt'])
        S.op('act', lambda e: e.activation(out=tt[:], in_=tt[:], func=AF.Exp), reads=['tt'], writes=['tt'])
        S.op('act', lambda e: e.activation(out=tt[:], in_=tt[:], func=AF.Ln, bias=P.ones[:, 0:1], scale=1.0), reads=['tt', 'ones'], writes=['tt'])
        S.op('dve', lambda e: e.tensor_tensor(out=bg[:, 16:32], in0=tt[:], in1=negA[:], op=ALU.mult), reads=['tt', 'negA'], writes=['bg1'])
        S.dma('pool', BG[rs, :], bg[:], reads=['bg0', 'bg1'], writes=['BG'])
        v = qk[:].rearrange("p (g h d) -> p g h d", h=2, d=32)
        o = qo[:].rearrange("p (g h d) -> p g h d", h=2, d=32)
        cb = bc(cs[:, 0, :], 1, 16); sb_ = bc(cs[:, 1, :], 1, 16)
        r0v = r0[:].rearrange("p (g d) -> p g d", d=32); r1v = r1[:].rearrange("p (g d) -> p g d", d=32)
        S.op('dve', lambda e: e.tensor_tensor(out=r0v, in0=v[:, :, 0, :], in1=cb, op=ALU.mult), reads=['qk', 'cs0'], writes=['r0'])
        S.op('pool', lambda e: e.tensor_tensor(out=r1v, in0=v[:, :, 1, :], in1=sb_, op=ALU.mult), reads=['qk', 'cs1'], writes=['r1'])
        S.op('dve', lambda e: e.tensor_tensor(out=o[:, :, 0, :], in0=r0v, in1=r1v, op=ALU.subtract), reads=['r0', 'r1'], writes=['qo0'])
        S.op('pool', lambda e: e.tensor_tensor(out=r0v, in0=v[:, :, 0, :], in1=sb_, op=ALU.mult), reads=['qk', 'cs1', 'r0'], writes=['r0'])
        S.op('dve', lambda e: e.tensor_tensor(out=r1v, in0=v[:, :, 1, :], in1=cb, op=ALU.mult), reads=['qk', 'cs0', 'r1'], writes=['r1'])
        S.op('pool', lambda e: e.tensor_tensor(out=o[:, :, 1, :], in0=r0v, in1=r1v, op=ALU.add), reads=['r0', 'r1'], writes=['qo1'])
        S.dma('pool', QKr[rs, :], qo[:], reads=['qo0', 'qo1'], writes=['QKr'])
    return P


def rope_tables():
    rows = 8192 // 64
    row = np.repeat(np.arange(rows, dtype=np.float32), 64)
    col = np.tile(np.arange(64, dtype=np.float32), rows)
    inv = (10000.0 ** (-np.arange(0, 32, 2, dtype=np.float32) / 32)).astype(np.float32)
    ang = np.concatenate([row[:, None] * inv, col[:, None] * inv], axis=-1).astype(np.float32)
    return np.cos(ang).astype(np.float32), np.sin(ang).astype(np.float32)


def shift_rows(a, s):
    b = np.zeros_like(a)
    if s == -1:
        b[..., 1:, :] = a[..., :-1, :]
    else:
        b[..., :-1, :] = a[..., 1:, :]
    return b


def run_l2(Plat, Pctx, conv_w0, a_log0, dt_bias0):
    P = build_l2()
    cos, sin = rope_tables()
    in_maps = []
    for core in range(NCORES):
        b, hf = core // 2, core % 2
        sl, sc = slice(hf * 4096, (hf + 1) * 4096), slice(hf * 128, (hf + 1) * 128)
        cat = lambda A, B: np.ascontiguousarray(np.concatenate([A, B], axis=0))
        ql, qc = Plat[b, :, :1536], Pctx[b, :, :1536]
        in_maps.append(dict(
            Pp=cat(shift_rows(ql, -1)[sl], shift_rows(qc, -1)[sc]), Pc=cat(ql[sl], qc[sc]), Pn=cat(shift_rows(ql, 1)[sl], shift_rows(qc, 1)[sc]),
            Pab=cat(Plat[b, sl, 2048:2080], Pctx[b, sc, 2048:2080]), Pqk=cat(Plat[b, sl, 2080:3104], Pctx[b, sc, 2080:3104]),
            cosT=cat(cos[sl], np.ones((128, 32), np.float32)), sinT=cat(sin[sl], np.zeros((128, 32), np.float32)),
            convw=np.ascontiguousarray(np.broadcast_to(conv_w0[None], (128, 3, 1536))), alog=rep128(a_log0.reshape(-1)), dtb=rep128(dt_bias0.reshape(-1))))
    res = P.run(in_maps)
    out = {}
    for name, w in (("QKV", 1536), ("BG", 32), ("QKr", 1024)):
        lat = np.empty((4, 8192, w), np.float32); cx = np.empty((4, 256, w), np.float32)
        for core in range(NCORES):
            b, hf = core // 2, core % 2
            r = res[core][name]
            lat[b, hf * 4096:(hf + 1) * 4096] = r[:4096]
            cx[b, hf * 128:(hf + 1) * 128] = r[4096:]
        out[name] = (lat, cx)
    return out


GD_CH = 132
GD_CTX = 4


import os
LIM = int(os.environ.get('LIM', '99'))


def build_l3(nch=GD_CH, nctx=GD_CTX, stage=9):
    P = Prog()
    S = P.S
    T = nch * 64
    Kt = P.din("Kt", [T, 512]); Vt = P.din("Vt", [T, 512])
    KT = P.din("KT", [nch, 64, 512]); QT = P.din("QT", [nch, 64, 512])
    Bt = P.din("Bt", [T, 8]); Gt = P.din("Gt", [T, 8])
    O = P.dout("O", [(nch - nctx) * 64, 512])
    N = 64

    def mask(name, op, sgn=1):
        m = P.sb(name, [N, N])
        S.op('pool', lambda g: g.memset(m[:], 1.0), writes=[name])
        S.op('pool', lambda g: g.affine_select(out=m[:], in_=m[:], pattern=[[-sgn, N]], compare_op=op, fill=0.0, base=0, channel_multiplier=sgn),
             reads=[name], writes=[name])
        return m
    mL = mask("mL", ALU.is_ge); mLs = mask("mLs", ALU.is_gt); mU = mask("mU", ALU.is_ge, -1); mUs = mask("mUs", ALU.is_gt, -1); I64 = mask("I64", ALU.is_equal)
    ones = P.sb("ones64", [N, N]); S.op('pool', lambda g: g.memset(ones[:], 1.0), writes=['ones64'])
    B = [P.ps("b%d" % i, [N, 512]) for i in range(8)]
    w3 = lambda t: t[:].rearrange("p (h j) -> p h j", j=64)
    sbw = lambda name: P.sb(name, [N, 512])
    kt = sbw("kt"); vt = sbw("vt"); kT = sbw("kT"); qT = sbw("qT")
    b8 = P.sb("b8", [N, 8]); g8 = P.sb("g8", [N, 8]); gc = P.sb("gc", [N, 8]); egc = P.sb("egc", [N, 8]); nb8 = P.sb("nb8", [N, 8]); bge = P.sb("bge", [N, 8])
    R = sbw("R"); arg = sbw("arg"); expgB = sbw("expgB"); t1 = sbw("t1"); t2 = sbw("t2")
    Dls = sbw("Dls"); Dus = sbw("Dus"); Du = sbw("Du"); E2 = sbw("E2")
    Q = [sbw("Q0"), sbw("Q1")]; QTt = [sbw("QT0"), sbw("QT1")]; PT = [sbw("PT0"), sbw("PT1")]
    AT = sbw("AT"); vb = sbw("vb"); kbg = sbw("kbg"); U = sbw("U"); WT = sbw("WT"); ktl = sbw("ktl"); qdT = sbw("qdT")
    vnew = sbw("vnew"); Sst = sbw("Sst"); ot = sbw("ot"); stmp = sbw("stmp")
    S.op('pool', lambda g: g.memset(Sst[:], 0.0), writes=['Sst'])
    hs = lambda t, h: t[:, h * 64:(h + 1) * 64]

    for c in range(nch):
        rs = slice(c * 64, (c + 1) * 64)
        S.dma('sp', kt[:], Kt[rs, :], writes=['kt']); S.dma('sp', vt[:], Vt[rs, :], writes=['vt'])
        S.dma('sp', kT[:], KT[c], writes=['kT']); S.dma('sp', qT[:], QT[c], writes=['qT'])
        S.dma('sp', b8[:], Bt[rs, :], writes=['b8']); S.dma('sp', g8[:], Gt[rs, :], writes=['g8'])
        if stage < -3: continue
        S.op('dve', lambda e: e.tensor_tensor(out=w3(R), in0=bc(g8[:], 2, 64), in1=bc(mU[:], 1, 8), op=ALU.mult), reads=['g8', 'mU'], writes=['R'])
        S.op('pe', lambda e: e.matmul(B[0][:], lhsT=ones[:], rhs=R[:], start=True, stop=True), reads=['ones64', 'R'], writes=['b0'])
        if stage < -2: continue
        S.op('pe', lambda e: e.matmul(B[4][:, 0:8], lhsT=mU[:], rhs=g8[:], start=True, stop=True), reads=['mU', 'g8'], writes=['b4'])
        S.op('act', lambda e: e.activation(out=gc[:], in_=B[4][:, 0:8], func=AF.Copy), reads=['b4'], writes=['gc'])
        if stage < -1: continue
        if LIM > 0:
            S.op('dve', lambda e: e.tensor_tensor(out=w3(arg), in0=bc(gc[:], 2, 64), in1=w3(B[0]), op=ALU.subtract), reads=['gc', 'b0'], writes=['arg'])
        if LIM > 1:
            S.op('dve', lambda e: e.tensor_scalar(out=expgB[:], in0=B[0][:], scalar1=-80.0, scalar2=None, op0=ALU.max), reads=['b0'], writes=['expgB'])
            S.op('act', lambda e: e.activation(out=expgB[:], in_=expgB[:], func=AF.Exp), reads=['expgB'], writes=['expgB'])
        if LIM > 2:
            S.op('dve', lambda e: e.tensor_scalar(out=egc[:], in0=gc[:], scalar1=-80.0, scalar2=None, op0=ALU.max), reads=['gc'], writes=['egc'])
            S.op('act', lambda e: e.activation(out=egc[:], in_=egc[:], func=AF.Exp), reads=['egc'], writes=['egc'])
        if LIM > 3:
            S.op('dve', lambda e: e.tensor_scalar(out=t1[:], in0=arg[:], scalar1=0.0, scalar2=-80.0, op0=ALU.min, op1=ALU.max), reads=['arg'], writes=['t1'])
        if LIM > 4:
            S.op('act', lambda e: e.activation(out=t1[:], in_=t1[:], func=AF.Exp), reads=['t1'], writes=['t1'])
        if LIM > 5:
            S.op('dve', lambda e: e.tensor_tensor(out=w3(Dls), in0=w3(t1), in1=bc(mLs[:], 1, 8), op=ALU.mult), reads=['t1', 'mLs'], writes=['Dls'])
        if LIM > 6:
            S.op('dve', lambda e: e.tensor_scalar(out=t2[:], in0=arg[:], scalar1=-1.0, scalar2=0.0, op0=ALU.mult, op1=ALU.min), reads=['arg'], writes=['t2'])
            S.op('dve', lambda e: e.tensor_scalar(out=t2[:], in0=t2[:], scalar1=-80.0, scalar2=None, op0=ALU.max), reads=['t2'], writes=['t2'])
        if LIM > 7:
            S.op('act', lambda e: e.activation(out=E2[:], in_=t2[:], func=AF.Exp), reads=['t2'], writes=['E2'])
        if LIM > 8:
            S.op('dve', lambda e: e.tensor_tensor(out=w3(Dus), in0=w3(E2), in1=bc(mUs[:], 1, 8), op=ALU.mult), reads=['E2', 'mUs'], writes=['Dus'])
        if LIM > 9:
            S.op('pool', lambda e: e.tensor_tensor(out=w3(Du), in0=w3(E2), in1=bc(mU[:], 1, 8), op=ALU.mult), reads=['E2', 'mU'], writes=['Du'])
        if stage < 1: continue
        S.op('dve', lambda e: e.tensor_tensor(out=w3(R), in0=bc(b8[:], 2, 64), in1=bc(I64[:], 1, 8), op=ALU.mult), reads=['b8', 'I64', 'R'], writes=['R'])
        S.op('pe', lambda e: e.matmul(B[1][:], lhsT=ones[:], rhs=R[:], start=True, stop=True), reads=['ones64', 'R'], writes=['b1'])
        S.op('dve', lambda e: e.tensor_scalar(out=nb8[:], in0=b8[:], scalar1=-1.0, scalar2=None, op0=ALU.mult), reads=['b8'], writes=['nb8'])
        if stage < 2: continue
        for h in range(8):
            S.op('pe', lambda e: e.matmul(hs(B[2], h), lhsT=hs(kT, h), rhs=hs(kT, h), start=True, stop=True), reads=['kT'], writes=['b2'])
        for h in range(8):
            S.op('pe', lambda e: e.matmul(hs(B[3], h), lhsT=hs(kT, h), rhs=hs(qT, h), start=True, stop=True), reads=['kT', 'qT'], writes=['b3'])
        if stage < 3: continue
        S.op('dve', lambda e: e.tensor_tensor(out=t1[:], in0=B[2][:], in1=Dls[:], op=ALU.mult), reads=['b2', 'Dls', 't1'], writes=['t1'])
        S.op('dve', lambda e: e.tensor_tensor(out=w3(Q[0]), in0=w3(t1), in1=bc(nb8[:], 2, 64), op=ALU.mult), reads=['t1', 'nb8'], writes=['Q0'])
        S.op('dve', lambda e: e.tensor_tensor(out=t2[:], in0=B[2][:], in1=Dus[:], op=ALU.mult), reads=['b2', 'Dus', 't2'], writes=['t2'])
        S.op('dve', lambda e: e.scalar_tensor_tensor(out=QTt[0][:], in0=B[1][:], scalar=-1.0, in1=t2[:], op0=ALU.mult, op1=ALU.mult), reads=['b1', 't2'], writes=['QT0'])
        S.op('dve', lambda e: e.tensor_tensor(out=AT[:], in0=B[3][:], in1=Du[:], op=ALU.mult), reads=['b3', 'Du'], writes=['AT'])
        S.op('pool', lambda e: e.tensor_tensor(out=w3(PT[0]), in0=w3(QTt[0]), in1=bc(I64[:], 1, 8), op=ALU.add), reads=['QT0', 'I64'], writes=['PT0'])
        if stage < 4: continue
        cur = 0
        for l in range(5):
            nx = 1 - cur
            qk_, qtk, qn, qtn = 'Q%d' % cur, 'QT%d' % cur, 'Q%d' % nx, 'QT%d' % nx
            for h in range(8):
                S.op('pe', lambda e: e.matmul(hs(B[4], h), lhsT=hs(QTt[cur], h), rhs=hs(Q[cur], h), start=True, stop=True), reads=[qk_, qtk], writes=['b4'])
            if l < 4:
                for h in range(8):
                    S.op('pe', lambda e: e.matmul(hs(B[5], h), lhsT=hs(Q[cur], h), rhs=hs(QTt[cur], h), start=True, stop=True), reads=[qk_, qtk], writes=['b5'])
            S.op('act', lambda e: e.activation(out=Q[nx][:], in_=B[4][:], func=AF.Copy), reads=['b4'], writes=[qn])
            if l < 4:
                S.op('dve', lambda e: e.tensor_copy(out=QTt[nx][:], in_=B[5][:]), reads=['b5'], writes=[qtn])
            pk, pn = 'PT%d' % (l % 2), 'PT%d' % ((l + 1) % 2)
            for h in range(8):
                S.op('pe', lambda e: e.matmul(hs(B[6], h), lhsT=hs(Q[nx], h), rhs=hs(PT[l % 2], h), start=True, stop=True), reads=[qn, pk], writes=['b6'])
            S.op('dve', lambda e: e.tensor_tensor(out=PT[(l + 1) % 2][:], in0=B[6][:], in1=PT[l % 2][:], op=ALU.add), reads=['b6', pk], writes=[pn])
            cur = nx
        TT = PT[1]; ttk = 'PT1'
        if stage < 5: continue
        S.op('pool', lambda e: e.tensor_tensor(out=w3(vb), in0=w3(vt), in1=bc(b8[:], 2, 64), op=ALU.mult), reads=['vt', 'b8'], writes=['vb'])
        S.op('dve', lambda e: e.tensor_tensor(out=bge[:], in0=b8[:], in1=egc[:], op=ALU.mult), reads=['b8', 'egc'], writes=['bge'])
        S.op('dve', lambda e: e.tensor_tensor(out=w3(kbg), in0=w3(kt), in1=bc(bge[:], 2, 64), op=ALU.mult), reads=['kt', 'bge'], writes=['kbg'])
        S.op('pool', lambda e: e.tensor_tensor(out=w3(ktl), in0=w3(kt), in1=bc(w3(E2)[:, :, 63], 2, 64), op=ALU.mult), reads=['kt', 'E2'], writes=['ktl'])
        S.op('dve', lambda e: e.tensor_tensor(out=qdT[:], in0=qT[:], in1=expgB[:], op=ALU.mult), reads=['qT', 'expgB'], writes=['qdT'])
        for h in range(8):
            S.op('pe', lambda e: e.matmul(hs(B[0], h), lhsT=hs(TT, h), rhs=hs(vb, h), start=True, stop=True), reads=[ttk, 'vb'], writes=['b0'])
        for h in range(8):
            S.op('pe', lambda e: e.matmul(hs(B[1], h), lhsT=hs(kbg, h), rhs=hs(TT, h), start=True, stop=True), reads=[ttk, 'kbg'], writes=['b1'])
        S.op('act', lambda e: e.activation(out=U[:], in_=B[0][:], func=AF.Copy), reads=['b0'], writes=['U'])
        S.op('dve', lambda e: e.tensor_copy(out=WT[:], in_=B[1][:]), reads=['b1'], writes=['WT'])
        if stage < 6: continue
        for h in range(8):
            S.op('pe', lambda e: e.matmul(hs(B[2], h), lhsT=hs(WT, h), rhs=hs(Sst, h), start=True, stop=True), reads=['WT', 'Sst'], writes=['b2'])
        S.op('dve', lambda e: e.tensor_tensor(out=vnew[:], in0=U[:], in1=B[2][:], op=ALU.subtract), reads=['U', 'b2'], writes=['vnew'])
        if c >= nctx:
            for h in range(8):
                S.op('pe', lambda e: e.matmul(hs(B[3], h), lhsT=hs(qdT, h), rhs=hs(Sst, h), start=True, stop=False), reads=['qdT', 'Sst'], writes=['b3'])
                S.op('pe', lambda e: e.matmul(hs(B[3], h), lhsT=hs(AT, h), rhs=hs(vnew, h), start=False, stop=True), reads=['AT', 'vnew'], writes=['b3'])
            S.op('act', lambda e: e.activation(out=ot[:], in_=B[3][:], func=AF.Copy), reads=['b3'], writes=['ot'])
            S.dma('pool', O[(c - nctx) * 64:(c - nctx + 1) * 64, :], ot[:], reads=['ot'], writes=['O'])
        for h in range(8):
            S.op('pe', lambda e: e.matmul(hs(B[7], h), lhsT=hs(ktl, h), rhs=hs(vnew, h), start=True, stop=True), reads=['ktl', 'vnew'], writes=['b7'])
        S.op('dve', lambda e: e.tensor_tensor(out=w3(stmp), in0=w3(Sst), in1=bc(w3(expgB)[:, :, 63], 2, 64), op=ALU.mult), reads=['Sst', 'expgB'], writes=['stmp'])
        S.op('dve', lambda e: e.tensor_tensor(out=Sst[:], in0=stmp[:], in1=B[7][:], op=ALU.add), reads=['stmp', 'b7'], writes=['Sst'])
    if stage < 9:
        S.dma('pool', O[0:64, :], Sst[:], reads=['Sst'], writes=['O'])
    return P


def run_l3(QKV, BG, nch=GD_CH, nctx=GD_CTX, stage=9):
    P = build_l3(nch, nctx, stage)
    L = (nch - nctx) * 64
    C = nctx * 64
    in_maps = []
    for core in range(NCORES):
        b, dr = core // 2, core % 2
        def seq(lat, cx):
            a, c_ = lat[b, :L], cx[b, :C]
            if dr == 1:
                a, c_ = a[::-1], c_[::-1]
            return np.concatenate([c_, a], axis=0)
        qkv = seq(QKV[0], QKV[1]); bg = seq(BG[0], BG[1])
        q, k, v = qkv[:, :512], qkv[:, 512:1024], qkv[:, 1024:1536]
        fm = lambda a: np.ascontiguousarray(a.reshape(nch, 64, 8, 64).transpose(0, 3, 2, 1).reshape(nch, 64, 512))
        in_maps.append(dict(Kt=np.ascontiguousarray(k), Vt=np.ascontiguousarray(v), KT=fm(k), QT=fm(q),
                            Bt=np.ascontiguousarray(bg[:, dr * 8:dr * 8 + 8]), Gt=np.ascontiguousarray(bg[:, 16 + dr * 8:16 + dr * 8 + 8])))
    res = P.run(in_maps)
    O = np.empty((4, 2, L, 512), np.float32)
    for core in range(NCORES):
        b, dr = core // 2, core % 2
        o = res[core]["O"]
        O[b, dr] = o[::-1] if dr == 1 else o
    return O


BF16 = mybir.dt.bfloat16
NKEY = 8448
NKT = NKEY // 128


def build_l4(nq=8192, lam_init=0.2):
    P = Prog()
    S = P.S
    qT = P.din("qT", [2, 2, 64, nq]); kT = P.din("kT", [2, 2, 64, NKEY]); V = P.din("V", [2, NKEY, 128])
    lam = P.din("lam", [128, 4, 64])
    DT = P.dout("DT", [2, 128, nq])
    consts(P)
    onesb = P.sb("onesb", [128, 128], BF16)
    S.op('dve', lambda e: e.tensor_copy(out=onesb[:], in_=P.ones[:, 0:128]), reads=['ones'], writes=['onesb'])
    lt = P.sb("lamt", [128, 4, 64]); S.dma('sp', lt[:], lam, writes=['lamt'])
    lp = P.sb("lamp", [128, 2, 64]); ls = P.sb("lams", [128, 4])
    S.op('dve', lambda e: e.tensor_tensor(out=lp[:, 0, :], in0=lt[:, 0, :], in1=lt[:, 1, :], op=ALU.mult), reads=['lamt'], writes=['lamp0'])
    S.op('dve', lambda e: e.tensor_tensor(out=lp[:, 1, :], in0=lt[:, 2, :], in1=lt[:, 3, :], op=ALU.mult), reads=['lamt'], writes=['lamp1'])
    S.op('dve', lambda e: e.tensor_reduce(out=ls[:, 0:2], in_=lp[:], axis=AX.X, op=ALU.add), reads=['lamp0', 'lamp1'], writes=['lams'])
    S.op('act', lambda e: e.activation(out=ls[:, 0:2], in_=ls[:, 0:2], func=AF.Exp), reads=['lams'], writes=['lams'])
    S.op('dve', lambda e: e.tensor_tensor(out=ls[:, 2:3], in0=ls[:, 1:2], in1=ls[:, 0:1], op=ALU.subtract), reads=['lams'], writes=['lams2'])
    S.op('dve', lambda e: e.tensor_scalar(out=ls[:, 3:4], in0=ls[:, 2:3], scalar1=-lam_init, scalar2=None, op0=ALU.add), reads=['lams2'], writes=['neglam'])
    kTt = P.sb("kTt", [64, 2, NKEY]); Vf = P.sb("Vf", [128, NKT, 128]); Vb = P.sb("Vb", [128, NKT, 128], BF16)
    qTt = [P.sb("qTt%d" % i, [64, 2, 512]) for i in range(2)]
    Pt = [P.sb("Pt%d" % i, [128, 512], BF16) for i in range(3)]
    pS = [P.ps("pS%d" % i, [128, 512]) for i in range(2)]
    pO = [P.ps("pO%d" % i, [128, 512]) for i in range(2)]
    pZ = [P.ps("pZ%d" % i, [128, 512]) for i in range(2)]
    rz = P.sb("rz", [128, 512]); Om = [P.sb("Om%d" % i, [128, 512]) for i in range(2)]
    Dt = [P.sb("Dt%d" % i, [128, 512]) for i in range(2)]
    it = 0
    for h in range(2):
        S.dma('sp', kTt[:], kT[h].rearrange("m d k -> d m k"), writes=['kTt'])
        S.dma('sp', Vf[:], V[h].rearrange("(t p) e -> p t e", p=128), writes=['Vf'])
        S.op('dve', lambda e: e.tensor_copy(out=Vb[:], in_=Vf[:]), reads=['Vf'], writes=['Vb'])
        for qc in range(nq // 512):
            qt = qTt[qc % 2]; qk = 'qTt%d' % (qc % 2)
            S.dma('sp', qt[:], qT[h, :, :, qc * 512:(qc + 1) * 512].rearrange("m d k -> d m k"), writes=[qk])
            for m in range(2):
                for kt in range(NKT):
                    ps = pS[it % 2]; psk = 'pS%d' % (it % 2)
                    pt = Pt[it % 3]; ptk = 'Pt%d' % (it % 3)
                    it += 1
                    S.op('pe', lambda e: e.matmul(ps[:], lhsT=kTt[:, m, kt * 128:(kt + 1) * 128], rhs=qt[:, m, :], start=True, stop=True),
                         reads=['kTt', qk], writes=[psk])
                    S.op('act', lambda e: e.activation(out=pt[:], in_=ps[:], func=AF.Exp, scale=0.125), reads=[psk], writes=[ptk])
                    S.op('pe', lambda e: e.matmul(pO[m][:], lhsT=Vb[:, kt, :], rhs=pt[:], start=(kt == 0), stop=(kt == NKT - 1)),
                         reads=['Vb', ptk], writes=['pO%d' % m])
                    S.op('pe', lambda e: e.matmul(pZ[m][:], lhsT=onesb[:], rhs=pt[:], start=(kt == 0), stop=(kt == NKT - 1)),
                         reads=['onesb', ptk], writes=['pZ%d' % m])
                S.op('dve', lambda e: e.reciprocal(out=rz[:], in_=pZ[m][:]), reads=['pZ%d' % m], writes=['rz'])
                S.op('dve', lambda e: e.tensor_tensor(out=Om[m][:], in0=pO[m][:], in1=rz[:], op=ALU.mult), reads=['pO%d' % m, 'rz'], writes=['Om%d' % m])
            dt_ = Dt[qc % 2]; dk = 'Dt%d' % (qc % 2)
            S.op('dve', lambda e: e.scalar_tensor_tensor(out=dt_[:], in0=Om[1][:], scalar=ls[:, 3:4], in1=Om[0][:], op0=ALU.mult, op1=ALU.add),
                 reads=['Om0', 'Om1', 'neglam'], writes=[dk])
            S.dma('pool', DT[h, :, qc * 512:(qc + 1) * 512], dt_[:], reads=[dk], writes=['DT'])
    return P


def run_l4(QKr, Plat, Pctx, lam_q1, lam_k1, lam_q2, lam_k2, nq=8192):
    P = build_l4(nq)
    lamin = np.ascontiguousarray(np.broadcast_to(np.stack([lam_q1, lam_k1, lam_q2, lam_k2])[None], (128, 4, 64))).astype(np.float32)
    in_maps = []
    for core in range(NCORES):
        b, hp = core // 2, core % 2
        q = QKr[0][b, :nq, 0:512].reshape(nq, 4, 2, 64)[:, 2 * hp:2 * hp + 2]
        k = np.concatenate([QKr[0][b, :, 512:1024], QKr[1][b, :, 512:1024]], axis=0).reshape(NKEY, 4, 2, 64)[:, 2 * hp:2 * hp + 2]
        v = np.concatenate([Plat[b, :, 3104:3616], Pctx[b, :, 3104:3616]], axis=0).reshape(NKEY, 4, 128)[:, 2 * hp:2 * hp + 2]
        in_maps.append(dict(qT=np.ascontiguousarray(q.transpose(1, 2, 3, 0)), kT=np.ascontiguousarray(k.transpose(1, 2, 3, 0)),
                            V=np.ascontiguousarray(v.transpose(1, 0, 2)), lam=lamin))
    res = P.run(in_maps)
    out = np.empty((4, nq, 512), np.float32)
    for core in range(NCORES):
        b, hp = core // 2, core % 2
        dt = res[core]["DT"]
        out[b, :, hp * 256:(hp + 1) * 256] = dt.transpose(2, 0, 1).reshape(nq, 256)
    return out


TOK = 4096
NTT = TOK // 128


def build_l5a(kind):
    P = Prog()
    S = P.S
    X = P.din("X", [TOK, D])
    if kind == 'ab':
        O0 = P.din("O0", [TOK, 512]); O1 = P.din("O1", [TOK, 512]); GATE = P.din("GATE", [TOK, 512]); DL = P.din("DL", [TOK, 512])
        gdnw = P.din("gdnw", [128, 64]); sublnw = P.din("sublnw", [128, 128])
    else:
        FM = P.din("FM", [TOK, D])
    wout = P.din("wout", [D, D])
    cT = P.din("cT", [128, 1, 8]); adaw = P.din("adaw", [D, 1024]); adab = P.din("adab", [128, 1024])
    X1 = P.dout("X1", [TOK, D])
    consts(P)
    csil = P.sb("csil", [128, 1, 8]); S.dma('sp', csil[:], cT, writes=['csil'])
    S.op('act', lambda e: e.activation(out=csil[:], in_=csil[:], func=AF.Silu), reads=['csil'], writes=['csil'])
    g1b = P.sb("g1b", [128, D])
    emit_mod(P, csil, 0, adaw, adab, 0, 1024, g1b, 'g1b', P.ones, "m")
    wt = P.sb("wt", [128, 8, D])
    wv = wout.rearrange("(k p) n -> p k n", p=128)
    for k in range(8):
        S.dma('sp', wt[:, k, :], wv[:, k, :], writes=['wt%d' % k])
    if kind == 'ab':
        gw = P.sb("gw", [128, 64]); S.dma('sp', gw[:], gdnw, writes=['gw'])
        sw = P.sb("sw", [128, 128]); S.dma('sp', sw[:], sublnw, writes=['sw'])
        o0 = P.sb("o0", [128, 512]); o1 = P.sb("o1", [128, 512]); gt = P.sb("gt", [128, 512]); dl = P.sb("dl", [128, 512])
        sq = P.sb("sq", [128, 512]); st = P.sb("st", [128, 3, 12])
    xt = P.sb("xt", [128, D]); mix = P.sb("mix", [128, D]); mixT = P.sb("mixT", [128, D]); x1 = P.sb("x1", [128, D])
    pT = P.ps("pT", [128, D]); pY = P.ps("pY", [128, D])
    for t in range(NTT):
        rs = slice(t * 128, (t + 1) * 128)
        S.dma('sp', xt[:], X[rs, :], writes=['xt'])
        if kind == 'ab':
            S.dma('sp', o0[:], O0[rs, :], writes=['o0']); S.dma('sp', o1[:], O1[rs, :], writes=['o1'])
            S.dma('sp', gt[:], GATE[rs, :], writes=['gt']); S.dma('sp', dl[:], DL[rs, :], writes=['dl'])
            S.op('dve', lambda e: e.tensor_tensor(out=o0[:], in0=o0[:], in1=o1[:], op=ALU.add), reads=['o0', 'o1'], writes=['o0'])
            S.op('act', lambda e: e.activation(out=sq[:], in_=o0[:], func=AF.Square), reads=['o0'], writes=['sq'])
            S.op('dve', lambda e: e.tensor_reduce(out=st[:, 0, 0:8], in_=sq[:].rearrange("p (g d) -> p g d", d=64), axis=AX.X, op=ALU.add), reads=['sq'], writes=['st0a'])
            S.op('act', lambda e: e.activation(out=sq[:], in_=dl[:], func=AF.Square), reads=['dl', 'st0a'], writes=['sq'])
            S.op('dve', lambda e: e.tensor_reduce(out=st[:, 0, 8:12], in_=sq[:].rearrange("p (g d) -> p g d", d=128), axis=AX.X, op=ALU.add), reads=['sq'], writes=['st0b'])
            S.op('act', lambda e: e.activation(out=st[:, 1, 0:8], in_=st[:, 0, 0:8], func=AF.Sqrt, bias=P.epsb[:, 0:1], scale=1.0 / 64), reads=['st0a', 'epsb'], writes=['st1a'])
            S.op('act', lambda e: e.activation(out=st[:, 1, 8:12], in_=st[:, 0, 8:12], func=AF.Sqrt, bias=P.epsb[:, 0:1], scale=1.0 / 128), reads=['st0b', 'epsb'], writes=['st1b'])
            S.op('dve', lambda e: e.reciprocal(out=st[:, 2, :], in_=st[:, 1, :]), reads=['st1a', 'st1b'], writes=['st2'])
            S.op('dve', lambda e: e.tensor_scalar(out=st[:, 2, 8:12], in0=st[:, 2, 8:12], scalar1=0.8, scalar2=None, op0=ALU.mult), reads=['st2'], writes=['st2'])
            v8 = lambda a: a.rearrange("p (g d) -> p g d", d=64)
            v4 = lambda a: a.rearrange("p (g d) -> p g d", d=128)
            S.op('dve', lambda e: e.tensor_tensor(out=v8(o0[:]), in0=v8(o0[:]), in1=bc(st[:, 2, 0:8], 2, 64), op=ALU.mult), reads=['o0', 'st2'], writes=['o0'])
            S.op('pool', lambda e: e.tensor_tensor(out=v8(o0[:]), in0=v8(o0[:]), in1=bc(gw[:], 1, 8), op=ALU.mult), reads=['o0', 'gw'], writes=['o0'])
            S.op('act', lambda e: e.activation(out=gt[:], in_=gt[:], func=AF.Silu), reads=['gt'], writes=['gt'])
            S.op('dve', lambda e: e.tensor_tensor(out=mix[:, 0:512], in0=o0[:], in1=gt[:], op=ALU.mult), reads=['o0', 'gt'], writes=['mix0'])
            S.op('dve', lambda e: e.tensor_tensor(out=v4(dl[:]), in0=v4(dl[:]), in1=bc(st[:, 2, 8:12], 2, 128), op=ALU.mult), reads=['dl', 'st2'], writes=['dl'])
            S.op('pool', lambda e: e.tensor_tensor(out=v4(mix[:, 512:1024]), in0=v4(dl[:]), in1=bc(sw[:], 1, 4), op=ALU.mult), reads=['dl', 'sw'], writes=['mix1'])
        else:
            S.dma('sp', mix[:], FM[rs, :], writes=['mix0', 'mix1'])
        for k in range(8):
            S.op('pe', lambda e: e.transpose(pT[:, k * 128:(k + 1) * 128], mix[:, k * 128:(k + 1) * 128], P.idf[:]),
                 reads=['mix%d' % (k // 4), 'ident'], writes=['pT%d' % (k // 4)])
        S.op('act', lambda e: e.activation(out=mixT[:, 0:512], in_=pT[:, 0:512], func=AF.Copy), reads=['pT0'], writes=['mixT0'])
        S.op('dve', lambda e: e.tensor_copy(out=mixT[:, 512:1024], in_=pT[:, 512:1024]), reads=['pT1'], writes=['mixT1'])
        for cb in range(2):
            for k in range(8):
                S.op('pe', lambda e: e.matmul(pY[:, cb * 512:(cb + 1) * 512], lhsT=mixT[:, k * 128:(k + 1) * 128], rhs=wt[:, k, cb * 512:(cb + 1) * 512],
                                              start=(k == 0), stop=(k == 7)), reads=['mixT%d' % (k // 4), 'wt%d' % k], writes=['pY%d' % cb])
        S.op('dve', lambda e: e.tensor_tensor(out=x1[:], in0=pY[:], in1=g1b[:], op=ALU.mult), reads=['pY0', 'pY1', 'g1b'], writes=['x1'])
        S.op('pool', lambda e: e.tensor_tensor(out=x1[:], in0=x1[:], in1=xt[:], op=ALU.add), reads=['x1', 'xt'], writes=['x1'])
        S.dma('pool', X1[rs, :], x1[:], reads=['x1'], writes=['X1'])
    return P


def run_l5a(kind, x, c, ada_w_i, ada_b_i, wout, **kw):
    P = build_l5a(kind)
    in_maps = []
    for core in range(NCORES):
        b, hf = core // 2, core % 2
        sl = slice(hf * TOK, (hf + 1) * TOK)
        m = dict(X=np.ascontiguousarray(x[b, sl]), wout=np.ascontiguousarray(wout), cT=cT_layout(c[b][None]),
                 adaw=np.ascontiguousarray(ada_w_i[:, 2048:3072]), adab=rep128(ada_b_i[2048:3072]))
        if kind == 'ab':
            m.update(O0=np.ascontiguousarray(kw['O'][b, 0, sl]), O1=np.ascontiguousarray(kw['O'][b, 1, sl]),
                     GATE=np.ascontiguousarray(kw['Plat'][b, sl, 1536:2048]), DL=np.ascontiguousarray(kw['dlat'][b, sl]),
                     gdnw=rep128(kw['gdn_norm_w']), sublnw=rep128(kw['subln_w']))
        else:
            m.update(FM=np.ascontiguousarray(kw['fm'][b, sl]))
        in_maps.append(m)
    res = P.run(in_maps)
    out = np.empty((4, 8192, D), np.float32)
    for core in range(NCORES):
        b, hf = core // 2, core % 2
        out[b, hf * TOK:(hf + 1) * TOK] = res[core]["X1"]
    return out


def build_l5b(tail, ntt=NTT):
    P = Prog()
    S = P.S
    tok = ntt * 128
    X1 = P.din("X1", [tok, D])
    cT = P.din("cT", [128, 1, 8]); adaw = P.din("adaw", [D, 3072]); adab = P.din("adab", [128, 3072]); n2w = P.din("n2w", [128, D])
    wq = P.din("wq", [D, 2048]); keysT = P.din("keysT", [128, 16, 128])
    Utab = P.din("Utab", [16384, D]); Vtab = P.din("Vtab", [16384, D])
    if tail == 'hnext':
        adawn = P.din("adawn", [D, 2048]); adabn = P.din("adabn", [128, 2048]); n1wn = P.din("n1wn", [128, D])
        Hn = P.dout("Hn", [tok, D])
    else:
        fnw = P.din("fnw", [128, D])
    Xo = P.dout("Xo", [tok, D])
    consts(P)
    csil = P.sb("csil", [128, 1, 8]); S.dma('sp', csil[:], cT, writes=['csil'])
    S.op('act', lambda e: e.activation(out=csil[:], in_=csil[:], func=AF.Silu), reads=['csil'], writes=['csil'])
    modb = P.sb("modb", [128, 3072])
    P.mod_bw = 3072
    pA = P.ps("pA", [128, D]); pQ = P.ps("pQ", [128, 2, 512]); pSc = P.ps("pSc", [128, 2048])
    pmx = ([pQ[:, 0, :], pQ[:, 1, :]], ['pQ0', 'pQ1'])
    emit_mod(P, csil, 0, adaw, adab, 0, 3072, modb, 'modb', P.ones, "m", pmx=pmx)
    nw = P.sb("nw", [128, D]); S.dma('sp', nw[:], n2w, writes=['nw'])
    wmod2 = P.sb("wmod2", [128, D])
    S.op('dve', lambda e: e.scalar_tensor_tensor(out=wmod2[:], in0=modb[:, 1024:2048], scalar=1.0, in1=nw[:], op0=ALU.add, op1=ALU.mult),
         reads=['modb', 'nw'], writes=['wmod2'])
    if tail == 'hnext':
        modn = P.sb("modn", [128, 2048])
        emit_mod(P, csil, 0, adawn, adabn, 0, 2048, modn, 'modn', P.ones, "m", pmx=pmx)
        S.dma('sp', nw[:], n1wn, reads=['wmod2'], writes=['nw'])
        wmodn = P.sb("wmodn", [128, D])
        S.op('dve', lambda e: e.scalar_tensor_tensor(out=wmodn[:], in0=modn[:, 1024:2048], scalar=1.0, in1=nw[:], op0=ALU.add, op1=ALU.mult),
             reads=['modn', 'nw'], writes=['wmodn'])
    else:
        S.dma('sp', nw[:], fnw, reads=['wmod2'], writes=['nw'])
    wqt = P.sb("wqt", [128, 8, 2048])
    wqv = wq.rearrange("(k p) n -> p k n", p=128)
    for k in range(8):
        S.dma('sp', wqt[:, k, :], wqv[:, k, :], writes=['wq%d' % k])
    kyt = P.sb("kyt", [128, 16, 128]); S.dma('sp', kyt[:], keysT, writes=['kyt'])
    zr = P.sb("zr", [128, 255]); S.op('pool', lambda g: g.memset(zr[:], 0.0), writes=['zr']); S.op('pool', lambda g: g.memset(zr[:, 127:128], 1.0), reads=['zr'], writes=['zr'])
    iot = P.sb("iot", [128, 16]); S.op('pool', lambda g: g.iota(iot[:], pattern=[[1, 16]], base=0, channel_multiplier=0, allow_small_or_imprecise_dtypes=True), writes=['iot'])
    xt = P.sb("xt", [128, D]); hn = P.sb("hn", [128, D]); hnT = P.sb("hnT", [128, D]); ss = P.sb("ss", [128, 4])
    qTs = P.sb("qTs", [128, 16, 128]); sc = P.sb("sc", [128, 2048]); wk = P.sb("wk", [128, 2048])
    m16 = P.sb("m16", [128, 16, 16]); i16 = P.sb("i16", [128, 16, 16], U32); i16f = P.sb("i16f", [128, 16, 16])
    c16 = P.sb("c16", [128, 8, 16]); p16 = P.sb("p16", [128, 8, 16], U32); pa = P.sb("pa", [128, 8, 16], U32); pb = P.sb("pb", [128, 8, 16], U32)
    paf = P.sb("paf", [128, 8, 16]); pbf = P.sb("pbf", [128, 8, 16]); sel = P.sb("sel", [128, 2, 128]); idxf = P.sb("idxf", [128, 128])
    gat = P.sb("gat", [128, 128]); gs = P.sb("gs", [128, 2, 8])
    idxTi = P.sb("idxTi", [128, 128], I32); gateT = P.sb("gateT", [128, 128]); actA = P.sb("actA", [128, 128]); Wm = P.sb("Wm", [128, 128])
    NG = 4
    Ug = [P.sb("Ug%d" % i, [128, D]) for i in range(NG)]
    Wz = [P.sb("Wz%d" % i, [128, 128]) for i in range(3)]
    pB = [pSc[:, 0:1024], pA[:, :]]
    pBk = [['pSc0', 'pSc1'], ['pA0', 'pA1']]
    pOut = pSc[:, 1024:2048]; pOk = ['pSc2', 'pSc3']
    cand = wk; junk = sc
    v4 = lambda a: a.rearrange("p (h a b) -> p h a b", a=16, b=16)
    for t in range(ntt):
        rs = slice(t * 128, (t + 1) * 128)
        S.dma('sp', xt[:], X1[rs, :], writes=['xt'])
        emit_rmsnorm_mod(P, xt, 'xt', wmod2, modb[:, 0:1024], ['wmod2', 'modb'], hn, 'hn', (hn, ss))
        for k in range(8):
            S.op('pe', lambda e: e.transpose(pA[:, k * 128:(k + 1) * 128], hn[:, k * 128:(k + 1) * 128], P.idf[:]), reads=['hn', 'ident'], writes=['pA%d' % (k // 4)])
        S.op('act', lambda e: e.activation(out=hnT[:, 0:512], in_=pA[:, 0:512], func=AF.Copy), reads=['pA0'], writes=['hnT0'])
        S.op('dve', lambda e: e.tensor_copy(out=hnT[:, 512:1024], in_=pA[:, 512:1024]), reads=['pA1'], writes=['hnT1'])
        for g in range(4):
            for j in range(4):
                hp = g * 4 + j
                for k in range(8):
                    S.op('pe', lambda e: e.matmul(pQ[:, g % 2, j * 128:(j + 1) * 128], lhsT=wqt[:, k, hp * 128:(hp + 1) * 128], rhs=hnT[:, k * 128:(k + 1) * 128],
                                                  start=(k == 0), stop=(k == 7)), reads=['wq%d' % k, 'hnT%d' % (k // 4)], writes=['pQ%d' % (g % 2)])
            eng = 'act' if g % 2 == 0 else 'dve'
            if eng == 'act':
                S.op('act', lambda e: e.activation(out=qTs[:, g * 4:(g + 1) * 4, :], in_=pQ[:, g % 2, :].rearrange("p (j n) -> p j n", n=128), func=AF.Copy), reads=['pQ%d' % (g % 2)], writes=['qTs%d' % g])
            else:
                S.op('dve', lambda e: e.tensor_copy(out=qTs[:, g * 4:(g + 1) * 4, :], in_=pQ[:, g % 2, :].rearrange("p (j n) -> p j n", n=128)), reads=['pQ%d' % (g % 2)], writes=['qTs%d' % g])
        for hp in range(16):
            S.op('pe', lambda e: e.matmul(pSc[:, hp * 128:(hp + 1) * 128], lhsT=qTs[:, hp, :], rhs=kyt[:, hp, :], start=True, stop=True),
                 reads=['qTs%d' % (hp // 4), 'kyt'], writes=['pSc%d' % (hp // 4)])
        for g in range(4):
            if g % 2 == 0:
                S.op('act', lambda e: e.activation(out=sc[:, g * 512:(g + 1) * 512], in_=pSc[:, g * 512:(g + 1) * 512], func=AF.Copy), reads=['pSc%d' % g], writes=['sc%d' % g])
            else:
                S.op('dve', lambda e: e.tensor_copy(out=sc[:, g * 512:(g + 1) * 512], in_=pSc[:, g * 512:(g + 1) * 512]), reads=['pSc%d' % g], writes=['sc%d' % g])
        for hp in range(16):
            blk = slice(hp * 128, (hp + 1) * 128)
            sk = 'sc%d' % (hp // 4)
            S.op('dve', lambda e: e.max(out=m16[:, hp, 0:8], in_=sc[:, blk]), reads=[sk], writes=['m16a'])
            S.op('dve', lambda e: e.match_replace(out=wk[:, blk], in_to_replace=m16[:, hp, 0:8], in_values=sc[:, blk], imm_value=-1e30), reads=[sk, 'm16a'], writes=['wk'])
            S.op('dve', lambda e: e.max(out=m16[:, hp, 8:16], in_=wk[:, blk]), reads=['wk'], writes=['m16b'])
            S.op('dve', lambda e: e.max_index(out=i16[:, hp, 0:8], in_max=m16[:, hp, 0:8], in_values=sc[:, blk]), reads=[sk, 'm16a'], writes=['i16'])
            S.op('dve', lambda e: e.max_index(out=i16[:, hp, 8:16], in_max=m16[:, hp, 8:16], in_values=sc[:, blk]), reads=[sk, 'm16b'], writes=['i16'])
        S.op('dve', lambda e: e.tensor_copy(out=i16f[:], in_=i16[:]), reads=['i16'], writes=['i16f'])
        m16v = m16[:].rearrange("p (h two) k -> p h two k", two=2)
        i16v = i16f[:].rearrange("p (h two) k -> p h two k", two=2)
        S.op('dve', lambda e: e.tensor_tensor(out=v4(cand[:]), in0=bc(m16v[:, :, 0, :], 3, 16), in1=bc(m16v[:, :, 1, :], 2, 16), op=ALU.add),
             reads=['m16a', 'm16b', 'wk'], writes=['cand'])
        for h in range(8):
            blk = slice(h * 256, (h + 1) * 256)
            S.op('dve', lambda e: e.max(out=c16[:, h, 0:8], in_=cand[:, blk]), reads=['cand'], writes=['c16a'])
            S.op('dve', lambda e: e.match_replace(out=junk[:, blk], in_to_replace=c16[:, h, 0:8], in_values=cand[:, blk], imm_value=-1e30), reads=['cand', 'c16a'] + ['sc%d' % i for i in range(4)], writes=['junk'])
            S.op('dve', lambda e: e.max(out=c16[:, h, 8:16], in_=junk[:, blk]), reads=['junk'], writes=['c16b'])
            S.op('dve', lambda e: e.max_index(out=p16[:, h, 0:8], in_max=c16[:, h, 0:8], in_values=cand[:, blk]), reads=['cand', 'c16a'], writes=['p16'])
            S.op('dve', lambda e: e.max_index(out=p16[:, h, 8:16], in_max=c16[:, h, 8:16], in_values=cand[:, blk]), reads=['cand', 'c16b'], writes=['p16'])
        S.op('dve', lambda e: e.tensor_single_scalar(out=pa[:], in_=p16[:], scalar=4, op=ALU.logical_shift_right), reads=['p16'], writes=['pa'])
        S.op('dve', lambda e: e.tensor_single_scalar(out=pb[:], in_=p16[:], scalar=15, op=ALU.bitwise_and), reads=['p16'], writes=['pb'])
        S.op('dve', lambda e: e.tensor_copy(out=paf[:], in_=pa[:]), reads=['pa'], writes=['paf'])
        S.op('dve', lambda e: e.tensor_copy(out=pbf[:], in_=pb[:]), reads=['pb'], writes=['pbf'])
        iob = bc(bc(iot[:], 1, 16), 1, 8)
        for w_, (pf, pk) in enumerate(((paf, 'paf'), (pbf, 'pbf'))):
            S.op('dve', lambda e: e.tensor_tensor(out=v4(junk[:]), in0=bc(pf[:], 3, 16), in1=iob, op=ALU.is_equal), reads=[pk, 'iot', 'junk'], writes=['junk'])
            S.op('dve', lambda e: e.tensor_tensor(out=v4(junk[:]), in0=v4(junk[:]), in1=bc(i16v[:, :, w_, :], 2, 16), op=ALU.mult), reads=['junk', 'i16f'], writes=['junk'])
            S.op('dve', lambda e: e.tensor_reduce(out=sel[:, w_, :], in_=junk[:].rearrange("p (x a) -> p x a", a=16), axis=AX.X, op=ALU.add), reads=['junk'], writes=['sel%d' % w_])
        S.op('dve', lambda e: e.scalar_tensor_tensor(out=idxf[:], in0=sel[:, 0, :], scalar=128.0, in1=sel[:, 1, :], op0=ALU.mult, op1=ALU.add), reads=['sel0', 'sel1'], writes=['idxf'])
        c16f = c16[:]
        S.op('dve', lambda e: e.tensor_tensor(out=gat[:].rearrange("p (h k) -> p h k", k=16), in0=c16f, in1=bc(c16[:, :, 0], 2, 16), op=ALU.subtract), reads=['c16a', 'c16b'], writes=['gat'])
        S.op('dve', lambda e: e.tensor_scalar(out=gat[:], in0=gat[:], scalar1=-80.0, scalar2=None, op0=ALU.max), reads=['gat'], writes=['gat'])
        S.op('act', lambda e: e.activation(out=gat[:], in_=gat[:], func=AF.Exp), reads=['gat'], writes=['gat'])
        S.op('dve', lambda e: e.tensor_reduce(out=gs[:, 0, :], in_=gat[:].rearrange("p (h k) -> p h k", k=16), axis=AX.X, op=ALU.add), reads=['gat'], writes=['gs0'])
        S.op('dve', lambda e: e.reciprocal(out=gs[:, 1, :], in_=gs[:, 0, :]), reads=['gs0'], writes=['gs1'])
        S.op('dve', lambda e: e.tensor_tensor(out=gat[:].rearrange("p (h k) -> p h k", k=16), in0=gat[:].rearrange("p (h k) -> p h k", k=16), in1=bc(gs[:, 1, :], 2, 16), op=ALU.mult), reads=['gat', 'gs1'], writes=['gat'])
        S.op('pe', lambda e: e.transpose(pQ[:, 0, 0:128], idxf[:], P.idf[:]), reads=['idxf', 'ident'], writes=['pQ0'])
        S.op('pe', lambda e: e.transpose(pQ[:, 1, 0:128], gat[:], P.idf[:]), reads=['gat', 'ident'], writes=['pQ1'])
        S.op('dve', lambda e: e.tensor_copy(out=idxTi[:], in_=pQ[:, 0, 0:128]), reads=['pQ0'], writes=['idxTi'])
        S.op('act', lambda e: e.activation(out=gateT[:], in_=pQ[:, 1, 0:128], func=AF.Copy), reads=['pQ1'], writes=['gateT'])
        for tk in range(128):
            ug = Ug[tk % NG]; uk = 'Ug%d' % (tk % NG)
            S.dmaf('pool', lambda g: g.indirect_dma_start(out=ug[:], out_offset=None, in_=Utab[:, :], in_offset=bass.IndirectOffsetOnAxis(ap=idxTi[:, tk:tk + 1], axis=0)),
                   reads=['idxTi'], writes=[uk])
            pb_ = pB[tk % 2]; pbk = pBk[tk % 2]
            for cb in range(2):
                S.op('pe', lambda e: e.matmul(pb_[:, cb * 512:(cb + 1) * 512], lhsT=bc(P.idf[:, tk], 1, 128), rhs=hn[:, cb * 512:(cb + 1) * 512], start=True, stop=True),
                     reads=['ident', 'hn'], writes=[pbk[cb]])
            S.op('dve', lambda e: e.scalar_tensor_tensor(out=ug[:], in0=ug[:], scalar=1.0, in1=pb_, op0=ALU.mult, op1=ALU.mult, accum_out=actA[:, tk:tk + 1]),
                 reads=[uk] + pbk, writes=[uk, 'actA'])
        S.op('act', lambda e: e.activation(out=Wm[:], in_=actA[:], func=AF.Gelu), reads=['actA'], writes=['Wm'])
        S.op('dve', lambda e: e.tensor_tensor(out=Wm[:], in0=Wm[:], in1=gateT[:], op=ALU.mult), reads=['Wm', 'gateT'], writes=['Wm'])
        for tk in range(128):
            vg = Ug[tk % NG]; vk = 'Ug%d' % (tk % NG)
            S.dmaf('pool', lambda g: g.indirect_dma_start(out=vg[:], out_offset=None, in_=Vtab[:, :], in_offset=bass.IndirectOffsetOnAxis(ap=idxTi[:, tk:tk + 1], axis=0)),
                   reads=['idxTi'], writes=[vk])
            wz = Wz[tk % 3]; wzk = 'Wz%d' % (tk % 3)
            S.op('act', lambda e: e.activation(out=wz[:], in_=zr[:, 127 - tk:255 - tk], func=AF.Copy, scale=Wm[:, tk:tk + 1]), reads=['zr', 'Wm'], writes=[wzk])
            for cb in range(2):
                S.op('pe', lambda e: e.matmul(pOut[:, cb * 512:(cb + 1) * 512], lhsT=wz[:], rhs=vg[:, cb * 512:(cb + 1) * 512], start=(tk == 0), stop=(tk == 127)),
                     reads=[wzk, vk], writes=[pOk[cb]])
        S.op('dve', lambda e: e.tensor_tensor(out=hn[:], in0=pOut, in1=modb[:, 2048:3072], op=ALU.mult), reads=pOk + ['modb', 'hn'], writes=['hn'])
        S.op('pool', lambda e: e.tensor_tensor(out=xt[:], in0=xt[:], in1=hn[:], op=ALU.add), reads=['xt', 'hn'], writes=['xt'])
        if tail == 'hnext':
            S.dma('sp', Xo[rs, :], xt[:], reads=['xt'], writes=['Xo'])
            emit_rmsnorm_mod(P, xt, 'xt', wmodn, modn[:, 0:1024], ['wmodn', 'modn'], hn, 'hn', (hn, ss))
            S.dma('sp', Hn[rs, :], hn[:], reads=['hn'], writes=['Hn'])
        else:
            emit_rmsnorm_mod(P, xt, 'xt', nw, None, ['nw'], hn, 'hn', (hn, ss))
            S.dma('sp', Xo[rs, :], hn[:], reads=['hn'], writes=['Xo'])
    return P


def run_l5b(tail, x1, c, ada_w_i, ada_b_i, norm2_w_i, wq, keys, utab, vtab, ntt=NTT, ncores=NCORES, **kw):
    P = build_l5b(tail, ntt)
    tok = ntt * 128
    keysT = np.ascontiguousarray(keys.reshape(16, 128, 128).transpose(2, 0, 1))
    in_maps = []
    for core in range(ncores):
        b, hf = core // 2, core % 2
        sl = slice(hf * TOK, hf * TOK + tok)
        m = dict(X1=np.ascontiguousarray(x1[b, sl]), cT=cT_layout(c[b][None]), adaw=np.ascontiguousarray(ada_w_i[:, 3072:6144]), adab=rep128(ada_b_i[3072:6144]),
                 n2w=rep128(norm2_w_i), wq=np.ascontiguousarray(wq), keysT=keysT, Utab=np.ascontiguousarray(utab), Vtab=np.ascontiguousarray(vtab))
        if tail == 'hnext':
            m.update(adawn=np.ascontiguousarray(kw['ada_w_n'][:, :2048]), adabn=rep128(kw['ada_b_n'][:2048]), n1wn=rep128(kw['norm1_w_n']))
        else:
            m.update(fnw=rep128(kw['final_norm_w']))
        in_maps.append(m)
    res = P.run(in_maps)
    xo = np.zeros((4, 8192, D), np.float32)
    hn = np.zeros((4, 8192, D), np.float32) if tail == 'hnext' else None
    for core in range(ncores):
        b, hf = core // 2, core % 2
        xo[b, hf * TOK:hf * TOK + tok] = res[core]["Xo"]
        if hn is not None:
            hn[b, hf * TOK:hf * TOK + tok] = res[core]["Hn"]
    return xo, hn


def build_l6():
    P = Prog()
    S = P.S
    nc = P.nc
    HT = P.din("HT", [4, 128, 8192])
    CS = P.din("CS", [2, 128, 512])
    W64 = P.din("W64", [128, 128])
    WB = P.din("WB", [128, 64, 2, 128])
    FM = P.dout("FM", [8192, 512])
    Zs = [nc.dram_tensor("Zs%d" % g, [8192, 512], F32, kind="Internal").ap() for g in range(2)]
    Us = [nc.dram_tensor("Us%d" % g, [128, 128, 256], F32, kind="Internal").ap() for g in range(2)]
    cs = P.sb("cs", [128, 2, 512]); S.dma('sp', cs[:], CS.rearrange("h p n -> p h n"), writes=['cs'])
    w64 = P.sb("w64", [128, 128]); S.dma('sp', w64[:], W64, writes=['w64'])
    pz = [P.ps("pz%d" % i, [128, 512]) for i in range(2)]
    pu = P.ps("pu", [128, 2048])
    py = [P.ps("py%d" % i, [128, 256]) for i in range(2)]
    TB = 2048
    hts = [P.sb("ht%d" % i, [128, 4, TB]) for i in range(2)]
    zts = [P.sb("zt%d" % i, [128, 512]) for i in range(2)]
    it = 0
    for tb in range(8192 // TB):
        ht = hts[tb % 2]; hk = 'ht%d' % (tb % 2)
        S.dma('sp', ht[:], HT[:, :, tb * TB:(tb + 1) * TB].rearrange("c p t -> p c t"), writes=[hk])
        for tt in range(TB // 128):
            for g in range(2):
                p_ = pz[it % 2]; pk = 'pz%d' % (it % 2); zt = zts[it % 2]; zk = 'zt%d' % (it % 2)
                it += 1
                for hf in range(2):
                    S.op('pe', lambda e: e.matmul(p_[:], lhsT=ht[:, 2 * g + hf, tt * 128:(tt + 1) * 128], rhs=cs[:, hf, :], start=(hf == 0), stop=(hf == 1)),
                         reads=[hk, 'cs'], writes=[pk])
                S.op('act' if g == 0 else 'dve', (lambda e: e.activation(out=zt[:], in_=p_[:], func=AF.Copy)) if g == 0 else (lambda e: e.tensor_copy(out=zt[:], in_=p_[:])),
                     reads=[pk], writes=[zk])
                r0 = tb * TB + tt * 128
                S.dma('pool', Zs[g][r0:r0 + 128, :], zt[:], reads=[zk], writes=['Zs%d' % g])
    LB = 8
    zin = [P.sb("zin%d" % i, [128, LB, 256]) for i in range(2)]
    uts = [P.sb("ut%d" % i, [128, LB, 256]) for i in range(2)]
    it = 0
    for g in range(2):
        zv = Zs[g].rearrange("(l1 l2) (ri kc) -> ri l1 l2 kc", l2=128, ri=2)
        uv = Us[g].rearrange("l2 m kc -> m l2 kc")
        for lb in range(128 // LB):
            zi = zin[it % 2]; zk = 'zin%d' % (it % 2); ut = uts[it % 2]; uk = 'ut%d' % (it % 2)
            it += 1
            for ri in range(2):
                S.dma('sp', zi[ri * 64:(ri + 1) * 64, :, :], zv[ri][:, lb * LB:(lb + 1) * LB, :], reads=['Zs%d' % g], writes=[zk + '_%d' % ri])
            for j in range(LB // 2):
                S.op('pe', lambda e: e.matmul(pu[:, j * 512:(j + 1) * 512], lhsT=w64[:], rhs=zi[:, 2 * j:2 * j + 2, :].rearrange("p a b -> p (a b)"), start=True, stop=True),
                     reads=['w64', zk + '_0', zk + '_1'], writes=['pu'])
            S.op('act' if lb % 2 == 0 else 'dve', (lambda e: e.activation(out=ut[:].rearrange("p a b -> p (a b)"), in_=pu[:], func=AF.Copy)) if lb % 2 == 0 else
                 (lambda e: e.tensor_copy(out=ut[:].rearrange("p a b -> p (a b)"), in_=pu[:])), reads=['pu'], writes=[uk])
            S.dma('pool', uv[:, lb * LB:(lb + 1) * LB, :], ut[:], reads=[uk], writes=['Us%d' % g])
    KB = 16
    wbs = [P.sb("wb%d" % i, [128, KB, 2, 128]) for i in range(2)]
    urs = [P.sb("ur%d" % i, [128, 2, KB, 256]) for i in range(2)]
    yts = [P.sb("yt%d" % i, [128, 256]) for i in range(2)]
    fv = FM.rearrange("(k2 k1) c -> k1 k2 c", k1=64)
    it = 0; ib = 0
    for g in range(2):
        for kb in range(64 // KB):
            wb = wbs[ib % 2]; wk_ = 'wb%d' % (ib % 2); ur = urs[ib % 2]; urk = 'ur%d' % (ib % 2)
            ib += 1
            S.dma('sp', wb[:], WB[:, kb * KB:(kb + 1) * KB, :, :], writes=[wk_])
            for ri in range(2):
                S.dma('sp', ur[:, ri, :, :], Us[g][:, ri * 64 + kb * KB:ri * 64 + (kb + 1) * KB, :], reads=['Us%d' % g], writes=[urk + '_%d' % ri])
            for kk in range(KB):
                k1 = kb * KB + kk
                p_ = py[it % 2]; pk = 'py%d' % (it % 2); yt = yts[it % 2]; yk = 'yt%d' % (it % 2)
                it += 1
                for ri in range(2):
                    S.op('pe', lambda e: e.matmul(p_[:], lhsT=wb[:, kk, ri, :], rhs=ur[:, ri, kk, :], start=(ri == 0), stop=(ri == 1)),
                         reads=[wk_, urk + '_0', urk + '_1'], writes=[pk])
                S.op('act' if it % 2 == 0 else 'dve', (lambda e: e.activation(out=yt[:], in_=p_[:], func=AF.Copy)) if it % 2 == 0 else (lambda e: e.tensor_copy(out=yt[:], in_=p_[:])),
                     reads=[pk], writes=[yk])
                S.dma('pool', fv[k1][:, g * 256:(g + 1) * 256], yt[:], reads=[yk], writes=['FM'])
    return P


def l6_consts():
    sc = 1.0 / math.sqrt(8192.0 * 256.0)
    ch = np.arange(256, dtype=np.float64)
    th = 2 * np.pi * np.outer(ch, ch) / 256.0
    CS = np.concatenate([np.cos(th), np.sin(th)], axis=1) * sc
    CS = CS.reshape(2, 128, 512).astype(np.float32)
    l1 = np.arange(64, dtype=np.float64)
    t64 = 2 * np.pi * np.outer(l1, l1) / 64.0
    c, s = np.cos(t64), np.sin(t64)
    W64 = np.block([[c, -s], [-s, -c]]).astype(np.float32)
    l2 = np.arange(128, dtype=np.float64)[:, None, None]
    k1 = np.arange(64, dtype=np.float64)[None, :, None]
    k2 = np.arange(128, dtype=np.float64)[None, None, :]
    thb = 2 * np.pi * (l2 * k2 / 128.0 + l2 * k1 / 8192.0)
    WB = np.stack([np.cos(thb), np.sin(thb)], axis=2).astype(np.float32)
    return CS, W64, np.ascontiguousarray(WB)


def run_l6(h1):
    P = build_l6()
    CS, W64, WB = l6_consts()
    in_maps = []
    for core in range(NCORES):
        b, gp = core // 2, core % 2
        ht = h1[b, :, gp * 512:(gp + 1) * 512].T.reshape(4, 128, 8192)
        in_maps.append(dict(HT=np.ascontiguousarray(ht), CS=CS, W64=W64, WB=WB))
    res = P.run(in_maps)
    out = np.empty((4, 8192, D), np.float32)
    for core in range(NCORES):
        b, gp = core // 2, core % 2
        out[b, :, gp * 512:(gp + 1) * 512] = res[core]["FM"]
    return out


def kernel(x, c, ctx, c_ctx, ada_w, ada_b, norm1_w, norm2_w, w_in, conv_w, a_log, dt_bias, gdn_norm_w,
           lam_q1, lam_k1, lam_q2, lam_k2, subln_w, w_out_ab, w_out_f, peer_wq, peer_keys, peer_u, peer_v, final_norm_w):
    f = lambda a: np.asarray(a, dtype=np.float32)
    x, c, ctx, c_ctx, ada_w, ada_b, norm1_w, norm2_w, w_in, conv_w, a_log, dt_bias, gdn_norm_w = map(
        f, (x, c, ctx, c_ctx, ada_w, ada_b, norm1_w, norm2_w, w_in, conv_w, a_log, dt_bias, gdn_norm_w))
    lam_q1, lam_k1, lam_q2, lam_k2, subln_w, w_out_ab, w_out_f, peer_wq, peer_keys, peer_u, peer_v, final_norm_w = map(
        f, (lam_q1, lam_k1, lam_q2, lam_k2, subln_w, w_out_ab, w_out_f, peer_wq, peer_keys, peer_u, peer_v, final_norm_w))
    Plat, Pctx = run_l1(x, c, ctx, c_ctx, ada_w[0], ada_b[0], norm1_w[0], w_in[0])
    o2 = run_l2(Plat, Pctx, conv_w[0], a_log[0], dt_bias[0])
    O = run_l3(o2['QKV'], o2['BG'])
    dlat = run_l4(o2['QKr'], Plat, Pctx, lam_q1[0], lam_k1[0], lam_q2[0], lam_k2[0])
    del o2
    x1 = run_l5a('ab', x, c, ada_w[0], ada_b[0], w_out_ab[0], O=O, Plat=Plat, dlat=dlat, gdn_norm_w=gdn_norm_w[0], subln_w=subln_w[0])
    del O, dlat, Plat, Pctx
    x2, h1 = run_l5b('hnext', x1, c, ada_w[0], ada_b[0], norm2_w[0], peer_wq[0], peer_keys[0], peer_u[0], peer_v[0],
                     ada_w_n=ada_w[1], ada_b_n=ada_b[1], norm1_w_n=norm1_w[1])
    del x1
    fm = run_l6(h1)
    x3 = run_l5a('f', x2, c, ada_w[1], ada_b[1], w_out_f[0], fm=fm)
    del x2, fm, h1
    zw = np.zeros((D, 2048), np.float32)
    zb = np.zeros((2048,), np.float32)
    _, out = run_l5b('hnext', x3, c, ada_w[1], ada_b[1], norm2_w[1], peer_wq[1], peer_keys[1], peer_u[1], peer_v[1],
                     ada_w_n=zw, ada_b_n=zb, norm1_w_n=final_norm_w)
    return out.astype(np.float32)
```

```python
import math
import numpy as np
import concourse.bass as bass
import concourse.mybir as mybir
from concourse.bass_utils import run_bass_kernel_spmd

F32 = mybir.dt.float32
I32 = mybir.dt.int32
U32 = mybir.dt.uint32
AF = mybir.ActivationFunctionType
ALU = mybir.AluOpType
AX = mybir.AxisListType

NCORES = 8
D = 1024
EPS = 1e-6


class Sched:
    LIMIT = 20000

    def __init__(self, nc):
        self.nc = nc
        self.eng = {'pe': nc.tensor, 'act': nc.scalar, 'dve': nc.vector, 'pool': nc.gpsimd, 'sp': nc.sync}
        self.epoch = {k: 0 for k in self.eng}
        self.sem = {(k, 0): nc.alloc_semaphore('s_%s_0' % k) for k in self.eng}
        self.cnt = {k: 0 for k in self.eng}
        self.seen = {k: {} for k in self.eng}
        self.ndsem = 24
        self.dsem = [nc.alloc_semaphore('d_%d' % i) for i in range(self.ndsem)]
        self.dcnt = [0] * self.ndsem
        self.dnext = 0
        self.lastw = {}
        self.readers = {}
        self.ninst = 0

    def _wait(self, e, tok, kindw):
        kind, key, val = tok
        if kind == 'e':
            src = key[0]
            if src == e and (e == 'pe' or kindw != 'raw'):
                return
        seen = self.seen[e]
        k = (kind, key)
        if seen.get(k, 0) >= val:
            return
        sem = self.sem[key] if kind == 'e' else self.dsem[key]
        self.eng[e].wait_ge(sem, val)
        seen[k] = val

    def _deps(self, e, reads, writes):
        for b in reads:
            t = self.lastw.get(b)
            if t is not None:
                self._wait(e, t, 'raw')
        for b in writes:
            t = self.lastw.get(b)
            if t is not None:
                self._wait(e, t, 'waw')
            for t in self.readers.get(b, ()):
                self._wait(e, t, 'war')

    def _commit(self, tok, reads, writes):
        for b in writes:
            self.lastw[b] = tok
            self.readers[b] = []
        for b in reads:
            if b in writes:
                continue
            self.readers.setdefault(b, []).append(tok)

    def op(self, e, fn, reads=(), writes=()):
        self._deps(e, reads, writes)
        ins = fn(self.eng[e])
        if self.cnt[e] >= self.LIMIT:
            self.epoch[e] += 1
            self.cnt[e] = 0
            self.sem[(e, self.epoch[e])] = self.nc.alloc_semaphore('s_%s_%d' % (e, self.epoch[e]))
        self.cnt[e] += 1
        key = (e, self.epoch[e])
        ins.then_inc(self.sem[key], 1)
        tok = ('e', key, self.cnt[e])
        self._commit(tok, reads, writes)
        self.ninst += 1
        return tok

    def dma(self, e, out, in_, reads=(), writes=(), **kw):
        return self.dmaf(e, lambda g: g.dma_start(out=out, in_=in_, **kw), reads, writes)

    def dmaf(self, e, fn, reads=(), writes=()):
        i = self.dnext
        self.dnext = (self.dnext + 1) % self.ndsem
        if self.dcnt[i] > 0:
            self._wait(e, ('d', i, self.dcnt[i]), 'raw')
        self._deps(e, reads, writes)
        ins = fn(self.eng[e])
        self.dcnt[i] += 16
        ins.then_inc(self.dsem[i], 16)
        tok = ('d', i, self.dcnt[i])
        self._commit(tok, reads, writes)
        self.ninst += 1
        return tok

    def finish(self, bufs, e='sp'):
        for b in bufs:
            t = self.lastw.get(b)
            if t is not None:
                self._wait(e, t, 'raw')


class Prog:
    def __init__(self):
        self.nc = bass.Bass("TRN2", target_bir_lowering=False)
        self.S = Sched(self.nc)
        self.outs = []
        self._n = 0

    def din(self, name, shape, dt=F32):
        return self.nc.dram_tensor(name, list(shape), dt, kind="ExternalInput").ap()

    def dout(self, name, shape, dt=F32):
        self.outs.append(name)
        return self.nc.dram_tensor(name, list(shape), dt, kind="ExternalOutput").ap()

    def sb(self, name, shape, dt=F32):
        return self.nc.alloc_sbuf_tensor(name, list(shape), dt)

    def ps(self, name, shape):
        return self.nc.alloc_psum_tensor(name, list(shape), F32)

    def ident(self):
        idf = self.sb("ident", [128, 128])
        S = self.S
        S.op('pool', lambda g: g.memset(idf[:], 1.0), writes=['ident'])
        S.op('pool', lambda g: g.affine_select(out=idf[:], in_=idf[:], pattern=[[-1, 128]], compare_op=ALU.is_equal,
                                               fill=0.0, base=0, channel_multiplier=1), reads=['ident'], writes=['ident'])
        return idf

    def run(self, in_maps):
        self.S.finish(self.outs)
        res = run_bass_kernel_spmd(self.nc, in_maps, core_ids=list(range(len(in_maps))))
        return res.results


def emit_mod(P, csil, v, adaw, adab, col0, ncols, outt, key, ones, tmpname, BW=256, pmx=None):
    S = P.S
    if not hasattr(P, '_modtmp'):
        P._modtmp = {}
    if tmpname not in P._modtmp:
        P._modtmp[tmpname] = (P.sb(tmpname + "_lt", [128, 8, 128]), [P.sb(tmpname + "_w%d" % i, [128, 8, BW]) for i in range(2)],
                              [P.ps(tmpname + "_pm%d" % i, [128, BW]) for i in range(2)] if pmx is None else None, P.sb(tmpname + "_b", [128, getattr(P, "mod_bw", 2048)]))
    lt, wts, pm, bt = P._modtmp[tmpname]
    pmk = [tmpname + '_pm0', tmpname + '_pm1']
    if pmx is not None:
        pm, pmk = pmx
    for k in range(8):
        S.op('dve', lambda e: e.tensor_scalar(out=lt[:, k, :], in0=ones[:, 0:128], scalar1=csil[:, v, k:k + 1], scalar2=None,
                                              op0=ALU.mult), reads=['csil', 'ones'], writes=[tmpname + '_lt%d' % k])
    wv = adaw.rearrange("(k p) n -> p k n", p=128)
    nb = (ncols + BW - 1) // BW
    S.dma('sp', bt[:, 0:ncols], adab[:, col0:col0 + ncols], writes=[tmpname + '_b'])
    for j in range(nb):
        c0 = col0 + j * BW
        w = min(BW, col0 + ncols - c0)
        wt = wts[j % 2]
        wk = tmpname + '_w%d' % (j % 2)
        S.dma('sp', wt[:, :, 0:w], wv[:, :, c0:c0 + w], writes=[wk])
        pk = pmk[j % 2]
        for k in range(8):
            S.op('pe', lambda e: e.matmul(pm[j % 2][:, 0:w], lhsT=lt[:, k, :], rhs=wt[:, k, 0:w], start=(k == 0), stop=(k == 7)),
                 reads=[wk, tmpname + '_lt%d' % k], writes=[pk])
        S.op('dve', lambda e: e.tensor_tensor(out=outt[:, j * BW:j * BW + w], in0=pm[j % 2][:, 0:w], in1=bt[:, j * BW:j * BW + w],
                                              op=ALU.add), reads=[pk, tmpname + '_b'], writes=[key])


def emit_rmsnorm_mod(P, xt, xkey, wmod, shb, modkeys, outt, okey, tmp):
    S = P.S
    junk, ss = tmp
    S.op('act', lambda e: e.activation(out=junk[:], in_=xt[:], func=AF.Square, accum_out=ss[:, 0:1]), reads=[xkey], writes=[okey, 'ss'])
    S.op('act', lambda e: e.activation(out=ss[:, 1:2], in_=ss[:, 0:1], func=AF.Sqrt, bias=P.epsb[:, 0:1], scale=1.0 / D), reads=['ss', 'epsb'], writes=['ss1'])
    S.op('dve', lambda e: e.reciprocal(out=ss[:, 2:3], in_=ss[:, 1:2]), reads=['ss1'], writes=['ss2'])
    S.op('dve', lambda e: e.scalar_tensor_tensor(out=outt[:], in0=xt[:], scalar=ss[:, 2:3], in1=wmod[:], op0=ALU.mult, op1=ALU.mult),
         reads=[xkey, 'ss2'] + modkeys, writes=[okey])
    if shb is not None:
        S.op('pool', lambda e: e.tensor_tensor(out=outt[:], in0=outt[:], in1=shb[:], op=ALU.add), reads=[okey] + modkeys, writes=[okey])


def consts(P):
    S = P.S
    P.ones = P.sb("ones", [128, 512])
    S.op('pool', lambda g: g.memset(P.ones[:], 1.0), writes=['ones'])
    P.epsb = P.sb("epsb", [128, 1])
    S.op('pool', lambda g: g.memset(P.epsb[:], EPS), writes=['epsb'])
    P.idf = P.ident()


L1_TILES = 33
IN_W = 3616


def build_l1():
    P = Prog()
    S = P.S
    X = P.din("X", [L1_TILES * 128, D])
    cT = P.din("cT", [128, 2, 8])
    adaw = P.din("adaw", [D, 2048])
    adab = P.din("adab", [128, 2048])
    n1w = P.din("n1w", [128, D])
    win = P.din("win", [D, IN_W])
    Pout = P.dout("P", [L1_TILES * 128, IN_W])
    consts(P)
    csil = P.sb("csil", [128, 2, 8])
    S.dma('sp', csil[:], cT, writes=['csil'])
    S.op('act', lambda e: e.activation(out=csil[:], in_=csil[:], func=AF.Silu), reads=['csil'], writes=['csil'])
    n1wt = P.sb("n1wt", [128, D])
    S.dma('sp', n1wt[:], n1w, writes=['n1w'])
    wint = P.sb("wint", [128, 8, IN_W])
    winv = win.rearrange("(k p) n -> p k n", p=128)
    for k in range(8):
        S.dma('sp', wint[:, k, :], winv[:, k, :], writes=['win%d' % k])
    mods = []
    for v in range(2):
        mb = P.sb("modb%d" % v, [128, 2048])
        emit_mod(P, csil, v, adaw, adab, 0, 2048, mb, 'modb%d' % v, P.ones, "m")
        wm = P.sb("wmod%d" % v, [128, D])
        S.op('dve', lambda e: e.scalar_tensor_tensor(out=wm[:], in0=mb[:, 1024:2048], scalar=1.0, in1=n1wt[:], op0=ALU.add, op1=ALU.mult),
             reads=['modb%d' % v, 'n1w'], writes=['wmod%d' % v])
        mods.append((wm, mb))
    xts = [P.sb("xt%d" % i, [128, D]) for i in range(2)]
    ss = P.sb("ss", [128, 4])
    ht = P.sb("ht", [128, D])
    junk = ht
    hT = P.sb("hT", [128, D])
    pT = P.ps("pT", [128, D])
    pp = [P.ps("pp%d" % i, [128, 512]) for i in range(4)]
    pts = [P.sb("pt0", [128, IN_W])] * 2
    for t in range(L1_TILES):
        v = 1 if t == L1_TILES - 1 else 0
        xt = xts[t % 2]
        xk = 'xt%d' % (t % 2)
        S.dma('sp', xt[:], X[t * 128:(t + 1) * 128, :], writes=[xk])
        wm, mb = mods[v]
        emit_rmsnorm_mod(P, xt, xk, wm, mb[:, 0:1024], ['wmod%d' % v, 'modb%d' % v], ht, 'ht', (junk, ss))
        for k in range(8):
            S.op('pe', lambda e: e.transpose(pT[:, k * 128:(k + 1) * 128], ht[:, k * 128:(k + 1) * 128], P.idf[:]),
                 reads=['ht', 'ident'], writes=['pT%d' % k])
        S.op('act', lambda e: e.activation(out=hT[:, 0:512], in_=pT[:, 0:512], func=AF.Copy), reads=['pT%d' % k for k in range(4)], writes=['hT0'])
        S.op('dve', lambda e: e.tensor_copy(out=hT[:, 512:1024], in_=pT[:, 512:1024]), reads=['pT%d' % k for k in range(4, 8)], writes=['hT1'])
        pt = pts[t % 2]
        ptk = 'pt0'
        ncb = (IN_W + 511) // 512
        for cb in range(ncb):
            c0 = cb * 512
            w = min(512, IN_W - c0)
            pq = pp[cb % 4]
            for k in range(8):
                S.op('pe', lambda e: e.matmul(pq[:, 0:w], lhsT=hT[:, k * 128:(k + 1) * 128], rhs=wint[:, k, c0:c0 + w], start=(k == 0), stop=(k == 7)),
                     reads=['hT%d' % (k // 4), 'win%d' % k], writes=['pp%d' % (cb % 4)])
            if cb % 2 == 0:
                S.op('act', lambda e: e.activation(out=pt[:, c0:c0 + w], in_=pq[:, 0:w], func=AF.Copy), reads=['pp%d' % (cb % 4)], writes=[ptk + '_%d' % cb])
            else:
                S.op('dve', lambda e: e.tensor_copy(out=pt[:, c0:c0 + w], in_=pq[:, 0:w]), reads=['pp%d' % (cb % 4)], writes=[ptk + '_%d' % cb])
        S.dma('pool', Pout[t * 128:(t + 1) * 128, :], pt[:], reads=[ptk + '_%d' % cb for cb in range(ncb)], writes=['P'])
    return P


def rep128(v):
    v = np.asarray(v, np.float32).reshape(1, -1)
    return np.ascontiguousarray(np.broadcast_to(v, (128, v.shape[1])))


def cT_layout(vecs):
    vecs = np.asarray(vecs, np.float32)
    return np.ascontiguousarray(vecs.reshape(vecs.shape[0], 8, 128).transpose(2, 0, 1))


def run_l1(x, c, ctx, c_ctx, ada_w0, ada_b0, norm1_w0, w_in0):
    P = build_l1()
    in_maps = []
    for core in range(NCORES):
        b, hf = core // 2, core % 2
        X = np.concatenate([x[b, hf * 4096:(hf + 1) * 4096], ctx[b, hf * 128:(hf + 1) * 128]], axis=0)
        in_maps.append(dict(X=np.ascontiguousarray(X), cT=cT_layout(np.stack([c[b], c_ctx])),
                            adaw=np.ascontiguousarray(ada_w0[:, :2048]), adab=rep128(ada_b0[:2048]),
                            n1w=rep128(norm1_w0), win=np.ascontiguousarray(w_in0)))
    res = P.run(in_maps)
    Plat = np.empty((4, 8192, IN_W), np.float32)
    Pctx = np.empty((4, 256, IN_W), np.float32)
    for core in range(NCORES):
        b, hf = core // 2, core % 2
        r = res[core]["P"]
        Plat[b, hf * 4096:(hf + 1) * 4096] = r[:4096]
        Pctx[b, hf * 128:(hf + 1) * 128] = r[4096:]
    return Plat, Pctx


def bc(ap, axis, n):
    a = ap.unsqueeze(axis)
    shp = list(a.shape)
    shp[axis] = n
    return a.broadcast_to(shp)


def build_l2():
    P = Prog()
    S = P.S
    NT = L1_TILES
    R = NT * 128
    Pp = P.din("Pp", [R, 1536]); Pc = P.din("Pc", [R, 1536]); Pn = P.din("Pn", [R, 1536])
    Pab = P.din("Pab", [R, 32]); Pqk = P.din("Pqk", [R, 1024])
    cosT = P.din("cosT", [R, 32]); sinT = P.din("sinT", [R, 32])
    convw = P.din("convw", [128, 3, 1536]); alog = P.din("alog", [128, 16]); dtb = P.din("dtb", [128, 16])
    QKV = P.dout("QKV", [R, 1536]); BG = P.dout("BG", [R, 32]); QKr = P.dout("QKr", [R, 1024])
    consts(P)
    cw = P.sb("cw", [128, 3, 1536]); S.dma('sp', cw[:], convw, writes=['cw'])
    negA = P.sb("negA", [128, 16]); S.dma('sp', negA[:], alog, writes=['negA'])
    S.op('act', lambda e: e.activation(out=negA[:], in_=negA[:], func=AF.Exp), reads=['negA'], writes=['negA'])
    S.op('dve', lambda e: e.tensor_scalar(out=negA[:], in0=negA[:], scalar1=-1.0, scalar2=None, op0=ALU.mult), reads=['negA'], writes=['negA'])
    dtbt = P.sb("dtbt", [128, 16]); S.dma('sp', dtbt[:], dtb, writes=['dtbt'])
    a0 = P.sb("a0", [128, 1536]); a1 = P.sb("a1", [128, 1536]); a2 = P.sb("a2", [128, 1536])
    qkv = P.sb("qkv", [128, 1536]); sq = P.sb("sq", [128, 1024]); st = P.sb("st", [128, 3, 16])
    ab = P.sb("ab", [128, 32]); bg = P.sb("bg", [128, 32]); tt = P.sb("tt", [128, 16])
    qk = P.sb("qk", [128, 1024]); qo = P.sb("qo", [128, 1024]); cs = P.sb("cs", [128, 2, 32])
    r0 = P.sb("r0", [128, 512]); r1 = P.sb("r1", [128, 512])
    for t in range(NT):
        rs = slice(t * 128, (t + 1) * 128)
        S.dma('sp', a0[:], Pp[rs, :], writes=['a0']); S.dma('sp', a1[:], Pc[rs, :], writes=['a1']); S.dma('sp', a2[:], Pn[rs, :], writes=['a2'])
        S.dma('sp', ab[:], Pab[rs, :], writes=['ab']); S.dma('sp', qk[:], Pqk[rs, :], writes=['qk'])
        S.dma('sp', cs[:, 0, :], cosT[rs, :], writes=['cs0']); S.dma('sp', cs[:, 1, :], sinT[rs, :], writes=['cs1'])
        S.op('dve', lambda e: e.tensor_tensor(out=a0[:], in0=a0[:], in1=cw[:, 0, :], op=ALU.mult), reads=['a0', 'cw'], writes=['a0'])
        S.op('pool', lambda e: e.tensor_tensor(out=a1[:], in0=a1[:], in1=cw[:, 1, :], op=ALU.mult), reads=['a1', 'cw'], writes=['a1'])
        S.op('dve', lambda e: e.tensor_tensor(out=a2[:], in0=a2[:], in1=cw[:, 2, :], op=ALU.mult), reads=['a2', 'cw'], writes=['a2'])
        S.op('pool', lambda e: e.tensor_tensor(out=a1[:], in0=a1[:], in1=a0[:], op=ALU.add), reads=['a1', 'a0'], writes=['a1'])
        S.op('dve', lambda e: e.tensor_tensor(out=a1[:], in0=a1[:], in1=a2[:], op=ALU.add), reads=['a1', 'a2'], writes=['a1'])
        S.op('act', lambda e: e.activation(out=qkv[:], in_=a1[:], func=AF.Silu), reads=['a1'], writes=['qkv'])
        S.op('act', lambda e: e.activation(out=sq[:], in_=qkv[:, 0:1024], func=AF.Square), reads=['qkv'], writes=['sq'])
        S.op('dve', lambda e: e.tensor_reduce(out=st[:, 0, :], in_=sq[:].rearrange("p (g d) -> p g d", d=64), axis=AX.X, op=ALU.add), reads=['sq'], writes=['st0'])
        S.op('act', lambda e: e.activation(out=st[:, 1, :], in_=st[:, 0, :], func=AF.Sqrt, bias=P.epsb[:, 0:1], scale=1.0), reads=['st0', 'epsb'], writes=['st1'])
        S.op('dve', lambda e: e.reciprocal(out=st[:, 2, :], in_=st[:, 1, :]), reads=['st1'], writes=['st2'])
        S.op('dve', lambda e: e.tensor_scalar(out=st[:, 2, 0:8], in0=st[:, 2, 0:8], scalar1=0.125, scalar2=None, op0=ALU.mult), reads=['st2'], writes=['st2'])
        S.op('dve', lambda e: e.tensor_tensor(out=qkv[:, 0:1024].rearrange("p (g d) -> p g d", d=64), in0=qkv[:, 0:1024].rearrange("p (g d) -> p g d", d=64),
                                              in1=bc(st[:, 2, :], 2, 64), op=ALU.mult), reads=['qkv', 'st2'], writes=['qkv'])
        S.dma('pool', QKV[rs, :], qkv[:], reads=['qkv'], writes=['QKV'])
        S.op('act', lambda e: e.activation(out=bg[:, 0:16], in_=ab[:, 0:16], func=AF.Sigmoid), reads=['ab'], writes=['bg0'])
        S.op('dve', lambda e: e.tensor_tensor(out=tt[:], in0=ab[:, 16:32], in1=dtbt[:], op=ALU.add), reads=['ab', 'dtbt'], writes=['tt'])
        S.op('act', lambda e: e.activation(out=tt[:], in_=tt[:], func=AF.Exp), reads=['tt'], writes=['tt'])
        S.op('act', lambda e: e.activation(out=tt[:], in_=tt[:], func=AF.Ln, bias=P.ones[:, 0:1], scale=1.0), reads=['tt', 'ones'], writes=['tt'])
        S.op('dve', lambda e: e.tensor_tensor(out=bg[:, 16:32], in0=tt[:], in1=negA[:], op=ALU.mult), reads=['tt', 'negA'], writes=['bg1'])
        S.dma('pool', BG[rs, :], bg[:], reads=['bg0', 'bg1'], writes=['BG'])
        v = qk[:].rearrange("p (g h d) -> p g h d", h=2, d=32)
        o = qo[:].rearrange("p (g h d) -> p g h d", h=2, d=32)
        cb = bc(cs[:, 0, :], 1, 16); sb_ = bc(cs[:, 1, :], 1, 16)
        r0v = r0[:].rearrange("p (g d) -> p g d", d=32); r1v = r1[:].rearrange("p (g d) -> p g d", d=32)
        S.op('dve', lambda e: e.tensor_tensor(out=r0v, in0=v[:, :, 0, :], in1=cb, op=ALU.mult), reads=['qk', 'cs0'], writes=['r0'])
        S.op('pool', lambda e: e.tensor_tensor(out=r1v, in0=v[:, :, 1, :], in1=sb_, op=ALU.mult), reads=['qk', 'cs1'], writes=['r1'])
        S.op('dve', lambda e: e.tensor_tensor(out=o[:, :, 0, :], in0=r0v, in1=r1v, op=ALU.subtract), reads=['r0', 'r1'], writes=['qo0'])
        S.op('pool', lambda e: e.tensor_tensor(out=r0v, in0=v[:, :, 0, :], in1=sb_, op=ALU.mult), reads=['qk', 'cs1', 'r0'], writes=['r0'])
        S.op('dve', lambda e: e.tensor_tensor(out=r1v, in0=v[:, :, 1, :], in1=cb, op=ALU.mult), reads=['qk', 'cs0', 'r1'], writes=['r1'])
        S.op('pool', lambda e: e.tensor_tensor(out=o[:, :, 1, :], in0=r0v, in1=r1v, op=ALU.add), reads=['r0', 'r1'], writes=['qo1'])
        S.dma('pool', QKr[rs, :], qo[:], reads=['qo0', 'qo1'], writes=['QKr'])
    return P


def rope_tables():
    rows = 8192 // 64
    row = np.repeat(np.arange(rows, dtype=np.float32), 64)
    col = np.tile(np.arange(64, dtype=np.float32), rows)
    inv = (10000.0 ** (-np.arange(0, 32, 2, dtype=np.float32) / 32)).astype(np.float32)
    ang = np.concatenate([row[:, None] * inv, col[:, None] * inv], axis=-1).astype(np.float32)
    return np.cos(ang).astype(np.float32), np.sin(ang).astype(np.float32)


def shift_rows(a, s):
    b = np.zeros_like(a)
    if s == -1:
        b[..., 1:, :] = a[..., :-1, :]
    else:
        b[..., :-1, :] = a[..., 1:, :]
    return b


def run_l2(Plat, Pctx, conv_w0, a_log0, dt_bias0):
    P = build_l2()
    cos, sin = rope_tables()
    in_maps = []
    for core in range(NCORES):
        b, hf = core // 2, core % 2
        sl, sc = slice(hf * 4096, (hf + 1) * 4096), slice(hf * 128, (hf + 1) * 128)
        cat = lambda A, B: np.ascontiguousarray(np.concatenate([A, B], axis=0))
        ql, qc = Plat[b, :, :1536], Pctx[b, :, :1536]
        in_maps.append(dict(
            Pp=cat(shift_rows(ql, -1)[sl], shift_rows(qc, -1)[sc]), Pc=cat(ql[sl], qc[sc]), Pn=cat(shift_rows(ql, 1)[sl], shift_rows(qc, 1)[sc]),
            Pab=cat(Plat[b, sl, 2048:2080], Pctx[b, sc, 2048:2080]), Pqk=cat(Plat[b, sl, 2080:3104], Pctx[b, sc, 2080:3104]),
            cosT=cat(cos[sl], np.ones((128, 32), np.float32)), sinT=cat(sin[sl], np.zeros((128, 32), np.float32)),
            convw=np.ascontiguousarray(np.broadcast_to(conv_w0[None], (128, 3, 1536))), alog=rep128(a_log0.reshape(-1)), dtb=rep128(dt_bias0.reshape(-1))))
    res = P.run(in_maps)
    out = {}
    for name, w in (("QKV", 1536), ("BG", 32), ("QKr", 1024)):
        lat = np.empty((4, 8192, w), np.float32); cx = np.empty((4, 256, w), np.float32)
        for core in range(NCORES):
            b, hf = core // 2, core % 2
            r = res[core][name]
            lat[b, hf * 4096:(hf + 1) * 4096] = r[:4096]
            cx[b, hf * 128:(hf + 1) * 128] = r[4096:]
        out[name] = (lat, cx)
    return out


GD_CH = 132
GD_CTX = 4


import os
LIM = int(os.environ.get('LIM', '99'))


def build_l3(nch=GD_CH, nctx=GD_CTX, stage=9):
    P = Prog()
    S = P.S
    T = nch * 64
    Kt = P.din("Kt", [T, 512]); Vt = P.din("Vt", [T, 512])
    KT = P.din("KT", [nch, 64, 512]); QT = P.din("QT", [nch, 64, 512])
    Bt = P.din("Bt", [T, 8]); Gt = P.din("Gt", [T, 8])
    O = P.dout("O", [(nch - nctx) * 64, 512])
    N = 64

    def mask(name, op, sgn=1):
        m = P.sb(name, [N, N])
        S.op('pool', lambda g: g.memset(m[:], 1.0), writes=[name])
        S.op('pool', lambda g: g.affine_select(out=m[:], in_=m[:], pattern=[[-sgn, N]], compare_op=op, fill=0.0, base=0, channel_multiplier=sgn),
             reads=[name], writes=[name])
        return m
    mL = mask("mL", ALU.is_ge); mLs = mask("mLs", ALU.is_gt); mU = mask("mU", ALU.is_ge, -1); mUs = mask("mUs", ALU.is_gt, -1); I64 = mask("I64", ALU.is_equal)
    ones = P.sb("ones64", [N, N]); S.op('pool', lambda g: g.memset(ones[:], 1.0), writes=['ones64'])
    B = [P.ps("b%d" % i, [N, 512]) for i in range(8)]
    w3 = lambda t: t[:].rearrange("p (h j) -> p h j", j=64)
    sbw = lambda name: P.sb(name, [N, 512])
    kt = sbw("kt"); vt = sbw("vt"); kT = sbw("kT"); qT = sbw("qT")
    b8 = P.sb("b8", [N, 8]); g8 = P.sb("g8", [N, 8]); gc = P.sb("gc", [N, 8]); egc = P.sb("egc", [N, 8]); nb8 = P.sb("nb8", [N, 8]); bge = P.sb("bge", [N, 8])
    R = sbw("R"); arg = sbw("arg"); expgB = sbw("expgB"); t1 = sbw("t1"); t2 = sbw("t2")
    Dls = sbw("Dls"); Dus = sbw("Dus"); Du = sbw("Du"); E2 = sbw("E2")
    Q = [sbw("Q0"), sbw("Q1")]; QTt = [sbw("QT0"), sbw("QT1")]; PT = [sbw("PT0"), sbw("PT1")]
    AT = sbw("AT"); vb = sbw("vb"); kbg = sbw("kbg"); U = sbw("U"); WT = sbw("WT"); ktl = sbw("ktl"); qdT = sbw("qdT")
    vnew = sbw("vnew"); Sst = sbw("Sst"); ot = sbw("ot"); stmp = sbw("stmp")
    S.op('pool', lambda g: g.memset(Sst[:], 0.0), writes=['Sst'])
    hs = lambda t, h: t[:, h * 64:(h + 1) * 64]

    for c in range(nch):
        rs = slice(c * 64, (c + 1) * 64)
        S.dma('sp', kt[:], Kt[rs, :], writes=['kt']); S.dma('sp', vt[:], Vt[rs, :], writes=['vt'])
        S.dma('sp', kT[:], KT[c], writes=['kT']); S.dma('sp', qT[:], QT[c], writes=['qT'])
        S.dma('sp', b8[:], Bt[rs, :], writes=['b8']); S.dma('sp', g8[:], Gt[rs, :], writes=['g8'])
        if stage < -3: continue
        S.op('dve', lambda e: e.tensor_tensor(out=w3(R), in0=bc(g8[:], 2, 64), in1=bc(mU[:], 1, 8), op=ALU.mult), reads=['g8', 'mU'], writes=['R'])
        S.op('pe', lambda e: e.matmul(B[0][:], lhsT=ones[:], rhs=R[:], start=True, stop=True), reads=['ones64', 'R'], writes=['b0'])
        if stage < -2: continue
        S.op('pe', lambda e: e.matmul(B[4][:, 0:8], lhsT=mU[:], rhs=g8[:], start=True, stop=True), reads=['mU', 'g8'], writes=['b4'])
        S.op('act', lambda e: e.activation(out=gc[:], in_=B[4][:, 0:8], func=AF.Copy), reads=['b4'], writes=['gc'])
        if stage < -1: continue
        if LIM > 0:
            S.op('dve', lambda e: e.tensor_tensor(out=w3(arg), in0=bc(gc[:], 2, 64), in1=w3(B[0]), op=ALU.subtract), reads=['gc', 'b0'], writes=['arg'])
        if LIM > 1:
            S.op('dve', lambda e: e.tensor_scalar(out=expgB[:], in0=B[0][:], scalar1=-80.0, scalar2=None, op0=ALU.max), reads=['b0'], writes=['expgB'])
            S.op('act', lambda e: e.activation(out=expgB[:], in_=expgB[:], func=AF.Exp), reads=['expgB'], writes=['expgB'])
        if LIM > 2:
            S.op('dve', lambda e: e.tensor_scalar(out=egc[:], in0=gc[:], scalar1=-80.0, scalar2=None, op0=ALU.max), reads=['gc'], writes=['egc'])
            S.op('act', lambda e: e.activation(out=egc[:], in_=egc[:], func=AF.Exp), reads=['egc'], writes=['egc'])
        if LIM > 3:
            S.op('dve', lambda e: e.tensor_scalar(out=t1[:], in0=arg[:], scalar1=0.0, scalar2=-80.0, op0=ALU.min, op1=ALU.max), reads=['arg'], writes=['t1'])
        if LIM > 4:
            S.op('act', lambda e: e.activation(out=t1[:], in_=t1[:], func=AF.Exp), reads=['t1'], writes=['t1'])
        if LIM > 5:
            S.op('dve', lambda e: e.tensor_tensor(out=w3(Dls), in0=w3(t1), in1=bc(mLs[:], 1, 8), op=ALU.mult), reads=['t1', 'mLs'], writes=['Dls'])
        if LIM > 6:
            S.op('dve', lambda e: e.tensor_scalar(out=t2[:], in0=arg[:], scalar1=-1.0, scalar2=0.0, op0=ALU.mult, op1=ALU.min), reads=['arg'], writes=['t2'])
            S.op('dve', lambda e: e.tensor_scalar(out=t2[:], in0=t2[:], scalar1=-80.0, scalar2=None, op0=ALU.max), reads=['t2'], writes=['t2'])
        if LIM > 7:
            S.op('act', lambda e: e.activation(out=E2[:], in_=t2[:], func=AF.Exp), reads=['t2'], writes=['E2'])
        if LIM > 8:
            S.op('dve', lambda e: e.tensor_tensor(out=w3(Dus), in0=w3(E2), in1=bc(mUs[:], 1, 8), op=ALU.mult), reads=['E2', 'mUs'], writes=['Dus'])
        if LIM > 9:
            S.op('pool', lambda e: e.tensor_tensor(out=w3(Du), in0=w3(E2), in1=bc(mU[:], 1, 8), op=ALU.mult), reads=['E2', 'mU'], writes=['Du'])
        if stage < 1: continue
        S.op('dve', lambda e: e.tensor_tensor(out=w3(R), in0=bc(b8[:], 2, 64), in1=bc(I64[:], 1, 8), op=ALU.mult), reads=['b8', 'I64', 'R'], writes=['R'])
        S.op('pe', lambda e: e.matmul(B[1][:], lhsT=ones[:], rhs=R[:], start=True, stop=True), reads=['ones64', 'R'], writes=['b1'])
        S.op('dve', lambda e: e.tensor_scalar(out=nb8[:], in0=b8[:], scalar1=-1.0, scalar2=None, op0=ALU.mult), reads=['b8'], writes=['nb8'])
        if stage < 2: continue
        for h in range(8):
            S.op('pe', lambda e: e.matmul(hs(B[2], h), lhsT=hs(kT, h), rhs=hs(kT, h), start=True, stop=True), reads=['kT'], writes=['b2'])
        for h in range(8):
            S.op('pe', lambda e: e.matmul(hs(B[3], h), lhsT=hs(kT, h), rhs=hs(qT, h), start=True, stop=True), reads=['kT', 'qT'], writes=['b3'])
        if stage < 3: continue
        S.op('dve', lambda e: e.tensor_tensor(out=t1[:], in0=B[2][:], in1=Dls[:], op=ALU.mult), reads=['b2', 'Dls', 't1'], writes=['t1'])
        S.op('dve', lambda e: e.tensor_tensor(out=w3(Q[0]), in0=w3(t1), in1=bc(nb8[:], 2, 64), op=ALU.mult), reads=['t1', 'nb8'], writes=['Q0'])
        S.op('dve', lambda e: e.tensor_tensor(out=t2[:], in0=B[2][:], in1=Dus[:], op=ALU.mult), reads=['b2', 'Dus', 't2'], writes=['t2'])
        S.op('dve', lambda e: e.scalar_tensor_tensor(out=QTt[0][:], in0=B[1][:], scalar=-1.0, in1=t2[:], op0=ALU.mult, op1=ALU.mult), reads=['b1', 't2'], writes=['QT0'])
        S.op('dve', lambda e: e.tensor_tensor(out=AT[:], in0=B[3][:], in1=Du[:], op=ALU.mult), reads=['b3', 'Du'], writes=['AT'])
        S.op('pool', lambda e: e.tensor_tensor(out=w3(PT[0]), in0=w3(QTt[0]), in1=bc(I64[:], 1, 8), op=ALU.add), reads=['QT0', 'I64'], writes=['PT0'])
        if stage < 4: continue
        cur = 0
        for l in range(5):
            nx = 1 - cur
            qk_, qtk, qn, qtn = 'Q%d' % cur, 'QT%d' % cur, 'Q%d' % nx, 'QT%d' % nx
            for h in range(8):
                S.op('pe', lambda e: e.matmul(hs(B[4], h), lhsT=hs(QTt[cur], h), rhs=hs(Q[cur], h), start=True, stop=True), reads=[qk_, qtk], writes=['b4'])
            if l < 4:
                for h in range(8):
                    S.op('pe', lambda e: e.matmul(hs(B[5], h), lhsT=hs(Q[cur], h), rhs=hs(QTt[cur], h), start=True, stop=True), reads=[qk_, qtk], writes=['b5'])
            S.op('act', lambda e: e.activation(out=Q[nx][:], in_=B[4][:], func=AF.Copy), reads=['b4'], writes=[qn])
            if l < 4:
                S.op('dve', lambda e: e.tensor_copy(out=QTt[nx][:], in_=B[5][:]), reads=['b5'], writes=[qtn])
            pk, pn = 'PT%d' % (l % 2), 'PT%d' % ((l + 1) % 2)
            for h in range(8):
                S.op('pe', lambda e: e.matmul(hs(B[6], h), lhsT=hs(Q[nx], h), rhs=hs(PT[l % 2], h), start=True, stop=True), reads=[qn, pk], writes=['b6'])
            S.op('dve', lambda e: e.tensor_tensor(out=PT[(l + 1) % 2][:], in0=B[6][:], in1=PT[l % 2][:], op=ALU.add), reads=['b6', pk], writes=[pn])
            cur = nx
        TT = PT[1]; ttk = 'PT1'
        if stage < 5: continue
        S.op('pool', lambda e: e.tensor_tensor(out=w3(vb), in0=w3(vt), in1=bc(b8[:], 2, 64), op=ALU.mult), reads=['vt', 'b8'], writes=['vb'])
        S.op('dve', lambda e: e.tensor_tensor(out=bge[:], in0=b8[:], in1=egc[:], op=ALU.mult), reads=['b8', 'egc'], writes=['bge'])
        S.op('dve', lambda e: e.tensor_tensor(out=w3(kbg), in0=w3(kt), in1=bc(bge[:], 2, 64), op=ALU.mult), reads=['kt', 'bge'], writes=['kbg'])
        S.op('pool', lambda e: e.tensor_tensor(out=w3(ktl), in0=w3(kt), in1=bc(w3(E2)[:, :, 63], 2, 64), op=ALU.mult), reads=['kt', 'E2'], writes=['ktl'])
        S.op('dve', lambda e: e.tensor_tensor(out=qdT[:], in0=qT[:], in1=expgB[:], op=ALU.mult), reads=['qT', 'expgB'], writes=['qdT'])
        for h in range(8):
            S.op('pe', lambda e: e.matmul(hs(B[0], h), lhsT=hs(TT, h), rhs=hs(vb, h), start=True, stop=True), reads=[ttk, 'vb'], writes=['b0'])
        for h in range(8):
            S.op('pe', lambda e: e.matmul(hs(B[1], h), lhsT=hs(kbg, h), rhs=hs(TT, h), start=True, stop=True), reads=[ttk, 'kbg'], writes=['b1'])
        S.op('act', lambda e: e.activation(out=U[:], in_=B[0][:], func=AF.Copy), reads=['b0'], writes=['U'])
        S.op('dve', lambda e: e.tensor_copy(out=WT[:], in_=B[1][:]), reads=['b1'], writes=['WT'])
        if stage < 6: continue
        for h in range(8):
            S.op('pe', lambda e: e.matmul(hs(B[2], h), lhsT=hs(WT, h), rhs=hs(Sst, h), start=True, stop=True), reads=['WT', 'Sst'], writes=['b2'])
        S.op('dve', lambda e: e.tensor_tensor(out=vnew[:], in0=U[:], in1=B[2][:], op=ALU.subtract), reads=['U', 'b2'], writes=['vnew'])
        if c >= nctx:
            for h in range(8):
                S.op('pe', lambda e: e.matmul(hs(B[3], h), lhsT=hs(qdT, h), rhs=hs(Sst, h), start=True, stop=False), reads=['qdT', 'Sst'], writes=['b3'])
                S.op('pe', lambda e: e.matmul(hs(B[3], h), lhsT=hs(AT, h), rhs=hs(vnew, h), start=False, stop=True), reads=['AT', 'vnew'], writes=['b3'])
            S.op('act', lambda e: e.activation(out=ot[:], in_=B[3][:], func=AF.Copy), reads=['b3'], writes=['ot'])
            S.dma('pool', O[(c - nctx) * 64:(c - nctx + 1) * 64, :], ot[:], reads=['ot'], writes=['O'])
        for h in range(8):
            S.op('pe', lambda e: e.matmul(hs(B[7], h), lhsT=hs(ktl, h), rhs=hs(vnew, h), start=True, stop=True), reads=['ktl', 'vnew'], writes=['b7'])
        S.op('dve', lambda e: e.tensor_tensor(out=w3(stmp), in0=w3(Sst), in1=bc(w3(expgB)[:, :, 63], 2, 64), op=ALU.mult), reads=['Sst', 'expgB'], writes=['stmp'])
        S.op('dve', lambda e: e.tensor_tensor(out=Sst[:], in0=stmp[:], in1=B[7][:], op=ALU.add), reads=['stmp', 'b7'], writes=['Sst'])
    if stage < 9:
        S.dma('pool', O[0:64, :], Sst[:], reads=['Sst'], writes=['O'])
    return P


def run_l3(QKV, BG, nch=GD_CH, nctx=GD_CTX, stage=9):
    P = build_l3(nch, nctx, stage)
    L = (nch - nctx) * 64
    C = nctx * 64
    in_maps = []
    for core in range(NCORES):
        b, dr = core // 2, core % 2
        def seq(lat, cx):
            a, c_ = lat[b, :L], cx[b, :C]
            if dr == 1:
                a, c_ = a[::-1], c_[::-1]
            return np.concatenate([c_, a], axis=0)
        qkv = seq(QKV[0], QKV[1]); bg = seq(BG[0], BG[1])
        q, k, v = qkv[:, :512], qkv[:, 512:1024], qkv[:, 1024:1536]
        fm = lambda a: np.ascontiguousarray(a.reshape(nch, 64, 8, 64).transpose(0, 3, 2, 1).reshape(nch, 64, 512))
        in_maps.append(dict(Kt=np.ascontiguousarray(k), Vt=np.ascontiguousarray(v), KT=fm(k), QT=fm(q),
                            Bt=np.ascontiguousarray(bg[:, dr * 8:dr * 8 + 8]), Gt=np.ascontiguousarray(bg[:, 16 + dr * 8:16 + dr * 8 + 8])))
    res = P.run(in_maps)
    O = np.empty((4, 2, L, 512), np.float32)
    for core in range(NCORES):
        b, dr = core // 2, core % 2
        o = res[core]["O"]
        O[b, dr] = o[::-1] if dr == 1 else o
    return O


BF16 = mybir.dt.bfloat16
NKEY = 8448
NKT = NKEY // 128


def build_l4(nq=8192, lam_init=0.2):
    P = Prog()
    S = P.S
    qT = P.din("qT", [2, 2, 64, nq]); kT = P.din("kT", [2, 2, 64, NKEY]); V = P.din("V", [2, NKEY, 128])
    lam = P.din("lam", [128, 4, 64])
    DT = P.dout("DT", [2, 128, nq])
    consts(P)
    onesb = P.sb("onesb", [128, 128], BF16)
    S.op('dve', lambda e: e.tensor_copy(out=onesb[:], in_=P.ones[:, 0:128]), reads=['ones'], writes=['onesb'])
    lt = P.sb("lamt", [128, 4, 64]); S.dma('sp', lt[:], lam, writes=['lamt'])
    lp = P.sb("lamp", [128, 2, 64]); ls = P.sb("lams", [128, 4])
    S.op('dve', lambda e: e.tensor_tensor(out=lp[:, 0, :], in0=lt[:, 0, :], in1=lt[:, 1, :], op=ALU.mult), reads=['lamt'], writes=['lamp0'])
    S.op('dve', lambda e: e.tensor_tensor(out=lp[:, 1, :], in0=lt[:, 2, :], in1=lt[:, 3, :], op=ALU.mult), reads=['lamt'], writes=['lamp1'])
    S.op('dve', lambda e: e.tensor_reduce(out=ls[:, 0:2], in_=lp[:], axis=AX.X, op=ALU.add), reads=['lamp0', 'lamp1'], writes=['lams'])
    S.op('act', lambda e: e.activation(out=ls[:, 0:2], in_=ls[:, 0:2], func=AF.Exp), reads=['lams'], writes=['lams'])
    S.op('dve', lambda e: e.tensor_tensor(out=ls[:, 2:3], in0=ls[:, 1:2], in1=ls[:, 0:1], op=ALU.subtract), reads=['lams'], writes=['lams2'])
    S.op('dve', lambda e: e.tensor_scalar(out=ls[:, 3:4], in0=ls[:, 2:3], scalar1=-lam_init, scalar2=None, op0=ALU.add), reads=['lams2'], writes=['neglam'])
    kTt = P.sb("kTt", [64, 2, NKEY]); Vf = P.sb("Vf", [128, NKT, 128]); Vb = P.sb("Vb", [128, NKT, 128], BF16)
    kTb = P.sb("kTb", [64, 2, NKEY], BF16)
    qTt = P.sb("qTt", [64, 2, 512])
    qTb = [P.sb("qTb%d" % i, [64, 2, 512], BF16) for i in range(2)]
    Pt = [P.sb("Pt%d" % i, [128, 512], BF16) for i in range(3)]
    pS = [P.ps("pS%d" % i, [128, 512]) for i in range(2)]
    pO = [P.ps("pO%d" % i, [128, 512]) for i in range(2)]
    pZ = [P.ps("pZ%d" % i, [128, 512]) for i in range(2)]
    rz = P.sb("rz", [128, 512]); Om = [P.sb("Om%d" % i, [128, 512]) for i in range(2)]
    Dt = [P.sb("Dt%d" % i, [128, 512]) for i in range(2)]
    items = [(h, qc, m, kt) for h in range(2) for qc in range(nq // 512) for m in range(2) for kt in range(NKT)]

    def emit_s(j):
        h, qc, m, kt = items[j]
        if qc == 0 and m == 0 and kt == 0:
            S.dma('sp', kTt[:], kT[h].rearrange("m d k -> d m k"), writes=['kTt'])
            S.dma('sp', Vf[:], V[h].rearrange("(t p) e -> p t e", p=128), writes=['Vf'])
            S.op('dve', lambda e: e.tensor_copy(out=Vb[:], in_=Vf[:]), reads=['Vf'], writes=['Vb'])
            S.op('dve', lambda e: e.tensor_copy(out=kTb[:], in_=kTt[:]), reads=['kTt'], writes=['kTb'])
        if m == 0 and kt == 0:
            S.dma('sp', qTt[:], qT[h, :, :, qc * 512:(qc + 1) * 512].rearrange("m d k -> d m k"), writes=['qTt'])
            S.op('dve', lambda e: e.tensor_copy(out=qTb[qc % 2][:], in_=qTt[:]), reads=['qTt'], writes=['qTb%d' % (qc % 2)])
        S.op('pe', lambda e: e.matmul(pS[j % 2][:], lhsT=kTb[:, m, kt * 128:(kt + 1) * 128], rhs=qTb[qc % 2][:, m, :], start=True, stop=True),
             reads=['kTb', 'qTb%d' % (qc % 2)], writes=['pS%d' % (j % 2)])

    emit_s(0)
    for j, (h, qc, m, kt) in enumerate(items):
        if j + 1 < len(items):
            emit_s(j + 1)
        ps = pS[j % 2]; psk = 'pS%d' % (j % 2)
        pt = Pt[j % 3]; ptk = 'Pt%d' % (j % 3)
        S.op('act', lambda e: e.activation(out=pt[:], in_=ps[:], func=AF.Exp, scale=0.125), reads=[psk], writes=[ptk])
        S.op('pe', lambda e: e.matmul(pO[m][:], lhsT=Vb[:, kt, :], rhs=pt[:], start=(kt == 0), stop=(kt == NKT - 1)),
             reads=['Vb', ptk], writes=['pO%d' % m])
        S.op('pe', lambda e: e.matmul(pZ[m][:], lhsT=onesb[:], rhs=pt[:], start=(kt == 0), stop=(kt == NKT - 1)),
             reads=['onesb', ptk], writes=['pZ%d' % m])
        if kt == NKT - 1:
            S.op('dve', lambda e: e.reciprocal(out=rz[:], in_=pZ[m][:]), reads=['pZ%d' % m], writes=['rz'])
            S.op('dve', lambda e: e.tensor_tensor(out=Om[m][:], in0=pO[m][:], in1=rz[:], op=ALU.mult), reads=['pO%d' % m, 'rz'], writes=['Om%d' % m])
            if m == 1:
                dt_ = Dt[qc % 2]; dk = 'Dt%d' % (qc % 2)
                S.op('dve', lambda e: e.scalar_tensor_tensor(out=dt_[:], in0=Om[1][:], scalar=ls[:, 3:4], in1=Om[0][:], op0=ALU.mult, op1=ALU.add),
                     reads=['Om0', 'Om1', 'neglam'], writes=[dk])
                S.dma('pool', DT[h, :, qc * 512:(qc + 1) * 512], dt_[:], reads=[dk], writes=['DT'])
    return P


def run_l4(QKr, Plat, Pctx, lam_q1, lam_k1, lam_q2, lam_k2, nq=8192):
    P = build_l4(nq)
    lamin = np.ascontiguousarray(np.broadcast_to(np.stack([lam_q1, lam_k1, lam_q2, lam_k2])[None], (128, 4, 64))).astype(np.float32)
    in_maps = []
    for core in range(NCORES):
        b, hp = core // 2, core % 2
        q = QKr[0][b, :nq, 0:512].reshape(nq, 4, 2, 64)[:, 2 * hp:2 * hp + 2]
        k = np.concatenate([QKr[0][b, :, 512:1024], QKr[1][b, :, 512:1024]], axis=0).reshape(NKEY, 4, 2, 64)[:, 2 * hp:2 * hp + 2]
        v = np.concatenate([Plat[b, :, 3104:3616], Pctx[b, :, 3104:3616]], axis=0).reshape(NKEY, 4, 128)[:, 2 * hp:2 * hp + 2]
        in_maps.append(dict(qT=np.ascontiguousarray(q.transpose(1, 2, 3, 0)), kT=np.ascontiguousarray(k.transpose(1, 2, 3, 0)),
                            V=np.ascontiguousarray(v.transpose(1, 0, 2)), lam=lamin))
    res = P.run(in_maps)
    out = np.empty((4, nq, 512), np.float32)
    for core in range(NCORES):
        b, hp = core // 2, core % 2
        dt = res[core]["DT"]
        out[b, :, hp * 256:(hp + 1) * 256] = dt.transpose(2, 0, 1).reshape(nq, 256)
    return out


TOK = 4096
NTT = TOK // 128


def build_l5a(kind):
    P = Prog()
    S = P.S
    X = P.din("X", [TOK, D])
    if kind == 'ab':
        O0 = P.din("O0", [TOK, 512]); O1 = P.din("O1", [TOK, 512]); GATE = P.din("GATE", [TOK, 512]); DL = P.din("DL", [TOK, 512])
        gdnw = P.din("gdnw", [128, 64]); sublnw = P.din("sublnw", [128, 128])
    else:
        FM = P.din("FM", [TOK, D])
    wout = P.din("wout", [D, D])
    cT = P.din("cT", [128, 1, 8]); adaw = P.din("adaw", [D, 1024]); adab = P.din("adab", [128, 1024])
    X1 = P.dout("X1", [TOK, D])
    consts(P)
    csil = P.sb("csil", [128, 1, 8]); S.dma('sp', csil[:], cT, writes=['csil'])
    S.op('act', lambda e: e.activation(out=csil[:], in_=csil[:], func=AF.Silu), reads=['csil'], writes=['csil'])
    g1b = P.sb("g1b", [128, D])
    emit_mod(P, csil, 0, adaw, adab, 0, 1024, g1b, 'g1b', P.ones, "m")
    wt = P.sb("wt", [128, 8, D])
    wv = wout.rearrange("(k p) n -> p k n", p=128)
    for k in range(8):
        S.dma('sp', wt[:, k, :], wv[:, k, :], writes=['wt%d' % k])
    if kind == 'ab':
        gw = P.sb("gw", [128, 64]); S.dma('sp', gw[:], gdnw, writes=['gw'])
        sw = P.sb("sw", [128, 128]); S.dma('sp', sw[:], sublnw, writes=['sw'])
        o0 = P.sb("o0", [128, 512]); o1 = P.sb("o1", [128, 512]); gt = P.sb("gt", [128, 512]); dl = P.sb("dl", [128, 512])
        sq = P.sb("sq", [128, 512]); st = P.sb("st", [128, 3, 12])
    xt = P.sb("xt", [128, D]); mix = P.sb("mix", [128, D]); mixT = P.sb("mixT", [128, D]); x1 = P.sb("x1", [128, D])
    pT = P.ps("pT", [128, D]); pY = P.ps("pY", [128, D])
    for t in range(NTT):
        rs = slice(t * 128, (t + 1) * 128)
        S.dma('sp', xt[:], X[rs, :], writes=['xt'])
        if kind == 'ab':
            S.dma('sp', o0[:], O0[rs, :], writes=['o0']); S.dma('sp', o1[:], O1[rs, :], writes=['o1'])
            S.dma('sp', gt[:], GATE[rs, :], writes=['gt']); S.dma('sp', dl[:], DL[rs, :], writes=['dl'])
            S.op('dve', lambda e: e.tensor_tensor(out=o0[:], in0=o0[:], in1=o1[:], op=ALU.add), reads=['o0', 'o1'], writes=['o0'])
            S.op('act', lambda e: e.activation(out=sq[:], in_=o0[:], func=AF.Square), reads=['o0'], writes=['sq'])
            S.op('dve', lambda e: e.tensor_reduce(out=st[:, 0, 0:8], in_=sq[:].rearrange("p (g d) -> p g d", d=64), axis=AX.X, op=ALU.add), reads=['sq'], writes=['st0a'])
            S.op('act', lambda e: e.activation(out=sq[:], in_=dl[:], func=AF.Square), reads=['dl', 'st0a'], writes=['sq'])
            S.op('dve', lambda e: e.tensor_reduce(out=st[:, 0, 8:12], in_=sq[:].rearrange("p (g d) -> p g d", d=128), axis=AX.X, op=ALU.add), reads=['sq'], writes=['st0b'])
            S.op('act', lambda e: e.activation(out=st[:, 1, 0:8], in_=st[:, 0, 0:8], func=AF.Sqrt, bias=P.epsb[:, 0:1], scale=1.0 / 64), reads=['st0a', 'epsb'], writes=['st1a'])
            S.op('act', lambda e: e.activation(out=st[:, 1, 8:12], in_=st[:, 0, 8:12], func=AF.Sqrt, bias=P.epsb[:, 0:1], scale=1.0 / 128), reads=['st0b', 'epsb'], writes=['st1b'])
            S.op('dve', lambda e: e.reciprocal(out=st[:, 2, :], in_=st[:, 1, :]), reads=['st1a', 'st1b'], writes=['st2'])
            S.op('dve', lambda e: e.tensor_scalar(out=st[:, 2, 8:12], in0=st[:, 2, 8:12], scalar1=0.8, scalar2=None, op0=ALU.mult), reads=['st2'], writes=['st2'])
            v8 = lambda a: a.rearrange("p (g d) -> p g d", d=64)
            v4 = lambda a: a.rearrange("p (g d) -> p g d", d=128)
            S.op('dve', lambda e: e.tensor_tensor(out=v8(o0[:]), in0=v8(o0[:]), in1=bc(st[:, 2, 0:8], 2, 64), op=ALU.mult), reads=['o0', 'st2'], writes=['o0'])
            S.op('pool', lambda e: e.tensor_tensor(out=v8(o0[:]), in0=v8(o0[:]), in1=bc(gw[:], 1, 8), op=ALU.mult), reads=['o0', 'gw'], writes=['o0'])
            S.op('act', lambda e: e.activation(out=gt[:], in_=gt[:], func=AF.Silu), reads=['gt'], writes=['gt'])
            S.op('dve', lambda e: e.tensor_tensor(out=mix[:, 0:512], in0=o0[:], in1=gt[:], op=ALU.mult), reads=['o0', 'gt'], writes=['mix0'])
            S.op('dve', lambda e: e.tensor_tensor(out=v4(dl[:]), in0=v4(dl[:]), in1=bc(st[:, 2, 8:12], 2, 128), op=ALU.mult), reads=['dl', 'st2'], writes=['dl'])
            S.op('pool', lambda e: e.tensor_tensor(out=v4(mix[:, 512:1024]), in0=v4(dl[:]), in1=bc(sw[:], 1, 4), op=ALU.mult), reads=['dl', 'sw'], writes=['mix1'])
        else:
            S.dma('sp', mix[:], FM[rs, :], writes=['mix0', 'mix1'])
        for k in range(8):
            S.op('pe', lambda e: e.transpose(pT[:, k * 128:(k + 1) * 128], mix[:, k * 128:(k + 1) * 128], P.idf[:]),
                 reads=['mix%d' % (k // 4), 'ident'], writes=['pT%d' % (k // 4)])
        S.op('act', lambda e: e.activation(out=mixT[:, 0:512], in_=pT[:, 0:512], func=AF.Copy), reads=['pT0'], writes=['mixT0'])
        S.op('dve', lambda e: e.tensor_copy(out=mixT[:, 512:1024], in_=pT[:, 512:1024]), reads=['pT1'], writes=['mixT1'])
        for cb in range(2):
            for k in range(8):
                S.op('pe', lambda e: e.matmul(pY[:, cb * 512:(cb + 1) * 512], lhsT=mixT[:, k * 128:(k + 1) * 128], rhs=wt[:, k, cb * 512:(cb + 1) * 512],
                                              start=(k == 0), stop=(k == 7)), reads=['mixT%d' % (k // 4), 'wt%d' % k], writes=['pY%d' % cb])
        S.op('dve', lambda e: e.tensor_tensor(out=x1[:], in0=pY[:], in1=g1b[:], op=ALU.mult), reads=['pY0', 'pY1', 'g1b'], writes=['x1'])
        S.op('pool', lambda e: e.tensor_tensor(out=x1[:], in0=x1[:], in1=xt[:], op=ALU.add), reads=['x1', 'xt'], writes=['x1'])
        S.dma('pool', X1[rs, :], x1[:], reads=['x1'], writes=['X1'])
    return P


def run_l5a(kind, x, c, ada_w_i, ada_b_i, wout, **kw):
    P = build_l5a(kind)
    in_maps = []
    for core in range(NCORES):
        b, hf = core // 2, core % 2
        sl = slice(hf * TOK, (hf + 1) * TOK)
        m = dict(X=np.ascontiguousarray(x[b, sl]), wout=np.ascontiguousarray(wout), cT=cT_layout(c[b][None]),
                 adaw=np.ascontiguousarray(ada_w_i[:, 2048:3072]), adab=rep128(ada_b_i[2048:3072]))
        if kind == 'ab':
            m.update(O0=np.ascontiguousarray(kw['O'][b, 0, sl]), O1=np.ascontiguousarray(kw['O'][b, 1, sl]),
                     GATE=np.ascontiguousarray(kw['Plat'][b, sl, 1536:2048]), DL=np.ascontiguousarray(kw['dlat'][b, sl]),
                     gdnw=rep128(kw['gdn_norm_w']), sublnw=rep128(kw['subln_w']))
        else:
            m.update(FM=np.ascontiguousarray(kw['fm'][b, sl]))
        in_maps.append(m)
    res = P.run(in_maps)
    out = np.empty((4, 8192, D), np.float32)
    for core in range(NCORES):
        b, hf = core // 2, core % 2
        out[b, hf * TOK:(hf + 1) * TOK] = res[core]["X1"]
    return out


def build_l5b(tail, ntt=NTT):
    P = Prog()
    S = P.S
    tok = ntt * 128
    X1 = P.din("X1", [tok, D])
    cT = P.din("cT", [128, 1, 8]); adaw = P.din("adaw", [D, 3072]); adab = P.din("adab", [128, 3072]); n2w = P.din("n2w", [128, D])
    wq = P.din("wq", [D, 2048]); keysT = P.din("keysT", [128, 16, 128])
    Utab = P.din("Utab", [16384, D]); Vtab = P.din("Vtab", [16384, D])
    if tail == 'hnext':
        adawn = P.din("adawn", [D, 2048]); adabn = P.din("adabn", [128, 2048]); n1wn = P.din("n1wn", [128, D])
        Hn = P.dout("Hn", [tok, D])
    else:
        fnw = P.din("fnw", [128, D])
    Xo = P.dout("Xo", [tok, D])
    consts(P)
    csil = P.sb("csil", [128, 1, 8]); S.dma('sp', csil[:], cT, writes=['csil'])
    S.op('act', lambda e: e.activation(out=csil[:], in_=csil[:], func=AF.Silu), reads=['csil'], writes=['csil'])
    modb = P.sb("modb", [128, 3072])
    P.mod_bw = 3072
    pA = P.ps("pA", [128, D]); pQ = P.ps("pQ", [128, 2, 512]); pSc = P.ps("pSc", [128, 2048])
    pmx = ([pQ[:, 0, :], pQ[:, 1, :]], ['pQ0', 'pQ1'])
    emit_mod(P, csil, 0, adaw, adab, 0, 3072, modb, 'modb', P.ones, "m", pmx=pmx)
    nw = P.sb("nw", [128, D]); S.dma('sp', nw[:], n2w, writes=['nw'])
    wmod2 = P.sb("wmod2", [128, D])
    S.op('dve', lambda e: e.scalar_tensor_tensor(out=wmod2[:], in0=modb[:, 1024:2048], scalar=1.0, in1=nw[:], op0=ALU.add, op1=ALU.mult),
         reads=['modb', 'nw'], writes=['wmod2'])
    if tail == 'hnext':
        modn = P.sb("modn", [128, 2048])
        emit_mod(P, csil, 0, adawn, adabn, 0, 2048, modn, 'modn', P.ones, "m", pmx=pmx)
        S.dma('sp', nw[:], n1wn, reads=['wmod2'], writes=['nw'])
        wmodn = P.sb("wmodn", [128, D])
        S.op('dve', lambda e: e.scalar_tensor_tensor(out=wmodn[:], in0=modn[:, 1024:2048], scalar=1.0, in1=nw[:], op0=ALU.add, op1=ALU.mult),
             reads=['modn', 'nw'], writes=['wmodn'])
    else:
        S.dma('sp', nw[:], fnw, reads=['wmod2'], writes=['nw'])
    wqt = P.sb("wqt", [128, 8, 2048])
    wqv = wq.rearrange("(k p) n -> p k n", p=128)
    for k in range(8):
        S.dma('sp', wqt[:, k, :], wqv[:, k, :], writes=['wq%d' % k])
    kyt = P.sb("kyt", [128, 16, 128]); S.dma('sp', kyt[:], keysT, writes=['kyt'])
    zr = P.sb("zr", [128, 255]); S.op('pool', lambda g: g.memset(zr[:], 0.0), writes=['zr']); S.op('pool', lambda g: g.memset(zr[:, 127:128], 1.0), reads=['zr'], writes=['zr'])
    iot = P.sb("iot", [128, 16]); S.op('pool', lambda g: g.iota(iot[:], pattern=[[1, 16]], base=0, channel_multiplier=0, allow_small_or_imprecise_dtypes=True), writes=['iot'])
    xt = P.sb("xt", [128, D]); hn = P.sb("hn", [128, D]); hnT = P.sb("hnT", [128, D]); ss = P.sb("ss", [128, 4])
    qTs = P.sb("qTs", [128, 16, 128]); sc = P.sb("sc", [128, 2048]); wk = P.sb("wk", [128, 2048])
    m16 = P.sb("m16", [128, 16, 16]); i16 = P.sb("i16", [128, 16, 16], U32); i16f = P.sb("i16f", [128, 16, 16])
    c16 = P.sb("c16", [128, 8, 16]); p16 = P.sb("p16", [128, 8, 16], U32); pa = P.sb("pa", [128, 8, 16], U32); pb = P.sb("pb", [128, 8, 16], U32)
    paf = P.sb("paf", [128, 8, 16]); pbf = P.sb("pbf", [128, 8, 16]); sel = P.sb("sel", [128, 2, 128]); idxf = P.sb("idxf", [128, 128])
    gat = P.sb("gat", [128, 128]); gs = P.sb("gs", [128, 2, 8])
    idxTi = P.sb("idxTi", [128, 128], I32); gateT = P.sb("gateT", [128, 128]); actA = P.sb("actA", [128, 128]); Wm = P.sb("Wm", [128, 128])
    NG = 4
    Ug = [P.sb("Ug%d" % i, [128, D]) for i in range(NG)]
    Wz = [P.sb("Wz%d" % i, [128, 128]) for i in range(3)]
    pB = [pSc[:, 0:1024], pA[:, :]]
    pBk = [['pSc0', 'pSc1'], ['pA0', 'pA1']]
    pOut = pSc[:, 1024:2048]; pOk = ['pSc2', 'pSc3']
    cand = wk; junk = sc
    v4 = lambda a: a.rearrange("p (h a b) -> p h a b", a=16, b=16)
    for t in range(ntt):
        rs = slice(t * 128, (t + 1) * 128)
        S.dma('sp', xt[:], X1[rs, :], writes=['xt'])
        emit_rmsnorm_mod(P, xt, 'xt', wmod2, modb[:, 0:1024], ['wmod2', 'modb'], hn, 'hn', (hn, ss))
        for k in range(8):
            S.op('pe', lambda e: e.transpose(pA[:, k * 128:(k + 1) * 128], hn[:, k * 128:(k + 1) * 128], P.idf[:]), reads=['hn', 'ident'], writes=['pA%d' % (k // 4)])
        S.op('act', lambda e: e.activation(out=hnT[:, 0:512], in_=pA[:, 0:512], func=AF.Copy), reads=['pA0'], writes=['hnT0'])
        S.op('dve', lambda e: e.tensor_copy(out=hnT[:, 512:1024], in_=pA[:, 512:1024]), reads=['pA1'], writes=['hnT1'])
        for g in range(4):
            for j in range(4):
                hp = g * 4 + j
                for k in range(8):
                    S.op('pe', lambda e: e.matmul(pQ[:, g % 2, j * 128:(j + 1) * 128], lhsT=wqt[:, k, hp * 128:(hp + 1) * 128], rhs=hnT[:, k * 128:(k + 1) * 128],
                                                  start=(k == 0), stop=(k == 7)), reads=['wq%d' % k, 'hnT%d' % (k // 4)], writes=['pQ%d' % (g % 2)])
            eng = 'act' if g % 2 == 0 else 'dve'
            if eng == 'act':
                S.op('act', lambda e: e.activation(out=qTs[:, g * 4:(g + 1) * 4, :], in_=pQ[:, g % 2, :].rearrange("p (j n) -> p j n", n=128), func=AF.Copy), reads=['pQ%d' % (g % 2)], writes=['qTs%d' % g])
            else:
                S.op('dve', lambda e: e.tensor_copy(out=qTs[:, g * 4:(g + 1) * 4, :], in_=pQ[:, g % 2, :].rearrange("p (j n) -> p j n", n=128)), reads=['pQ%d' % (g % 2)], writes=['qTs%d' % g])
        for hp in range(16):
            S.op('pe', lambda e: e.matmul(pSc[:, hp * 128:(hp + 1) * 128], lhsT=qTs[:, hp, :], rhs=kyt[:, hp, :], start=True, stop=True),
                 reads=['qTs%d' % (hp // 4), 'kyt'], writes=['pSc%d' % (hp // 4)])
        for g in range(4):
            if g % 2 == 0:
                S.op('act', lambda e: e.activation(out=sc[:, g * 512:(g + 1) * 512], in_=pSc[:, g * 512:(g + 1) * 512], func=AF.Copy), reads=['pSc%d' % g], writes=['sc%d' % g])
            else:
                S.op('dve', lambda e: e.tensor_copy(out=sc[:, g * 512:(g + 1) * 512], in_=pSc[:, g * 512:(g + 1) * 512]), reads=['pSc%d' % g], writes=['sc%d' % g])
        for hp in range(16):
            blk = slice(hp * 128, (hp + 1) * 128)
            sk = 'sc%d' % (hp // 4)
            S.op('dve', lambda e: e.max(out=m16[:, hp, 0:8], in_=sc[:, blk]), reads=[sk], writes=['m16a'])
            S.op('dve', lambda e: e.match_replace(out=wk[:, blk], in_to_replace=m16[:, hp, 0:8], in_values=sc[:, blk], imm_value=-1e30), reads=[sk, 'm16a'], writes=['wk'])
            S.op('dve', lambda e: e.max(out=m16[:, hp, 8:16], in_=wk[:, blk]), reads=['wk'], writes=['m16b'])
            S.op('dve', lambda e: e.max_index(out=i16[:, hp, 0:8], in_max=m16[:, hp, 0:8], in_values=sc[:, blk]), reads=[sk, 'm16a'], writes=['i16'])
            S.op('dve', lambda e: e.max_index(out=i16[:, hp, 8:16], in_max=m16[:, hp, 8:16], in_values=sc[:, blk]), reads=[sk, 'm16b'], writes=['i16'])
        S.op('dve', lambda e: e.tensor_copy(out=i16f[:], in_=i16[:]), reads=['i16'], writes=['i16f'])
        m16v = m16[:].rearrange("p (h two) k -> p h two k", two=2)
        i16v = i16f[:].rearrange("p (h two) k -> p h two k", two=2)
        S.op('dve', lambda e: e.tensor_tensor(out=v4(cand[:]), in0=bc(m16v[:, :, 0, :], 3, 16), in1=bc(m16v[:, :, 1, :], 2, 16), op=ALU.add),
             reads=['m16a', 'm16b', 'wk'], writes=['cand'])
        for h in range(8):
            blk = slice(h * 256, (h + 1) * 256)
            S.op('dve', lambda e: e.max(out=c16[:, h, 0:8], in_=cand[:, blk]), reads=['cand'], writes=['c16a'])
            S.op('dve', lambda e: e.match_replace(out=junk[:, blk], in_to_replace=c16[:, h, 0:8], in_values=cand[:, blk], imm_value=-1e30), reads=['cand', 'c16a'] + ['sc%d' % i for i in range(4)], writes=['junk'])
            S.op('dve', lambda e: e.max(out=c16[:, h, 8:16], in_=junk[:, blk]), reads=['junk'], writes=['c16b'])
            S.op('dve', lambda e: e.max_index(out=p16[:, h, 0:8], in_max=c16[:, h, 0:8], in_values=cand[:, blk]), reads=['cand', 'c16a'], writes=['p16'])
            S.op('dve', lambda e: e.max_index(out=p16[:, h, 8:16], in_max=c16[:, h, 8:16], in_values=cand[:, blk]), reads=['cand', 'c16b'], writes=['p16'])
        S.op('dve', lambda e: e.tensor_single_scalar(out=pa[:], in_=p16[:], scalar=4, op=ALU.logical_shift_right), reads=['p16'], writes=['pa'])
        S.op('dve', lambda e: e.tensor_single_scalar(out=pb[:], in_=p16[:], scalar=15, op=ALU.bitwise_and), reads=['p16'], writes=['pb'])
        S.op('dve', lambda e: e.tensor_copy(out=paf[:], in_=pa[:]), reads=['pa'], writes=['paf'])
        S.op('dve', lambda e: e.tensor_copy(out=pbf[:], in_=pb[:]), reads=['pb'], writes=['pbf'])
        iob = bc(bc(iot[:], 1, 16), 1, 8)
        for w_, (pf, pk) in enumerate(((paf, 'paf'), (pbf, 'pbf'))):
            S.op('dve', lambda e: e.tensor_tensor(out=v4(junk[:]), in0=bc(pf[:], 3, 16), in1=iob, op=ALU.is_equal), reads=[pk, 'iot', 'junk'], writes=['junk'])
            S.op('dve', lambda e: e.tensor_tensor(out=v4(junk[:]), in0=v4(junk[:]), in1=bc(i16v[:, :, w_, :], 2, 16), op=ALU.mult), reads=['junk', 'i16f'], writes=['junk'])
            S.op('dve', lambda e: e.tensor_reduce(out=sel[:, w_, :], in_=junk[:].rearrange("p (x a) -> p x a", a=16), axis=AX.X, op=ALU.add), reads=['junk'], writes=['sel%d' % w_])
        S.op('dve', lambda e: e.scalar_tensor_tensor(out=idxf[:], in0=sel[:, 0, :], scalar=128.0, in1=sel[:, 1, :], op0=ALU.mult, op1=ALU.add), reads=['sel0', 'sel1'], writes=['idxf'])
        c16f = c16[:]
        S.op('dve', lambda e: e.tensor_tensor(out=gat[:].rearrange("p (h k) -> p h k", k=16), in0=c16f, in1=bc(c16[:, :, 0], 2, 16), op=ALU.subtract), reads=['c16a', 'c16b'], writes=['gat'])
        S.op('dve', lambda e: e.tensor_scalar(out=gat[:], in0=gat[:], scalar1=-80.0, scalar2=None, op0=ALU.max), reads=['gat'], writes=['gat'])
        S.op('act', lambda e: e.activation(out=gat[:], in_=gat[:], func=AF.Exp), reads=['gat'], writes=['gat'])
        S.op('dve', lambda e: e.tensor_reduce(out=gs[:, 0, :], in_=gat[:].rearrange("p (h k) -> p h k", k=16), axis=AX.X, op=ALU.add), reads=['gat'], writes=['gs0'])
        S.op('dve', lambda e: e.reciprocal(out=gs[:, 1, :], in_=gs[:, 0, :]), reads=['gs0'], writes=['gs1'])
        S.op('dve', lambda e: e.tensor_tensor(out=gat[:].rearrange("p (h k) -> p h k", k=16), in0=gat[:].rearrange("p (h k) -> p h k", k=16), in1=bc(gs[:, 1, :], 2, 16), op=ALU.mult), reads=['gat', 'gs1'], writes=['gat'])
        S.op('pe', lambda e: e.transpose(pQ[:, 0, 0:128], idxf[:], P.idf[:]), reads=['idxf', 'ident'], writes=['pQ0'])
        S.op('pe', lambda e: e.transpose(pQ[:, 1, 0:128], gat[:], P.idf[:]), reads=['gat', 'ident'], writes=['pQ1'])
        S.op('dve', lambda e: e.tensor_copy(out=idxTi[:], in_=pQ[:, 0, 0:128]), reads=['pQ0'], writes=['idxTi'])
        S.op('act', lambda e: e.activation(out=gateT[:], in_=pQ[:, 1, 0:128], func=AF.Copy), reads=['pQ1'], writes=['gateT'])
        for tk in range(128):
            ug = Ug[tk % NG]; uk = 'Ug%d' % (tk % NG)
            S.dmaf('pool', lambda g: g.indirect_dma_start(out=ug[:], out_offset=None, in_=Utab[:, :], in_offset=bass.IndirectOffsetOnAxis(ap=idxTi[:, tk:tk + 1], axis=0)),
                   reads=['idxTi'], writes=[uk])
            pb_ = pB[tk % 2]; pbk = pBk[tk % 2]
            for cb in range(2):
                S.op('pe', lambda e: e.matmul(pb_[:, cb * 512:(cb + 1) * 512], lhsT=bc(P.idf[:, tk], 1, 128), rhs=hn[:, cb * 512:(cb + 1) * 512], start=True, stop=True),
                     reads=['ident', 'hn'], writes=[pbk[cb]])
            S.op('dve', lambda e: e.scalar_tensor_tensor(out=ug[:], in0=ug[:], scalar=1.0, in1=pb_, op0=ALU.mult, op1=ALU.mult, accum_out=actA[:, tk:tk + 1]),
                 reads=[uk] + pbk, writes=[uk, 'actA'])
        S.op('act', lambda e: e.activation(out=Wm[:], in_=actA[:], func=AF.Gelu), reads=['actA'], writes=['Wm'])
        S.op('dve', lambda e: e.tensor_tensor(out=Wm[:], in0=Wm[:], in1=gateT[:], op=ALU.mult), reads=['Wm', 'gateT'], writes=['Wm'])
        for tk in range(128):
            vg = Ug[tk % NG]; vk = 'Ug%d' % (tk % NG)
            S.dmaf('pool', lambda g: g.indirect_dma_start(out=vg[:], out_offset=None, in_=Vtab[:, :], in_offset=bass.IndirectOffsetOnAxis(ap=idxTi[:, tk:tk + 1], axis=0)),
                   reads=['idxTi'], writes=[vk])
            wz = Wz[tk % 3]; wzk = 'Wz%d' % (tk % 3)
            S.op('act', lambda e: e.activation(out=wz[:], in_=zr[:, 127 - tk:255 - tk], func=AF.Copy, scale=Wm[:, tk:tk + 1]), reads=['zr', 'Wm'], writes=[wzk])
            for cb in range(2):
                S.op('pe', lambda e: e.matmul(pOut[:, cb * 512:(cb + 1) * 512], lhsT=wz[:], rhs=vg[:, cb * 512:(cb + 1) * 512], start=(tk == 0), stop=(tk == 127)),
                     reads=[wzk, vk], writes=[pOk[cb]])
        S.op('dve', lambda e: e.tensor_tensor(out=hn[:], in0=pOut, in1=modb[:, 2048:3072], op=ALU.mult), reads=pOk + ['modb', 'hn'], writes=['hn'])
        S.op('pool', lambda e: e.tensor_tensor(out=xt[:], in0=xt[:], in1=hn[:], op=ALU.add), reads=['xt', 'hn'], writes=['xt'])
        if tail == 'hnext':
            S.dma('sp', Xo[rs, :], xt[:], reads=['xt'], writes=['Xo'])
            emit_rmsnorm_mod(P, xt, 'xt', wmodn, modn[:, 0:1024], ['wmodn', 'modn'], hn, 'hn', (hn, ss))
            S.dma('sp', Hn[rs, :], hn[:], reads=['hn'], writes=['Hn'])
        else:
            emit_rmsnorm_mod(P, xt, 'xt', nw, None, ['nw'], hn, 'hn', (hn, ss))
            S.dma('sp', Xo[rs, :], hn[:], reads=['hn'], writes=['Xo'])
    return P


def run_l5b(tail, x1, c, ada_w_i, ada_b_i, norm2_w_i, wq, keys, utab, vtab, ntt=NTT, ncores=NCORES, **kw):
    P = build_l5b(tail, ntt)
    tok = ntt * 128
    keysT = np.ascontiguousarray(keys.reshape(16, 128, 128).transpose(2, 0, 1))
    in_maps = []
    for core in range(ncores):
        b, hf = core // 2, core % 2
        sl = slice(hf * TOK, hf * TOK + tok)
        m = dict(X1=np.ascontiguousarray(x1[b, sl]), cT=cT_layout(c[b][None]), adaw=np.ascontiguousarray(ada_w_i[:, 3072:6144]), adab=rep128(ada_b_i[3072:6144]),
                 n2w=rep128(norm2_w_i), wq=np.ascontiguousarray(wq), keysT=keysT, Utab=np.ascontiguousarray(utab), Vtab=np.ascontiguousarray(vtab))
        if tail == 'hnext':
            m.update(adawn=np.ascontiguousarray(kw['ada_w_n'][:, :2048]), adabn=rep128(kw['ada_b_n'][:2048]), n1wn=rep128(kw['norm1_w_n']))
        else:
            m.update(fnw=rep128(kw['final_norm_w']))
        in_maps.append(m)
    res = P.run(in_maps)
    xo = np.zeros((4, 8192, D), np.float32)
    hn = np.zeros((4, 8192, D), np.float32) if tail == 'hnext' else None
    for core in range(ncores):
        b, hf = core // 2, core % 2
        xo[b, hf * TOK:hf * TOK + tok] = res[core]["Xo"]
        if hn is not None:
            hn[b, hf * TOK:hf * TOK + tok] = res[core]["Hn"]
    return xo, hn


def build_l6():
    P = Prog()
    S = P.S
    nc = P.nc
    HT = P.din("HT", [4, 128, 8192])
    CS = P.din("CS", [2, 128, 512])
    W64 = P.din("W64", [128, 128])
    WB = P.din("WB", [128, 64, 2, 128])
    FM = P.dout("FM", [8192, 512])
    Zs = [nc.dram_tensor("Zs%d" % g, [8192, 512], F32, kind="Internal").ap() for g in range(2)]
    Us = [nc.dram_tensor("Us%d" % g, [128, 128, 256], F32, kind="Internal").ap() for g in range(2)]
    cs = P.sb("cs", [128, 2, 512]); S.dma('sp', cs[:], CS.rearrange("h p n -> p h n"), writes=['cs'])
    w64 = P.sb("w64", [128, 128]); S.dma('sp', w64[:], W64, writes=['w64'])
    pz = [P.ps("pz%d" % i, [128, 512]) for i in range(2)]
    pu = P.ps("pu", [128, 2048])
    py = [P.ps("py%d" % i, [128, 256]) for i in range(2)]
    TB = 2048
    hts = [P.sb("ht%d" % i, [128, 4, TB]) for i in range(2)]
    zts = [P.sb("zt%d" % i, [128, 512]) for i in range(2)]
    it = 0
    for tb in range(8192 // TB):
        ht = hts[tb % 2]; hk = 'ht%d' % (tb % 2)
        S.dma('sp', ht[:], HT[:, :, tb * TB:(tb + 1) * TB].rearrange("c p t -> p c t"), writes=[hk])
        for tt in range(TB // 128):
            for g in range(2):
                p_ = pz[it % 2]; pk = 'pz%d' % (it % 2); zt = zts[it % 2]; zk = 'zt%d' % (it % 2)
                it += 1
                for hf in range(2):
                    S.op('pe', lambda e: e.matmul(p_[:], lhsT=ht[:, 2 * g + hf, tt * 128:(tt + 1) * 128], rhs=cs[:, hf, :], start=(hf == 0), stop=(hf == 1)),
                         reads=[hk, 'cs'], writes=[pk])
                S.op('act' if g == 0 else 'dve', (lambda e: e.activation(out=zt[:], in_=p_[:], func=AF.Copy)) if g == 0 else (lambda e: e.tensor_copy(out=zt[:], in_=p_[:])),
                     reads=[pk], writes=[zk])
                r0 = tb * TB + tt * 128
                S.dma('pool', Zs[g][r0:r0 + 128, :], zt[:], reads=[zk], writes=['Zs%d' % g])
    LB = 8
    zin = [P.sb("zin%d" % i, [128, LB, 256]) for i in range(2)]
    uts = [P.sb("ut%d" % i, [128, LB, 256]) for i in range(2)]
    it = 0
    for g in range(2):
        zv = Zs[g].rearrange("(l1 l2) (ri kc) -> ri l1 l2 kc", l2=128, ri=2)
        uv = Us[g].rearrange("l2 m kc -> m l2 kc")
        for lb in range(128 // LB):
            zi = zin[it % 2]; zk = 'zin%d' % (it % 2); ut = uts[it % 2]; uk = 'ut%d' % (it % 2)
            it += 1
            for ri in range(2):
                S.dma('sp', zi[ri * 64:(ri + 1) * 64, :, :], zv[ri][:, lb * LB:(lb + 1) * LB, :], reads=['Zs%d' % g], writes=[zk + '_%d' % ri])
            for j in range(LB // 2):
                S.op('pe', lambda e: e.matmul(pu[:, j * 512:(j + 1) * 512], lhsT=w64[:], rhs=zi[:, 2 * j:2 * j + 2, :].rearrange("p a b -> p (a b)"), start=True, stop=True),
                     reads=['w64', zk + '_0', zk + '_1'], writes=['pu'])
            S.op('act' if lb % 2 == 0 else 'dve', (lambda e: e.activation(out=ut[:].rearrange("p a b -> p (a b)"), in_=pu[:], func=AF.Copy)) if lb % 2 == 0 else
                 (lambda e: e.tensor_copy(out=ut[:].rearrange("p a b -> p (a b)"), in_=pu[:])), reads=['pu'], writes=[uk])
            S.dma('pool', uv[:, lb * LB:(lb + 1) * LB, :], ut[:], reads=[uk], writes=['Us%d' % g])
    KB = 16
    wbs = [P.sb("wb%d" % i, [128, KB, 2, 128]) for i in range(2)]
    urs = [P.sb("ur%d" % i, [128, 2, KB, 256]) for i in range(2)]
    yts = [P.sb("yt%d" % i, [128, 256]) for i in range(2)]
    fv = FM.rearrange("(k2 k1) c -> k1 k2 c", k1=64)
    it = 0; ib = 0
    for g in range(2):
        for kb in range(64 // KB):
            wb = wbs[ib % 2]; wk_ = 'wb%d' % (ib % 2); ur = urs[ib % 2]; urk = 'ur%d' % (ib % 2)
            ib += 1
            S.dma('sp', wb[:], WB[:, kb * KB:(kb + 1) * KB, :, :], writes=[wk_])
            for ri in range(2):
                S.dma('sp', ur[:, ri, :, :], Us[g][:, ri * 64 + kb * KB:ri * 64 + (kb + 1) * KB, :], reads=['Us%d' % g], writes=[urk + '_%d' % ri])
            for kk in range(KB):
                k1 = kb * KB + kk
                p_ = py[it % 2]; pk = 'py%d' % (it % 2); yt = yts[it % 2]; yk = 'yt%d' % (it % 2)
                it += 1
                for ri in range(2):
                    S.op('pe', lambda e: e.matmul(p_[:], lhsT=wb[:, kk, ri, :], rhs=ur[:, ri, kk, :], start=(ri == 0), stop=(ri == 1)),
                         reads=[wk_, urk + '_0', urk + '_1'], writes=[pk])
                S.op('act' if it % 2 == 0 else 'dve', (lambda e: e.activation(out=yt[:], in_=p_[:], func=AF.Copy)) if it % 2 == 0 else (lambda e: e.tensor_copy(out=yt[:], in_=p_[:])),
                     reads=[pk], writes=[yk])
                S.dma('pool', fv[k1][:, g * 256:(g + 1) * 256], yt[:], reads=[yk], writes=['FM'])
    return P


def l6_consts():
    sc = 1.0 / math.sqrt(8192.0 * 256.0)
    ch = np.arange(256, dtype=np.float64)
    th = 2 * np.pi * np.outer(ch, ch) / 256.0
    CS = np.concatenate([np.cos(th), np.sin(th)], axis=1) * sc
    CS = CS.reshape(2, 128, 512).astype(np.float32)
    l1 = np.arange(64, dtype=np.float64)
    t64 = 2 * np.pi * np.outer(l1, l1) / 64.0
    c, s = np.cos(t64), np.sin(t64)
    W64 = np.block([[c, -s], [-s, -c]]).astype(np.float32)
    l2 = np.arange(128, dtype=np.float64)[:, None, None]
    k1 = np.arange(64, dtype=np.float64)[None, :, None]
    k2 = np.arange(128, dtype=np.float64)[None, None, :]
    thb = 2 * np.pi * (l2 * k2 / 128.0 + l2 * k1 / 8192.0)
    WB = np.stack([np.cos(thb), np.sin(thb)], axis=2).astype(np.float32)
    return CS, W64, np.ascontiguousarray(WB)


def run_l6(h1):
    P = build_l6()
    CS, W64, WB = l6_consts()
    in_maps = []
    for core in range(NCORES):
        b, gp = core // 2, core % 2
        ht = h1[b, :, gp * 512:(gp + 1) * 512].T.reshape(4, 128, 8192)
        in_maps.append(dict(HT=np.ascontiguousarray(ht), CS=CS, W64=W64, WB=WB))
    res = P.run(in_maps)
    out = np.empty((4, 8192, D), np.float32)
    for core in range(NCORES):
        b, gp = core // 2, core % 2
        out[b, :, gp * 512:(gp + 1) * 512] = res[core]["FM"]
    return out


def kernel(x, c, ctx, c_ctx, ada_w, ada_b, norm1_w, norm2_w, w_in, conv_w, a_log, dt_bias, gdn_norm_w,
           lam_q1, lam_k1, lam_q2, lam_k2, subln_w, w_out_ab, w_out_f, peer_wq, peer_keys, peer_u, peer_v, final_norm_w):
    f = lambda a: np.asarray(a, dtype=np.float32)
    x, c, ctx, c_ctx, ada_w, ada_b, norm1_w, norm2_w, w_in, conv_w, a_log, dt_bias, gdn_norm_w = map(
        f, (x, c, ctx, c_ctx, ada_w, ada_b, norm1_w, norm2_w, w_in, conv_w, a_log, dt_bias, gdn_norm_w))
    lam_q1, lam_k1, lam_q2, lam_k2, subln_w, w_out_ab, w_out_f, peer_wq, peer_keys, peer_u, peer_v, final_norm_w = map(
        f, (lam_q1, lam_k1, lam_q2, lam_k2, subln_w, w_out_ab, w_out_f, peer_wq, peer_keys, peer_u, peer_v, final_norm_w))
    Plat, Pctx = run_l1(x, c, ctx, c_ctx, ada_w[0], ada_b[0], norm1_w[0], w_in[0])
    o2 = run_l2(Plat, Pctx, conv_w[0], a_log[0], dt_bias[0])
    O = run_l3(o2['QKV'], o2['BG'])
    dlat = run_l4(o2['QKr'], Plat, Pctx, lam_q1[0], lam_k1[0], lam_q2[0], lam_k2[0])
    del o2
    x1 = run_l5a('ab', x, c, ada_w[0], ada_b[0], w_out_ab[0], O=O, Plat=Plat, dlat=dlat, gdn_norm_w=gdn_norm_w[0], subln_w=subln_w[0])
    del O, dlat, Plat, Pctx
    x2, h1 = run_l5b('hnext', x1, c, ada_w[0], ada_b[0], norm2_w[0], peer_wq[0], peer_keys[0], peer_u[0], peer_v[0],
                     ada_w_n=ada_w[1], ada_b_n=ada_b[1], norm1_w_n=norm1_w[1])
    del x1
    fm = run_l6(h1)
    x3 = run_l5a('f', x2, c, ada_w[1], ada_b[1], w_out_f[0], fm=fm)
    del x2, fm, h1
    zw = np.zeros((D, 2048), np.float32)
    zb = np.zeros((2048,), np.float32)
    _, out = run_l5b('hnext', x3, c, ada_w[1], ada_b[1], norm2_w[1], peer_wq[1], peer_keys[1], peer_u[1], peer_v[1],
                     ada_w_n=zw, ada_b_n=zb, norm1_w_n=final_norm_w)
    return out.astype(np.float32)
```

```python
import math
import numpy as np
import concourse.bass as bass
import concourse.mybir as mybir
from concourse.bass_utils import run_bass_kernel_spmd

F32 = mybir.dt.float32
I32 = mybir.dt.int32
U32 = mybir.dt.uint32
AF = mybir.ActivationFunctionType
ALU = mybir.AluOpType
AX = mybir.AxisListType

NCORES = 8
D = 1024
EPS = 1e-6


class Sched:
    LIMIT = 20000

    def __init__(self, nc):
        self.nc = nc
        self.eng = {'pe': nc.tensor, 'act': nc.scalar, 'dve': nc.vector, 'pool': nc.gpsimd, 'sp': nc.sync}
        self.epoch = {k: 0 for k in self.eng}
        self.sem = {(k, 0): nc.alloc_semaphore('s_%s_0' % k) for k in self.eng}
        self.cnt = {k: 0 for k in self.eng}
        self.seen = {k: {} for k in self.eng}
        self.ndsem = 24
        self.dsem = [nc.alloc_semaphore('d_%d' % i) for i in range(self.ndsem)]
        self.dcnt = [0] * self.ndsem
        self.dnext = 0
        self.lastw = {}
        self.readers = {}
        self.dwr = {}
        self.ninst = 0

    def _wait(self, e, tok, kindw):
        kind, key, val = tok
        if kind == 'e':
            src = key[0]
            if src == e and (e == 'pe' or kindw != 'raw'):
                return
        seen = self.seen[e]
        k = (kind, key)
        if seen.get(k, 0) >= val:
            return
        sem = self.sem[key] if kind == 'e' else self.dsem[key]
        self.eng[e].wait_ge(sem, val)
        seen[k] = val

    def _deps(self, e, reads, writes):
        for b in reads:
            t = self.lastw.get(b)
            if t is not None:
                self._wait(e, t, 'raw')
            for i, v in self.dwr.get(b, {}).items():
                self._wait(e, ('d', i, v), 'raw')
        for b in writes:
            t = self.lastw.get(b)
            if t is not None:
                self._wait(e, t, 'waw')
            for t in self.readers.get(b, ()):
                self._wait(e, t, 'war')

    def _commit(self, tok, reads, writes):
        for b in writes:
            self.lastw[b] = tok
            self.readers[b] = []
            if tok[0] == 'd':
                self.dwr.setdefault(b, {})[tok[1]] = tok[2]
            else:
                self.dwr.pop(b, None)
        for b in reads:
            if b in writes:
                continue
            self.readers.setdefault(b, []).append(tok)

    def op(self, e, fn, reads=(), writes=()):
        self._deps(e, reads, writes)
        ins = fn(self.eng[e])
        if self.cnt[e] >= self.LIMIT:
            self.epoch[e] += 1
            self.cnt[e] = 0
            self.sem[(e, self.epoch[e])] = self.nc.alloc_semaphore('s_%s_%d' % (e, self.epoch[e]))
        self.cnt[e] += 1
        key = (e, self.epoch[e])
        ins.then_inc(self.sem[key], 1)
        tok = ('e', key, self.cnt[e])
        self._commit(tok, reads, writes)
        self.ninst += 1
        return tok

    def dma(self, e, out, in_, reads=(), writes=(), **kw):
        return self.dmaf(e, lambda g: g.dma_start(out=out, in_=in_, **kw), reads, writes)

    def dmaf(self, e, fn, reads=(), writes=()):
        i = self.dnext
        self.dnext = (self.dnext + 1) % self.ndsem
        if self.dcnt[i] > 0:
            self._wait(e, ('d', i, self.dcnt[i]), 'raw')
        self._deps(e, reads, writes)
        ins = fn(self.eng[e])
        self.dcnt[i] += 16
        ins.then_inc(self.dsem[i], 16)
        tok = ('d', i, self.dcnt[i])
        self._commit(tok, reads, writes)
        self.ninst += 1
        return tok

    def finish(self, bufs, e='sp'):
        for b in bufs:
            t = self.lastw.get(b)
            if t is not None:
                self._wait(e, t, 'raw')
        for i in range(self.ndsem):
            if self.dcnt[i] > 0:
                self._wait(e, ('d', i, self.dcnt[i]), 'raw')


class Prog:
    def __init__(self):
        self.nc = bass.Bass("TRN2", target_bir_lowering=False)
        self.S = Sched(self.nc)
        self.outs = []
        self._n = 0

    def din(self, name, shape, dt=F32):
        return self.nc.dram_tensor(name, list(shape), dt, kind="ExternalInput").ap()

    def dout(self, name, shape, dt=F32):
        self.outs.append(name)
        return self.nc.dram_tensor(name, list(shape), dt, kind="ExternalOutput").ap()

    def sb(self, name, shape, dt=F32):
        return self.nc.alloc_sbuf_tensor(name, list(shape), dt)

    def ps(self, name, shape):
        return self.nc.alloc_psum_tensor(name, list(shape), F32)

    def ident(self):
        idf = self.sb("ident", [128, 128])
        S = self.S
        S.op('pool', lambda g: g.memset(idf[:], 1.0), writes=['ident'])
        S.op('pool', lambda g: g.affine_select(out=idf[:], in_=idf[:], pattern=[[-1, 128]], compare_op=ALU.is_equal,
                                               fill=0.0, base=0, channel_multiplier=1), reads=['ident'], writes=['ident'])
        return idf

    def run(self, in_maps):
        self.S.finish(self.outs)
        res = run_bass_kernel_spmd(self.nc, in_maps, core_ids=list(range(len(in_maps))))
        return res.results


def emit_mod(P, csil, v, adaw, adab, col0, ncols, outt, key, ones, tmpname, BW=256, pmx=None):
    S = P.S
    if not hasattr(P, '_modtmp'):
        P._modtmp = {}
    if tmpname not in P._modtmp:
        P._modtmp[tmpname] = (P.sb(tmpname + "_lt", [128, 8, 128]), [P.sb(tmpname + "_w%d" % i, [128, 8, BW]) for i in range(2)],
                              [P.ps(tmpname + "_pm%d" % i, [128, BW]) for i in range(2)] if pmx is None else None, P.sb(tmpname + "_b", [128, getattr(P, "mod_bw", 2048)]))
    lt, wts, pm, bt = P._modtmp[tmpname]
    pmk = [tmpname + '_pm0', tmpname + '_pm1']
    if pmx is not None:
        pm, pmk = pmx
    for k in range(8):
        S.op('dve', lambda e: e.tensor_scalar(out=lt[:, k, :], in0=ones[:, 0:128], scalar1=csil[:, v, k:k + 1], scalar2=None,
                                              op0=ALU.mult), reads=['csil', 'ones'], writes=[tmpname + '_lt%d' % k])
    wv = adaw.rearrange("(k p) n -> p k n", p=128)
    nb = (ncols + BW - 1) // BW
    S.dma('sp', bt[:, 0:ncols], adab[:, col0:col0 + ncols], writes=[tmpname + '_b'])
    for j in range(nb):
        c0 = col0 + j * BW
        w = min(BW, col0 + ncols - c0)
        wt = wts[j % 2]
        wk = tmpname + '_w%d' % (j % 2)
        S.dma('sp', wt[:, :, 0:w], wv[:, :, c0:c0 + w], writes=[wk])
        pk = pmk[j % 2]
        for k in range(8):
            S.op('pe', lambda e: e.matmul(pm[j % 2][:, 0:w], lhsT=lt[:, k, :], rhs=wt[:, k, 0:w], start=(k == 0), stop=(k == 7)),
                 reads=[wk, tmpname + '_lt%d' % k], writes=[pk])
        S.op('dve', lambda e: e.tensor_tensor(out=outt[:, j * BW:j * BW + w], in0=pm[j % 2][:, 0:w], in1=bt[:, j * BW:j * BW + w],
                                              op=ALU.add), reads=[pk, tmpname + '_b'], writes=[key])


def emit_rmsnorm_mod(P, xt, xkey, wmod, shb, modkeys, outt, okey, tmp):
    S = P.S
    junk, ss = tmp
    S.op('act', lambda e: e.activation(out=junk[:], in_=xt[:], func=AF.Square, accum_out=ss[:, 0:1]), reads=[xkey], writes=[okey, 'ss'])
    S.op('act', lambda e: e.activation(out=ss[:, 1:2], in_=ss[:, 0:1], func=AF.Sqrt, bias=P.epsb[:, 0:1], scale=1.0 / D), reads=['ss', 'epsb'], writes=['ss1'])
    S.op('dve', lambda e: e.reciprocal(out=ss[:, 2:3], in_=ss[:, 1:2]), reads=['ss1'], writes=['ss2'])
    S.op('dve', lambda e: e.scalar_tensor_tensor(out=outt[:], in0=xt[:], scalar=ss[:, 2:3], in1=wmod[:], op0=ALU.mult, op1=ALU.mult),
         reads=[xkey, 'ss2'] + modkeys, writes=[okey])
    if shb is not None:
        S.op('pool', lambda e: e.tensor_tensor(out=outt[:], in0=outt[:], in1=shb[:], op=ALU.add), reads=[okey] + modkeys, writes=[okey])


def consts(P):
    S = P.S
    P.ones = P.sb("ones", [128, 512])
    S.op('pool', lambda g: g.memset(P.ones[:], 1.0), writes=['ones'])
    P.epsb = P.sb("epsb", [128, 1])
    S.op('pool', lambda g: g.memset(P.epsb[:], EPS), writes=['epsb'])
    P.idf = P.ident()


L1_TILES = 33
IN_W = 3616


def build_l1():
    P = Prog()
    S = P.S
    X = P.din("X", [L1_TILES * 128, D])
    cT = P.din("cT", [128, 2, 8])
    adaw = P.din("adaw", [D, 2048])
    adab = P.din("adab", [128, 2048])
    n1w = P.din("n1w", [128, D])
    win = P.din("win", [D, IN_W])
    Pout = P.dout("P", [L1_TILES * 128, IN_W])
    consts(P)
    csil = P.sb("csil", [128, 2, 8])
    S.dma('sp', csil[:], cT, writes=['csil'])
    S.op('act', lambda e: e.activation(out=csil[:], in_=csil[:], func=AF.Silu), reads=['csil'], writes=['csil'])
    n1wt = P.sb("n1wt", [128, D])
    S.dma('sp', n1wt[:], n1w, writes=['n1w'])
    wint = P.sb("wint", [128, 8, IN_W])
    winv = win.rearrange("(k p) n -> p k n", p=128)
    for k in range(8):
        S.dma('sp', wint[:, k, :], winv[:, k, :], writes=['win%d' % k])
    mods = []
    for v in range(2):
        mb = P.sb("modb%d" % v, [128, 2048])
        emit_mod(P, csil, v, adaw, adab, 0, 2048, mb, 'modb%d' % v, P.ones, "m")
        wm = P.sb("wmod%d" % v, [128, D])
        S.op('dve', lambda e: e.scalar_tensor_tensor(out=wm[:], in0=mb[:, 1024:2048], scalar=1.0, in1=n1wt[:], op0=ALU.add, op1=ALU.mult),
             reads=['modb%d' % v, 'n1w'], writes=['wmod%d' % v])
        mods.append((wm, mb))
    xts = [P.sb("xt%d" % i, [128, D]) for i in range(2)]
    ss = P.sb("ss", [128, 4])
    ht = P.sb("ht", [128, D])
    junk = ht
    hT = P.sb("hT", [128, D])
    pT = P.ps("pT", [128, D])
    pp = [P.ps("pp%d" % i, [128, 512]) for i in range(4)]
    pts = [P.sb("pt0", [128, IN_W])] * 2
    for t in range(L1_TILES):
        v = 1 if t == L1_TILES - 1 else 0
        xt = xts[t % 2]
        xk = 'xt%d' % (t % 2)
        S.dma('sp', xt[:], X[t * 128:(t + 1) * 128, :], writes=[xk])
        wm, mb = mods[v]
        emit_rmsnorm_mod(P, xt, xk, wm, mb[:, 0:1024], ['wmod%d' % v, 'modb%d' % v], ht, 'ht', (junk, ss))
        for k in range(8):
            S.op('pe', lambda e: e.transpose(pT[:, k * 128:(k + 1) * 128], ht[:, k * 128:(k + 1) * 128], P.idf[:]),
                 reads=['ht', 'ident'], writes=['pT%d' % k])
        S.op('act', lambda e: e.activation(out=hT[:, 0:512], in_=pT[:, 0:512], func=AF.Copy), reads=['pT%d' % k for k in range(4)], writes=['hT0'])
        S.op('dve', lambda e: e.tensor_copy(out=hT[:, 512:1024], in_=pT[:, 512:1024]), reads=['pT%d' % k for k in range(4, 8)], writes=['hT1'])
        pt = pts[t % 2]
        ptk = 'pt0'
        ncb = (IN_W + 511) // 512
        for cb in range(ncb):
            c0 = cb * 512
            w = min(512, IN_W - c0)
            pq = pp[cb % 4]
            for k in range(8):
                S.op('pe', lambda e: e.matmul(pq[:, 0:w], lhsT=hT[:, k * 128:(k + 1) * 128], rhs=wint[:, k, c0:c0 + w], start=(k == 0), stop=(k == 7)),
                     reads=['hT%d' % (k // 4), 'win%d' % k], writes=['pp%d' % (cb % 4)])
            if cb % 2 == 0:
                S.op('act', lambda e: e.activation(out=pt[:, c0:c0 + w], in_=pq[:, 0:w], func=AF.Copy), reads=['pp%d' % (cb % 4)], writes=[ptk + '_%d' % cb])
            else:
                S.op('dve', lambda e: e.tensor_copy(out=pt[:, c0:c0 + w], in_=pq[:, 0:w]), reads=['pp%d' % (cb % 4)], writes=[ptk + '_%d' % cb])
        S.dma('pool', Pout[t * 128:(t + 1) * 128, :], pt[:], reads=[ptk + '_%d' % cb for cb in range(ncb)], writes=['P'])
    return P


def rep128(v):
    v = np.asarray(v, np.float32).reshape(1, -1)
    return np.ascontiguousarray(np.broadcast_to(v, (128, v.shape[1])))


def cT_layout(vecs):
    vecs = np.asarray(vecs, np.float32)
    return np.ascontiguousarray(vecs.reshape(vecs.shape[0], 8, 128).transpose(2, 0, 1))


def run_l1(x, c, ctx, c_ctx, ada_w0, ada_b0, norm1_w0, w_in0):
    P = build_l1()
    in_maps = []
    for core in range(NCORES):
        b, hf = core // 2, core % 2
        X = np.concatenate([x[b, hf * 4096:(hf + 1) * 4096], ctx[b, hf * 128:(hf + 1) * 128]], axis=0)
        in_maps.append(dict(X=np.ascontiguousarray(X), cT=cT_layout(np.stack([c[b], c_ctx])),
                            adaw=np.ascontiguousarray(ada_w0[:, :2048]), adab=rep128(ada_b0[:2048]),
                            n1w=rep128(norm1_w0), win=np.ascontiguousarray(w_in0)))
    res = P.run(in_maps)
    Plat = np.empty((4, 8192, IN_W), np.float32)
    Pctx = np.empty((4, 256, IN_W), np.float32)
    for core in range(NCORES):
        b, hf = core // 2, core % 2
        r = res[core]["P"]
        Plat[b, hf * 4096:(hf + 1) * 4096] = r[:4096]
        Pctx[b, hf * 128:(hf + 1) * 128] = r[4096:]
    return Plat, Pctx


def bc(ap, axis, n):
    a = ap.unsqueeze(axis)
    shp = list(a.shape)
    shp[axis] = n
    return a.broadcast_to(shp)


def build_l2():
    P = Prog()
    S = P.S
    NT = L1_TILES
    R = NT * 128
    Pp = P.din("Pp", [R, 1536]); Pc = P.din("Pc", [R, 1536]); Pn = P.din("Pn", [R, 1536])
    Pab = P.din("Pab", [R, 32]); Pqk = P.din("Pqk", [R, 1024])
    cosT = P.din("cosT", [R, 32]); sinT = P.din("sinT", [R, 32])
    convw = P.din("convw", [128, 3, 1536]); alog = P.din("alog", [128, 16]); dtb = P.din("dtb", [128, 16])
    QKV = P.dout("QKV", [R, 1536]); BG = P.dout("BG", [R, 32]); QKr = P.dout("QKr", [R, 1024])
    consts(P)
    cw = P.sb("cw", [128, 3, 1536]); S.dma('sp', cw[:], convw, writes=['cw'])
    negA = P.sb("negA", [128, 16]); S.dma('sp', negA[:], alog, writes=['negA'])
    S.op('act', lambda e: e.activation(out=negA[:], in_=negA[:], func=AF.Exp), reads=['negA'], writes=['negA'])
    S.op('dve', lambda e: e.tensor_scalar(out=negA[:], in0=negA[:], scalar1=-1.0, scalar2=None, op0=ALU.mult), reads=['negA'], writes=['negA'])
    dtbt = P.sb("dtbt", [128, 16]); S.dma('sp', dtbt[:], dtb, writes=['dtbt'])
    a0 = P.sb("a0", [128, 1536]); a1 = P.sb("a1", [128, 1536]); a2 = P.sb("a2", [128, 1536])
    qkv = P.sb("qkv", [128, 1536]); sq = P.sb("sq", [128, 1024]); st = P.sb("st", [128, 3, 16])
    ab = P.sb("ab", [128, 32]); bg = P.sb("bg", [128, 32]); tt = P.sb("tt", [128, 16])
    qk = P.sb("qk", [128, 1024]); qo = P.sb("qo", [128, 1024]); cs = P.sb("cs", [128, 2, 32])
    r0 = P.sb("r0", [128, 512]); r1 = P.sb("r1", [128, 512])
    for t in range(NT):
        rs = slice(t * 128, (t + 1) * 128)
        S.dma('sp', a0[:], Pp[rs, :], writes=['a0']); S.dma('sp', a1[:], Pc[rs, :], writes=['a1']); S.dma('sp', a2[:], Pn[rs, :], writes=['a2'])
        S.dma('sp', ab[:], Pab[rs, :], writes=['ab']); S.dma('sp', qk[:], Pqk[rs, :], writes=['qk'])
        S.dma('sp', cs[:, 0, :], cosT[rs, :], writes=['cs0']); S.dma('sp', cs[:, 1, :], sinT[rs, :], writes=['cs1'])
        S.op('dve', lambda e: e.tensor_tensor(out=a0[:], in0=a0[:], in1=cw[:, 0, :], op=ALU.mult), reads=['a0', 'cw'], writes=['a0'])
        S.op('pool', lambda e: e.tensor_tensor(out=a1[:], in0=a1[:], in1=cw[:, 1, :], op=ALU.mult), reads=['a1', 'cw'], writes=['a1'])
        S.op('dve', lambda e: e.tensor_tensor(out=a2[:], in0=a2[:], in1=cw[:, 2, :], op=ALU.mult), reads=['a2', 'cw'], writes=['a2'])
        S.op('pool', lambda e: e.tensor_tensor(out=a1[:], in0=a1[:], in1=a0[:], op=ALU.add), reads=['a1', 'a0'], writes=['a1'])
        S.op('dve', lambda e: e.tensor_tensor(out=a1[:], in0=a1[:], in1=a2[:], op=ALU.add), reads=['a1', 'a2'], writes=['a1'])
        S.op('act', lambda e: e.activation(out=qkv[:], in_=a1[:], func=AF.Silu), reads=['a1'], writes=['qkv'])
        S.op('act', lambda e: e.activation(out=sq[:], in_=qkv[:, 0:1024], func=AF.Square), reads=['qkv'], writes=['sq'])
        S.op('dve', lambda e: e.tensor_reduce(out=st[:, 0, :], in_=sq[:].rearrange("p (g d) -> p g d", d=64), axis=AX.X, op=ALU.add), reads=['sq'], writes=['st0'])
        S.op('act', lambda e: e.activation(out=st[:, 1, :], in_=st[:, 0, :], func=AF.Sqrt, bias=P.epsb[:, 0:1], scale=1.0), reads=['st0', 'epsb'], writes=['st1'])
        S.op('dve', lambda e: e.reciprocal(out=st[:, 2, :], in_=st[:, 1, :]), reads=['st1'], writes=['st2'])
        S.op('dve', lambda e: e.tensor_scalar(out=st[:, 2, 0:8], in0=st[:, 2, 0:8], scalar1=0.125, scalar2=None, op0=ALU.mult), reads=['st2'], writes=['st2'])
        S.op('dve', lambda e: e.tensor_tensor(out=qkv[:, 0:1024].rearrange("p (g d) -> p g d", d=64), in0=qkv[:, 0:1024].rearrange("p (g d) -> p g d", d=64),
                                              in1=bc(st[:, 2, :], 2, 64), op=ALU.mult), reads=['qkv', 'st2'], writes=['qkv'])
        S.dma('pool', QKV[rs, :], qkv[:], reads=['qkv'], writes=['QKV'])
        S.op('act', lambda e: e.activation(out=bg[:, 0:16], in_=ab[:, 0:16], func=AF.Sigmoid), reads=['ab'], writes=['bg0'])
        S.op('dve', lambda e: e.tensor_tensor(out=tt[:], in0=ab[:, 16:32], in1=dtbt[:], op=ALU.add), reads=['ab', 'dtbt'], writes=['tt'])
        S.op('act', lambda e: e.activation(out=tt[:], in_=tt[:], func=AF.Exp), reads=['tt'], writes=['tt'])
        S.op('act', lambda e: e.activation(out=tt[:], in_=tt[:], func=AF.Ln, bias=P.ones[:, 0:1], scale=1.0), reads=['tt', 'ones'], writes=['tt'])
        S.op('dve', lambda e: e.tensor_tensor(out=bg[:, 16:32], in0=tt[:], in1=negA[:], op=ALU.mult), reads=['tt', 'negA'], writes=['bg1'])
        S.dma('pool', BG[rs, :], bg[:], reads=['bg0', 'bg1'], writes=['BG'])
        v = qk[:].rearrange("p (g h d) -> p g h d", h=2, d=32)
        o = qo[:].rearrange("p (g h d) -> p g h d", h=2, d=32)
        cb = bc(cs[:, 0, :], 1, 16); sb_ = bc(cs[:, 1, :], 1, 16)
        r0v = r0[:].rearrange("p (g d) -> p g d", d=32); r1v = r1[:].rearrange("p (g d) -> p g d", d=32)
        S.op('dve', lambda e: e.tensor_tensor(out=r0v, in0=v[:, :, 0, :], in1=cb, op=ALU.mult), reads=['qk', 'cs0'], writes=['r0'])
        S.op('pool', lambda e: e.tensor_tensor(out=r1v, in0=v[:, :, 1, :], in1=sb_, op=ALU.mult), reads=['qk', 'cs1'], writes=['r1'])
        S.op('dve', lambda e: e.tensor_tensor(out=o[:, :, 0, :], in0=r0v, in1=r1v, op=ALU.subtract), reads=['r0', 'r1'], writes=['qo0'])
        S.op('pool', lambda e: e.tensor_tensor(out=r0v, in0=v[:, :, 0, :], in1=sb_, op=ALU.mult), reads=['qk', 'cs1', 'r0'], writes=['r0'])
        S.op('dve', lambda e: e.tensor_tensor(out=r1v, in0=v[:, :, 1, :], in1=cb, op=ALU.mult), reads=['qk', 'cs0', 'r1'], writes=['r1'])
        S.op('pool', lambda e: e.tensor_tensor(out=o[:, :, 1, :], in0=r0v, in1=r1v, op=ALU.add), reads=['r0', 'r1'], writes=['qo1'])
        S.dma('pool', QKr[rs, :], qo[:], reads=['qo0', 'qo1'], writes=['QKr'])
    return P


def rope_tables():
    rows = 8192 // 64
    row = np.repeat(np.arange(rows, dtype=np.float32), 64)
    col = np.tile(np.arange(64, dtype=np.float32), rows)
    inv = (10000.0 ** (-np.arange(0, 32, 2, dtype=np.float32) / 32)).astype(np.float32)
    ang = np.concatenate([row[:, None] * inv, col[:, None] * inv], axis=-1).astype(np.float32)
    return np.cos(ang).astype(np.float32), np.sin(ang).astype(np.float32)


def shift_rows(a, s):
    b = np.zeros_like(a)
    if s == -1:
        b[..., 1:, :] = a[..., :-1, :]
    else:
        b[..., :-1, :] = a[..., 1:, :]
    return b


def run_l2(Plat, Pctx, conv_w0, a_log0, dt_bias0):
    P = build_l2()
    cos, sin = rope_tables()
    in_maps = []
    for core in range(NCORES):
        b, hf = core // 2, core % 2
        sl, sc = slice(hf * 4096, (hf + 1) * 4096), slice(hf * 128, (hf + 1) * 128)
        cat = lambda A, B: np.ascontiguousarray(np.concatenate([A, B], axis=0))
        ql, qc = Plat[b, :, :1536], Pctx[b, :, :1536]
        in_maps.append(dict(
            Pp=cat(shift_rows(ql, -1)[sl], shift_rows(qc, -1)[sc]), Pc=cat(ql[sl], qc[sc]), Pn=cat(shift_rows(ql, 1)[sl], shift_rows(qc, 1)[sc]),
            Pab=cat(Plat[b, sl, 2048:2080], Pctx[b, sc, 2048:2080]), Pqk=cat(Plat[b, sl, 2080:3104], Pctx[b, sc, 2080:3104]),
            cosT=cat(cos[sl], np.ones((128, 32), np.float32)), sinT=cat(sin[sl], np.zeros((128, 32), np.float32)),
            convw=np.ascontiguousarray(np.broadcast_to(conv_w0[None], (128, 3, 1536))), alog=rep128(a_log0.reshape(-1)), dtb=rep128(dt_bias0.reshape(-1))))
    res = P.run(in_maps)
    out = {}
    for name, w in (("QKV", 1536), ("BG", 32), ("QKr", 1024)):
        lat = np.empty((4, 8192, w), np.float32); cx = np.empty((4, 256, w), np.float32)
        for core in range(NCORES):
            b, hf = core // 2, core % 2
            r = res[core][name]
            lat[b, hf * 4096:(hf + 1) * 4096] = r[:4096]
            cx[b, hf * 128:(hf + 1) * 128] = r[4096:]
        out[name] = (lat, cx)
    return out


GD_CH = 132
GD_CTX = 4


import os
LIM = int(os.environ.get('LIM', '99'))


def build_l3(nch=GD_CH, nctx=GD_CTX, stage=9):
    P = Prog()
    S = P.S
    T = nch * 64
    Kt = P.din("Kt", [T, 512]); Vt = P.din("Vt", [T, 512])
    KT = P.din("KT", [nch, 64, 512]); QT = P.din("QT", [nch, 64, 512])
    Bt = P.din("Bt", [T, 8]); Gt = P.din("Gt", [T, 8])
    O = P.dout("O", [(nch - nctx) * 64, 512])
    N = 64

    def mask(name, op, sgn=1):
        m = P.sb(name, [N, N])
        S.op('pool', lambda g: g.memset(m[:], 1.0), writes=[name])
        S.op('pool', lambda g: g.affine_select(out=m[:], in_=m[:], pattern=[[-sgn, N]], compare_op=op, fill=0.0, base=0, channel_multiplier=sgn),
             reads=[name], writes=[name])
        return m
    mL = mask("mL", ALU.is_ge); mLs = mask("mLs", ALU.is_gt); mU = mask("mU", ALU.is_ge, -1); mUs = mask("mUs", ALU.is_gt, -1); I64 = mask("I64", ALU.is_equal)
    ones = P.sb("ones64", [N, N]); S.op('pool', lambda g: g.memset(ones[:], 1.0), writes=['ones64'])
    B = [P.ps("b%d" % i, [N, 512]) for i in range(8)]
    w3 = lambda t: t[:].rearrange("p (h j) -> p h j", j=64)
    sbw = lambda name: P.sb(name, [N, 512])
    kt = sbw("kt"); vt = sbw("vt"); kT = sbw("kT"); qT = sbw("qT")
    b8 = P.sb("b8", [N, 8]); g8 = P.sb("g8", [N, 8]); gc = P.sb("gc", [N, 8]); egc = P.sb("egc", [N, 8]); nb8 = P.sb("nb8", [N, 8]); bge = P.sb("bge", [N, 8])
    R = sbw("R"); arg = sbw("arg"); expgB = sbw("expgB"); t1 = sbw("t1"); t2 = sbw("t2")
    Dls = sbw("Dls"); Dus = sbw("Dus"); Du = sbw("Du"); E2 = sbw("E2")
    Q = [sbw("Q0"), sbw("Q1")]; QTt = [sbw("QT0"), sbw("QT1")]; PT = [sbw("PT0"), sbw("PT1")]
    AT = sbw("AT"); vb = sbw("vb"); kbg = sbw("kbg"); U = sbw("U"); WT = sbw("WT"); ktl = sbw("ktl"); qdT = sbw("qdT")
    vnew = sbw("vnew"); Sst = sbw("Sst"); ot = sbw("ot"); stmp = sbw("stmp")
    S.op('pool', lambda g: g.memset(Sst[:], 0.0), writes=['Sst'])
    hs = lambda t, h: t[:, h * 64:(h + 1) * 64]

    for c in range(nch):
        rs = slice(c * 64, (c + 1) * 64)
        S.dma('sp', kt[:], Kt[rs, :], writes=['kt']); S.dma('sp', vt[:], Vt[rs, :], writes=['vt'])
        S.dma('sp', kT[:], KT[c], writes=['kT']); S.dma('sp', qT[:], QT[c], writes=['qT'])
        S.dma('sp', b8[:], Bt[rs, :], writes=['b8']); S.dma('sp', g8[:], Gt[rs, :], writes=['g8'])
        if stage < -3: continue
        S.op('dve', lambda e: e.tensor_tensor(out=w3(R), in0=bc(g8[:], 2, 64), in1=bc(mU[:], 1, 8), op=ALU.mult), reads=['g8', 'mU'], writes=['R'])
        S.op('pe', lambda e: e.matmul(B[0][:], lhsT=ones[:], rhs=R[:], start=True, stop=True), reads=['ones64', 'R'], writes=['b0'])
        if stage < -2: continue
        S.op('pe', lambda e: e.matmul(B[4][:, 0:8], lhsT=mU[:], rhs=g8[:], start=True, stop=True), reads=['mU', 'g8'], writes=['b4'])
        S.op('act', lambda e: e.activation(out=gc[:], in_=B[4][:, 0:8], func=AF.Copy), reads=['b4'], writes=['gc'])
        if stage < -1: continue
        if LIM > 0:
            S.op('dve', lambda e: e.tensor_tensor(out=w3(arg), in0=bc(gc[:], 2, 64), in1=w3(B[0]), op=ALU.subtract), reads=['gc', 'b0'], writes=['arg'])
        if LIM > 1:
            S.op('dve', lambda e: e.tensor_scalar(out=expgB[:], in0=B[0][:], scalar1=-80.0, scalar2=None, op0=ALU.max), reads=['b0'], writes=['expgB'])
            S.op('act', lambda e: e.activation(out=expgB[:], in_=expgB[:], func=AF.Exp), reads=['expgB'], writes=['expgB'])
        if LIM > 2:
            S.op('dve', lambda e: e.tensor_scalar(out=egc[:], in0=gc[:], scalar1=-80.0, scalar2=None, op0=ALU.max), reads=['gc'], writes=['egc'])
            S.op('act', lambda e: e.activation(out=egc[:], in_=egc[:], func=AF.Exp), reads=['egc'], writes=['egc'])
        if LIM > 3:
            S.op('dve', lambda e: e.tensor_scalar(out=t1[:], in0=arg[:], scalar1=0.0, scalar2=-80.0, op0=ALU.min, op1=ALU.max), reads=['arg'], writes=['t1'])
        if LIM > 4:
            S.op('act', lambda e: e.activation(out=t1[:], in_=t1[:], func=AF.Exp), reads=['t1'], writes=['t1'])
        if LIM > 5:
            S.op('dve', lambda e: e.tensor_tensor(out=w3(Dls), in0=w3(t1), in1=bc(mLs[:], 1, 8), op=ALU.mult), reads=['t1', 'mLs'], writes=['Dls'])
        if LIM > 6:
            S.op('dve', lambda e: e.tensor_scalar(out=t2[:], in0=arg[:], scalar1=-1.0, scalar2=0.0, op0=ALU.mult, op1=ALU.min), reads=['arg'], writes=['t2'])
            S.op('dve', lambda e: e.tensor_scalar(out=t2[:], in0=t2[:], scalar1=-80.0, scalar2=None, op0=ALU.max), reads=['t2'], writes=['t2'])
        if LIM > 7:
            S.op('act', lambda e: e.activation(out=E2[:], in_=t2[:], func=AF.Exp), reads=['t2'], writes=['E2'])
        if LIM > 8:
            S.op('dve', lambda e: e.tensor_tensor(out=w3(Dus), in0=w3(E2), in1=bc(mUs[:], 1, 8), op=ALU.mult), reads=['E2', 'mUs'], writes=['Dus'])
        if LIM > 9:
            S.op('pool', lambda e: e.tensor_tensor(out=w3(Du), in0=w3(E2), in1=bc(mU[:], 1, 8), op=ALU.mult), reads=['E2', 'mU'], writes=['Du'])
        if stage < 1: continue
        S.op('dve', lambda e: e.tensor_tensor(out=w3(R), in0=bc(b8[:], 2, 64), in1=bc(I64[:], 1, 8), op=ALU.mult), reads=['b8', 'I64', 'R'], writes=['R'])
        S.op('pe', lambda e: e.matmul(B[1][:], lhsT=ones[:], rhs=R[:], start=True, stop=True), reads=['ones64', 'R'], writes=['b1'])
        S.op('dve', lambda e: e.tensor_scalar(out=nb8[:], in0=b8[:], scalar1=-1.0, scalar2=None, op0=ALU.mult), reads=['b8'], writes=['nb8'])
        if stage < 2: continue
        for h in range(8):
            S.op('pe', lambda e: e.matmul(hs(B[2], h), lhsT=hs(kT, h), rhs=hs(kT, h), start=True, stop=True), reads=['kT'], writes=['b2'])
        for h in range(8):
            S.op('pe', lambda e: e.matmul(hs(B[3], h), lhsT=hs(kT, h), rhs=hs(qT, h), start=True, stop=True), reads=['kT', 'qT'], writes=['b3'])
        if stage < 3: continue
        S.op('dve', lambda e: e.tensor_tensor(out=t1[:], in0=B[2][:], in1=Dls[:], op=ALU.mult), reads=['b2', 'Dls', 't1'], writes=['t1'])
        S.op('dve', lambda e: e.tensor_tensor(out=w3(Q[0]), in0=w3(t1), in1=bc(nb8[:], 2, 64), op=ALU.mult), reads=['t1', 'nb8'], writes=['Q0'])
        S.op('dve', lambda e: e.tensor_tensor(out=t2[:], in0=B[2][:], in1=Dus[:], op=ALU.mult), reads=['b2', 'Dus', 't2'], writes=['t2'])
        S.op('dve', lambda e: e.scalar_tensor_tensor(out=QTt[0][:], in0=B[1][:], scalar=-1.0, in1=t2[:], op0=ALU.mult, op1=ALU.mult), reads=['b1', 't2'], writes=['QT0'])
        S.op('dve', lambda e: e.tensor_tensor(out=AT[:], in0=B[3][:], in1=Du[:], op=ALU.mult), reads=['b3', 'Du'], writes=['AT'])
        S.op('pool', lambda e: e.tensor_tensor(out=w3(PT[0]), in0=w3(QTt[0]), in1=bc(I64[:], 1, 8), op=ALU.add), reads=['QT0', 'I64'], writes=['PT0'])
        if stage < 4: continue
        cur = 0
        for l in range(5):
            nx = 1 - cur
            qk_, qtk, qn, qtn = 'Q%d' % cur, 'QT%d' % cur, 'Q%d' % nx, 'QT%d' % nx
            for h in range(8):
                S.op('pe', lambda e: e.matmul(hs(B[4], h), lhsT=hs(QTt[cur], h), rhs=hs(Q[cur], h), start=True, stop=True), reads=[qk_, qtk], writes=['b4'])
            if l < 4:
                for h in range(8):
                    S.op('pe', lambda e: e.matmul(hs(B[5], h), lhsT=hs(Q[cur], h), rhs=hs(QTt[cur], h), start=True, stop=True), reads=[qk_, qtk], writes=['b5'])
            S.op('act', lambda e: e.activation(out=Q[nx][:], in_=B[4][:], func=AF.Copy), reads=['b4'], writes=[qn])
            if l < 4:
                S.op('dve', lambda e: e.tensor_copy(out=QTt[nx][:], in_=B[5][:]), reads=['b5'], writes=[qtn])
            pk, pn = 'PT%d' % (l % 2), 'PT%d' % ((l + 1) % 2)
            for h in range(8):
                S.op('pe', lambda e: e.matmul(hs(B[6], h), lhsT=hs(Q[nx], h), rhs=hs(PT[l % 2], h), start=True, stop=True), reads=[qn, pk], writes=['b6'])
            S.op('dve', lambda e: e.tensor_tensor(out=PT[(l + 1) % 2][:], in0=B[6][:], in1=PT[l % 2][:], op=ALU.add), reads=['b6', pk], writes=[pn])
            cur = nx
        TT = PT[1]; ttk = 'PT1'
        if stage < 5: continue
        S.op('pool', lambda e: e.tensor_tensor(out=w3(vb), in0=w3(vt), in1=bc(b8[:], 2, 64), op=ALU.mult), reads=['vt', 'b8'], writes=['vb'])
        S.op('dve', lambda e: e.tensor_tensor(out=bge[:], in0=b8[:], in1=egc[:], op=ALU.mult), reads=['b8', 'egc'], writes=['bge'])
        S.op('dve', lambda e: e.tensor_tensor(out=w3(kbg), in0=w3(kt), in1=bc(bge[:], 2, 64), op=ALU.mult), reads=['kt', 'bge'], writes=['kbg'])
        S.op('pool', lambda e: e.tensor_tensor(out=w3(ktl), in0=w3(kt), in1=bc(w3(E2)[:, :, 63], 2, 64), op=ALU.mult), reads=['kt', 'E2'], writes=['ktl'])
        S.op('dve', lambda e: e.tensor_tensor(out=qdT[:], in0=qT[:], in1=expgB[:], op=ALU.mult), reads=['qT', 'expgB'], writes=['qdT'])
        for h in range(8):
            S.op('pe', lambda e: e.matmul(hs(B[0], h), lhsT=hs(TT, h), rhs=hs(vb, h), start=True, stop=True), reads=[ttk, 'vb'], writes=['b0'])
        for h in range(8):
            S.op('pe', lambda e: e.matmul(hs(B[1], h), lhsT=hs(kbg, h), rhs=hs(TT, h), start=True, stop=True), reads=[ttk, 'kbg'], writes=['b1'])
        S.op('act', lambda e: e.activation(out=U[:], in_=B[0][:], func=AF.Copy), reads=['b0'], writes=['U'])
        S.op('dve', lambda e: e.tensor_copy(out=WT[:], in_=B[1][:]), reads=['b1'], writes=['WT'])
        if stage < 6: continue
        for h in range(8):
            S.op('pe', lambda e: e.matmul(hs(B[2], h), lhsT=hs(WT, h), rhs=hs(Sst, h), start=True, stop=True), reads=['WT', 'Sst'], writes=['b2'])
        S.op('dve', lambda e: e.tensor_tensor(out=vnew[:], in0=U[:], in1=B[2][:], op=ALU.subtract), reads=['U', 'b2'], writes=['vnew'])
        if c >= nctx:
            for h in range(8):
                S.op('pe', lambda e: e.matmul(hs(B[3], h), lhsT=hs(qdT, h), rhs=hs(Sst, h), start=True, stop=False), reads=['qdT', 'Sst'], writes=['b3'])
                S.op('pe', lambda e: e.matmul(hs(B[3], h), lhsT=hs(AT, h), rhs=hs(vnew, h), start=False, stop=True), reads=['AT', 'vnew'], writes=['b3'])
            S.op('act', lambda e: e.activation(out=ot[:], in_=B[3][:], func=AF.Copy), reads=['b3'], writes=['ot'])
            S.dma('pool', O[(c - nctx) * 64:(c - nctx + 1) * 64, :], ot[:], reads=['ot'], writes=['O'])
        for h in range(8):
            S.op('pe', lambda e: e.matmul(hs(B[7], h), lhsT=hs(ktl, h), rhs=hs(vnew, h), start=True, stop=True), reads=['ktl', 'vnew'], writes=['b7'])
        S.op('dve', lambda e: e.tensor_tensor(out=w3(stmp), in0=w3(Sst), in1=bc(w3(expgB)[:, :, 63], 2, 64), op=ALU.mult), reads=['Sst', 'expgB'], writes=['stmp'])
        S.op('dve', lambda e: e.tensor_tensor(out=Sst[:], in0=stmp[:], in1=B[7][:], op=ALU.add), reads=['stmp', 'b7'], writes=['Sst'])
    if stage < 9:
        S.dma('pool', O[0:64, :], Sst[:], reads=['Sst'], writes=['O'])
    return P


def run_l3(QKV, BG, nch=GD_CH, nctx=GD_CTX, stage=9):
    P = build_l3(nch, nctx, stage)
    L = (nch - nctx) * 64
    C = nctx * 64
    in_maps = []
    for core in range(NCORES):
        b, dr = core // 2, core % 2
        def seq(lat, cx):
            a, c_ = lat[b, :L], cx[b, :C]
            if dr == 1:
                a, c_ = a[::-1], c_[::-1]
            return np.concatenate([c_, a], axis=0)
        qkv = seq(QKV[0], QKV[1]); bg = seq(BG[0], BG[1])
        q, k, v = qkv[:, :512], qkv[:, 512:1024], qkv[:, 1024:1536]
        fm = lambda a: np.ascontiguousarray(a.reshape(nch, 64, 8, 64).transpose(0, 3, 2, 1).reshape(nch, 64, 512))
        in_maps.append(dict(Kt=np.ascontiguousarray(k), Vt=np.ascontiguousarray(v), KT=fm(k), QT=fm(q),
                            Bt=np.ascontiguousarray(bg[:, dr * 8:dr * 8 + 8]), Gt=np.ascontiguousarray(bg[:, 16 + dr * 8:16 + dr * 8 + 8])))
    res = P.run(in_maps)
    O = np.empty((4, 2, L, 512), np.float32)
    for core in range(NCORES):
        b, dr = core // 2, core % 2
        o = res[core]["O"]
        O[b, dr] = o[::-1] if dr == 1 else o
    return O


BF16 = mybir.dt.bfloat16
NKEY = 8448
NKT = NKEY // 128


def build_l4(nq=8192, lam_init=0.2):
    P = Prog()
    S = P.S
    qT = P.din("qT", [2, 2, 64, nq]); kT = P.din("kT", [2, 2, 64, NKEY]); V = P.din("V", [2, NKEY, 128])
    lam = P.din("lam", [128, 4, 64])
    DT = P.dout("DT", [2, 128, nq])
    consts(P)
    onesb = P.sb("onesb", [128, 128], BF16)
    S.op('dve', lambda e: e.tensor_copy(out=onesb[:], in_=P.ones[:, 0:128]), reads=['ones'], writes=['onesb'])
    lt = P.sb("lamt", [128, 4, 64]); S.dma('sp', lt[:], lam, writes=['lamt'])
    lp = P.sb("lamp", [128, 2, 64]); ls = P.sb("lams", [128, 4])
    S.op('dve', lambda e: e.tensor_tensor(out=lp[:, 0, :], in0=lt[:, 0, :], in1=lt[:, 1, :], op=ALU.mult), reads=['lamt'], writes=['lamp0'])
    S.op('dve', lambda e: e.tensor_tensor(out=lp[:, 1, :], in0=lt[:, 2, :], in1=lt[:, 3, :], op=ALU.mult), reads=['lamt'], writes=['lamp1'])
    S.op('dve', lambda e: e.tensor_reduce(out=ls[:, 0:2], in_=lp[:], axis=AX.X, op=ALU.add), reads=['lamp0', 'lamp1'], writes=['lams'])
    S.op('act', lambda e: e.activation(out=ls[:, 0:2], in_=ls[:, 0:2], func=AF.Exp), reads=['lams'], writes=['lams'])
    S.op('dve', lambda e: e.tensor_tensor(out=ls[:, 2:3], in0=ls[:, 1:2], in1=ls[:, 0:1], op=ALU.subtract), reads=['lams'], writes=['lams2'])
    S.op('dve', lambda e: e.tensor_scalar(out=ls[:, 3:4], in0=ls[:, 2:3], scalar1=-lam_init, scalar2=None, op0=ALU.add), reads=['lams2'], writes=['neglam'])
    kTt = P.sb("kTt", [64, 2, NKEY]); Vf = P.sb("Vf", [128, NKT, 128]); Vb = P.sb("Vb", [128, NKT, 128], BF16)
    kTb = P.sb("kTb", [64, 2, NKEY], BF16)
    qTt = P.sb("qTt", [64, 2, 512])
    qTb = [P.sb("qTb%d" % i, [64, 2, 512], BF16) for i in range(2)]
    Pt = [P.sb("Pt%d" % i, [128, 512], BF16) for i in range(3)]
    pS = [P.ps("pS%d" % i, [128, 512]) for i in range(2)]
    pO = [P.ps("pO%d" % i, [128, 512]) for i in range(2)]
    pZ = [P.ps("pZ%d" % i, [128, 512]) for i in range(2)]
    rz = P.sb("rz", [128, 512]); Om = [P.sb("Om%d" % i, [128, 512]) for i in range(2)]
    Dt = [P.sb("Dt%d" % i, [128, 512]) for i in range(2)]
    items = [(h, qc, m, kt) for h in range(2) for qc in range(nq // 512) for m in range(2) for kt in range(NKT)]

    def emit_s(j):
        h, qc, m, kt = items[j]
        if qc == 0 and m == 0 and kt == 0:
            S.dma('sp', kTt[:], kT[h].rearrange("m d k -> d m k"), writes=['kTt'])
            S.dma('sp', Vf[:], V[h].rearrange("(t p) e -> p t e", p=128), writes=['Vf'])
            S.op('dve', lambda e: e.tensor_copy(out=Vb[:], in_=Vf[:]), reads=['Vf'], writes=['Vb'])
            S.op('dve', lambda e: e.tensor_copy(out=kTb[:], in_=kTt[:]), reads=['kTt'], writes=['kTb'])
        if m == 0 and kt == 0:
            S.dma('sp', qTt[:], qT[h, :, :, qc * 512:(qc + 1) * 512].rearrange("m d k -> d m k"), writes=['qTt'])
            S.op('dve', lambda e: e.tensor_copy(out=qTb[qc % 2][:], in_=qTt[:]), reads=['qTt'], writes=['qTb%d' % (qc % 2)])
        S.op('pe', lambda e: e.matmul(pS[j % 2][:], lhsT=kTb[:, m, kt * 128:(kt + 1) * 128], rhs=qTb[qc % 2][:, m, :], start=True, stop=True),
             reads=['kTb', 'qTb%d' % (qc % 2)], writes=['pS%d' % (j % 2)])

    emit_s(0)
    for j, (h, qc, m, kt) in enumerate(items):
        if j + 1 < len(items):
            emit_s(j + 1)
        ps = pS[j % 2]; psk = 'pS%d' % (j % 2)
        pt = Pt[j % 3]; ptk = 'Pt%d' % (j % 3)
        S.op('act', lambda e: e.activation(out=pt[:], in_=ps[:], func=AF.Exp, scale=0.125), reads=[psk], writes=[ptk])
        S.op('pe', lambda e: e.matmul(pO[m][:], lhsT=Vb[:, kt, :], rhs=pt[:], start=(kt == 0), stop=(kt == NKT - 1)),
             reads=['Vb', ptk], writes=['pO%d' % m])
        S.op('pe', lambda e: e.matmul(pZ[m][:], lhsT=onesb[:], rhs=pt[:], start=(kt == 0), stop=(kt == NKT - 1)),
             reads=['onesb', ptk], writes=['pZ%d' % m])
        if kt == NKT - 1:
            S.op('dve', lambda e: e.reciprocal(out=rz[:], in_=pZ[m][:]), reads=['pZ%d' % m], writes=['rz'])
            S.op('dve', lambda e: e.tensor_tensor(out=Om[m][:], in0=pO[m][:], in1=rz[:], op=ALU.mult), reads=['pO%d' % m, 'rz'], writes=['Om%d' % m])
            if m == 1:
                dt_ = Dt[qc % 2]; dk = 'Dt%d' % (qc % 2)
                S.op('dve', lambda e: e.scalar_tensor_tensor(out=dt_[:], in0=Om[1][:], scalar=ls[:, 3:4], in1=Om[0][:], op0=ALU.mult, op1=ALU.add),
                     reads=['Om0', 'Om1', 'neglam'], writes=[dk])
                S.dma('pool', DT[h, :, qc * 512:(qc + 1) * 512], dt_[:], reads=[dk], writes=['DT'])
    return P


def run_l4(QKr, Plat, Pctx, lam_q1, lam_k1, lam_q2, lam_k2, nq=8192):
    P = build_l4(nq)
    lamin = np.ascontiguousarray(np.broadcast_to(np.stack([lam_q1, lam_k1, lam_q2, lam_k2])[None], (128, 4, 64))).astype(np.float32)
    in_maps = []
    for core in range(NCORES):
        b, hp = core // 2, core % 2
        q = QKr[0][b, :nq, 0:512].reshape(nq, 4, 2, 64)[:, 2 * hp:2 * hp + 2]
        k = np.concatenate([QKr[0][b, :, 512:1024], QKr[1][b, :, 512:1024]], axis=0).reshape(NKEY, 4, 2, 64)[:, 2 * hp:2 * hp + 2]
        v = np.concatenate([Plat[b, :, 3104:3616], Pctx[b, :, 3104:3616]], axis=0).reshape(NKEY, 4, 128)[:, 2 * hp:2 * hp + 2]
        in_maps.append(dict(qT=np.ascontiguousarray(q.transpose(1, 2, 3, 0)), kT=np.ascontiguousarray(k.transpose(1, 2, 3, 0)),
                            V=np.ascontiguousarray(v.transpose(1, 0, 2)), lam=lamin))
    res = P.run(in_maps)
    out = np.empty((4, nq, 512), np.float32)
    for core in range(NCORES):
        b, hp = core // 2, core % 2
        dt = res[core]["DT"]
        out[b, :, hp * 256:(hp + 1) * 256] = dt.transpose(2, 0, 1).reshape(nq, 256)
    return out


TOK = 4096
NTT = TOK // 128


def build_l5a(kind):
    P = Prog()
    S = P.S
    X = P.din("X", [TOK, D])
    if kind == 'ab':
        O0 = P.din("O0", [TOK, 512]); O1 = P.din("O1", [TOK, 512]); GATE = P.din("GATE", [TOK, 512]); DL = P.din("DL", [TOK, 512])
        gdnw = P.din("gdnw", [128, 64]); sublnw = P.din("sublnw", [128, 128])
    else:
        FM = P.din("FM", [TOK, D])
    wout = P.din("wout", [D, D])
    cT = P.din("cT", [128, 1, 8]); adaw = P.din("adaw", [D, 1024]); adab = P.din("adab", [128, 1024])
    X1 = P.dout("X1", [TOK, D])
    consts(P)
    csil = P.sb("csil", [128, 1, 8]); S.dma('sp', csil[:], cT, writes=['csil'])
    S.op('act', lambda e: e.activation(out=csil[:], in_=csil[:], func=AF.Silu), reads=['csil'], writes=['csil'])
    g1b = P.sb("g1b", [128, D])
    emit_mod(P, csil, 0, adaw, adab, 0, 1024, g1b, 'g1b', P.ones, "m")
    wt = P.sb("wt", [128, 8, D])
    wv = wout.rearrange("(k p) n -> p k n", p=128)
    for k in range(8):
        S.dma('sp', wt[:, k, :], wv[:, k, :], writes=['wt%d' % k])
    if kind == 'ab':
        gw = P.sb("gw", [128, 64]); S.dma('sp', gw[:], gdnw, writes=['gw'])
        sw = P.sb("sw", [128, 128]); S.dma('sp', sw[:], sublnw, writes=['sw'])
        o0 = P.sb("o0", [128, 512]); o1 = P.sb("o1", [128, 512]); gt = P.sb("gt", [128, 512]); dl = P.sb("dl", [128, 512])
        sq = P.sb("sq", [128, 512]); st = P.sb("st", [128, 3, 12])
    xt = P.sb("xt", [128, D]); mix = P.sb("mix", [128, D]); mixT = P.sb("mixT", [128, D]); x1 = P.sb("x1", [128, D])
    pT = P.ps("pT", [128, D]); pY = P.ps("pY", [128, D])
    for t in range(NTT):
        rs = slice(t * 128, (t + 1) * 128)
        S.dma('sp', xt[:], X[rs, :], writes=['xt'])
        if kind == 'ab':
            S.dma('sp', o0[:], O0[rs, :], writes=['o0']); S.dma('sp', o1[:], O1[rs, :], writes=['o1'])
            S.dma('sp', gt[:], GATE[rs, :], writes=['gt']); S.dma('sp', dl[:], DL[rs, :], writes=['dl'])
            S.op('dve', lambda e: e.tensor_tensor(out=o0[:], in0=o0[:], in1=o1[:], op=ALU.add), reads=['o0', 'o1'], writes=['o0'])
            S.op('act', lambda e: e.activation(out=sq[:], in_=o0[:], func=AF.Square), reads=['o0'], writes=['sq'])
            S.op('dve', lambda e: e.tensor_reduce(out=st[:, 0, 0:8], in_=sq[:].rearrange("p (g d) -> p g d", d=64), axis=AX.X, op=ALU.add), reads=['sq'], writes=['st0a'])
            S.op('act', lambda e: e.activation(out=sq[:], in_=dl[:], func=AF.Square), reads=['dl', 'st0a'], writes=['sq'])
            S.op('dve', lambda e: e.tensor_reduce(out=st[:, 0, 8:12], in_=sq[:].rearrange("p (g d) -> p g d", d=128), axis=AX.X, op=ALU.add), reads=['sq'], writes=['st0b'])
            S.op('act', lambda e: e.activation(out=st[:, 1, 0:8], in_=st[:, 0, 0:8], func=AF.Sqrt, bias=P.epsb[:, 0:1], scale=1.0 / 64), reads=['st0a', 'epsb'], writes=['st1a'])
            S.op('act', lambda e: e.activation(out=st[:, 1, 8:12], in_=st[:, 0, 8:12], func=AF.Sqrt, bias=P.epsb[:, 0:1], scale=1.0 / 128), reads=['st0b', 'epsb'], writes=['st1b'])
            S.op('dve', lambda e: e.reciprocal(out=st[:, 2, :], in_=st[:, 1, :]), reads=['st1a', 'st1b'], writes=['st2'])
            S.op('dve', lambda e: e.tensor_scalar(out=st[:, 2, 8:12], in0=st[:, 2, 8:12], scalar1=0.8, scalar2=None, op0=ALU.mult), reads=['st2'], writes=['st2'])
            v8 = lambda a: a.rearrange("p (g d) -> p g d", d=64)
            v4 = lambda a: a.rearrange("p (g d) -> p g d", d=128)
            S.op('dve', lambda e: e.tensor_tensor(out=v8(o0[:]), in0=v8(o0[:]), in1=bc(st[:, 2, 0:8], 2, 64), op=ALU.mult), reads=['o0', 'st2'], writes=['o0'])
            S.op('pool', lambda e: e.tensor_tensor(out=v8(o0[:]), in0=v8(o0[:]), in1=bc(gw[:], 1, 8), op=ALU.mult), reads=['o0', 'gw'], writes=['o0'])
            S.op('act', lambda e: e.activation(out=gt[:], in_=gt[:], func=AF.Silu), reads=['gt'], writes=['gt'])
            S.op('dve', lambda e: e.tensor_tensor(out=mix[:, 0:512], in0=o0[:], in1=gt[:], op=ALU.mult), reads=['o0', 'gt'], writes=['mix0'])
            S.op('dve', lambda e: e.tensor_tensor(out=v4(dl[:]), in0=v4(dl[:]), in1=bc(st[:, 2, 8:12], 2, 128), op=ALU.mult), reads=['dl', 'st2'], writes=['dl'])
            S.op('pool', lambda e: e.tensor_tensor(out=v4(mix[:, 512:1024]), in0=v4(dl[:]), in1=bc(sw[:], 1, 4), op=ALU.mult), reads=['dl', 'sw'], writes=['mix1'])
        else:
            S.dma('sp', mix[:], FM[rs, :], writes=['mix0', 'mix1'])
        for k in range(8):
            S.op('pe', lambda e: e.transpose(pT[:, k * 128:(k + 1) * 128], mix[:, k * 128:(k + 1) * 128], P.idf[:]),
                 reads=['mix%d' % (k // 4), 'ident'], writes=['pT%d' % (k // 4)])
        S.op('act', lambda e: e.activation(out=mixT[:, 0:512], in_=pT[:, 0:512], func=AF.Copy), reads=['pT0'], writes=['mixT0'])
        S.op('dve', lambda e: e.tensor_copy(out=mixT[:, 512:1024], in_=pT[:, 512:1024]), reads=['pT1'], writes=['mixT1'])
        for cb in range(2):
            for k in range(8):
                S.op('pe', lambda e: e.matmul(pY[:, cb * 512:(cb + 1) * 512], lhsT=mixT[:, k * 128:(k + 1) * 128], rhs=wt[:, k, cb * 512:(cb + 1) * 512],
                                              start=(k == 0), stop=(k == 7)), reads=['mixT%d' % (k // 4), 'wt%d' % k], writes=['pY%d' % cb])
        S.op('dve', lambda e: e.tensor_tensor(out=x1[:], in0=pY[:], in1=g1b[:], op=ALU.mult), reads=['pY0', 'pY1', 'g1b'], writes=['x1'])
        S.op('pool', lambda e: e.tensor_tensor(out=x1[:], in0=x1[:], in1=xt[:], op=ALU.add), reads=['x1', 'xt'], writes=['x1'])
        S.dma('pool', X1[rs, :], x1[:], reads=['x1'], writes=['X1'])
    return P


def run_l5a(kind, x, c, ada_w_i, ada_b_i, wout, **kw):
    P = build_l5a(kind)
    in_maps = []
    for core in range(NCORES):
        b, hf = core // 2, core % 2
        sl = slice(hf * TOK, (hf + 1) * TOK)
        m = dict(X=np.ascontiguousarray(x[b, sl]), wout=np.ascontiguousarray(wout), cT=cT_layout(c[b][None]),
                 adaw=np.ascontiguousarray(ada_w_i[:, 2048:3072]), adab=rep128(ada_b_i[2048:3072]))
        if kind == 'ab':
            m.update(O0=np.ascontiguousarray(kw['O'][b, 0, sl]), O1=np.ascontiguousarray(kw['O'][b, 1, sl]),
                     GATE=np.ascontiguousarray(kw['Plat'][b, sl, 1536:2048]), DL=np.ascontiguousarray(kw['dlat'][b, sl]),
                     gdnw=rep128(kw['gdn_norm_w']), sublnw=rep128(kw['subln_w']))
        else:
            m.update(FM=np.ascontiguousarray(kw['fm'][b, sl]))
        in_maps.append(m)
    res = P.run(in_maps)
    out = np.empty((4, 8192, D), np.float32)
    for core in range(NCORES):
        b, hf = core // 2, core % 2
        out[b, hf * TOK:(hf + 1) * TOK] = res[core]["X1"]
    return out


def build_l5b(tail, ntt=NTT):
    P = Prog()
    S = P.S
    tok = ntt * 128
    X1 = P.din("X1", [tok, D])
    cT = P.din("cT", [128, 1, 8]); adaw = P.din("adaw", [D, 3072]); adab = P.din("adab", [128, 3072]); n2w = P.din("n2w", [128, D])
    wq = P.din("wq", [D, 2048]); keysT = P.din("keysT", [128, 16, 128])
    Utab = P.din("Utab", [16384, D]); Vtab = P.din("Vtab", [16384, D])
    if tail == 'hnext':
        adawn = P.din("adawn", [D, 2048]); adabn = P.din("adabn", [128, 2048]); n1wn = P.din("n1wn", [128, D])
        Hn = P.dout("Hn", [tok, D])
    else:
        fnw = P.din("fnw", [128, D])
    Xo = P.dout("Xo", [tok, D])
    consts(P)
    csil = P.sb("csil", [128, 1, 8]); S.dma('sp', csil[:], cT, writes=['csil'])
    S.op('act', lambda e: e.activation(out=csil[:], in_=csil[:], func=AF.Silu), reads=['csil'], writes=['csil'])
    modb = P.sb("modb", [128, 3072])
    P.mod_bw = 3072
    pA = P.ps("pA", [128, D]); pQ = P.ps("pQ", [128, 2, 512]); pSc = P.ps("pSc", [128, 2048])
    pmx = ([pQ[:, 0, :], pQ[:, 1, :]], ['pQ0', 'pQ1'])
    emit_mod(P, csil, 0, adaw, adab, 0, 3072, modb, 'modb', P.ones, "m", pmx=pmx)
    nw = P.sb("nw", [128, D]); S.dma('sp', nw[:], n2w, writes=['nw'])
    wmod2 = P.sb("wmod2", [128, D])
    S.op('dve', lambda e: e.scalar_tensor_tensor(out=wmod2[:], in0=modb[:, 1024:2048], scalar=1.0, in1=nw[:], op0=ALU.add, op1=ALU.mult),
         reads=['modb', 'nw'], writes=['wmod2'])
    if tail == 'hnext':
        modn = P.sb("modn", [128, 2048])
        emit_mod(P, csil, 0, adawn, adabn, 0, 2048, modn, 'modn', P.ones, "m", pmx=pmx)
        S.dma('sp', nw[:], n1wn, reads=['wmod2'], writes=['nw'])
        wmodn = P.sb("wmodn", [128, D])
        S.op('dve', lambda e: e.scalar_tensor_tensor(out=wmodn[:], in0=modn[:, 1024:2048], scalar=1.0, in1=nw[:], op0=ALU.add, op1=ALU.mult),
             reads=['modn', 'nw'], writes=['wmodn'])
    else:
        S.dma('sp', nw[:], fnw, reads=['wmod2'], writes=['nw'])
    wqt = P.sb("wqt", [128, 8, 2048])
    wqv = wq.rearrange("(k p) n -> p k n", p=128)
    for k in range(8):
        S.dma('sp', wqt[:, k, :], wqv[:, k, :], writes=['wq%d' % k])
    kyt = P.sb("kyt", [128, 16, 128]); S.dma('sp', kyt[:], keysT, writes=['kyt'])
    zr = P.sb("zr", [128, 255]); S.op('pool', lambda g: g.memset(zr[:], 0.0), writes=['zr']); S.op('pool', lambda g: g.memset(zr[:, 127:128], 1.0), reads=['zr'], writes=['zr'])
    iot = P.sb("iot", [128, 16]); S.op('pool', lambda g: g.iota(iot[:], pattern=[[1, 16]], base=0, channel_multiplier=0, allow_small_or_imprecise_dtypes=True), writes=['iot'])
    xt = P.sb("xt", [128, D]); hn = P.sb("hn", [128, D]); hnT = P.sb("hnT", [128, D]); ss = P.sb("ss", [128, 4])
    qTs = P.sb("qTs", [128, 16, 128]); sc = P.sb("sc", [128, 2048]); wk = P.sb("wk", [128, 2048])
    m16 = P.sb("m16", [128, 16, 16]); i16 = P.sb("i16", [128, 16, 16], U32); i16f = P.sb("i16f", [128, 16, 16])
    c16 = P.sb("c16", [128, 8, 16]); p16 = P.sb("p16", [128, 8, 16], U32); pa = P.sb("pa", [128, 8, 16], U32); pb = P.sb("pb", [128, 8, 16], U32)
    paf = P.sb("paf", [128, 8, 16]); pbf = P.sb("pbf", [128, 8, 16]); sel = P.sb("sel", [128, 2, 128]); idxf = P.sb("idxf", [128, 128])
    gat = P.sb("gat", [128, 128]); gs = P.sb("gs", [128, 2, 8])
    idxTi = P.sb("idxTi", [128, 128], I32); gateT = P.sb("gateT", [128, 128]); actA = P.sb("actA", [128, 128]); Wm = P.sb("Wm", [128, 128])
    NG = 4
    Ug = [P.sb("Ug%d" % i, [128, D], BF16) for i in range(NG)]
    Wz = [P.sb("Wz%d" % i, [128, 128], BF16) for i in range(3)]
    ujunk = P.sb("ujunk", [128, D], BF16)
    Ub = P.nc.dram_tensor("Ub16", [16384, D], BF16, kind="Internal").ap()
    Vb = P.nc.dram_tensor("Vb16", [16384, D], BF16, kind="Internal").ap()
    cvb = [P.sb("cvb%d" % i, [128, D], BF16) for i in range(2)]
    stg = [sc, wk]
    ci = 0
    for src, dst, dk in ((Utab, Ub, 'Ub16'), (Vtab, Vb, 'Vb16')):
        for r in range(128):
            st_ = stg[ci % 2]; sk_ = 'stg%d' % (ci % 2); cb_ = cvb[ci % 2]; ck_ = 'cvb%d' % (ci % 2)
            S.dma('sp', st_[:, 0:D], src[r * 128:(r + 1) * 128, :], writes=[sk_])
            if ci % 2 == 0:
                S.op('act', lambda e: e.activation(out=cb_[:], in_=st_[:, 0:D], func=AF.Copy), reads=[sk_], writes=[ck_])
            else:
                S.op('dve', lambda e: e.tensor_copy(out=cb_[:], in_=st_[:, 0:D]), reads=[sk_], writes=[ck_])
            S.dma('pool', dst[r * 128:(r + 1) * 128, :], cb_[:], reads=[ck_], writes=[dk])
            ci += 1
    pB = [pSc[:, 0:1024], pA[:, :]]
    pBk = [['pSc0', 'pSc1'], ['pA0', 'pA1']]
    pOut = pSc[:, 1024:2048]; pOk = ['pSc2', 'pSc3']
    cand = wk; junk = sc
    v4 = lambda a: a.rearrange("p (h a b) -> p h a b", a=16, b=16)
    for t in range(ntt):
        rs = slice(t * 128, (t + 1) * 128)
        S.dma('sp', xt[:], X1[rs, :], writes=['xt'])
        emit_rmsnorm_mod(P, xt, 'xt', wmod2, modb[:, 0:1024], ['wmod2', 'modb'], hn, 'hn', (hn, ss))
        for k in range(8):
            S.op('pe', lambda e: e.transpose(pA[:, k * 128:(k + 1) * 128], hn[:, k * 128:(k + 1) * 128], P.idf[:]), reads=['hn', 'ident'], writes=['pA%d' % (k // 4)])
        S.op('act', lambda e: e.activation(out=hnT[:, 0:512], in_=pA[:, 0:512], func=AF.Copy), reads=['pA0'], writes=['hnT0'])
        S.op('dve', lambda e: e.tensor_copy(out=hnT[:, 512:1024], in_=pA[:, 512:1024]), reads=['pA1'], writes=['hnT1'])
        for g in range(4):
            for j in range(4):
                hp = g * 4 + j
                for k in range(8):
                    S.op('pe', lambda e: e.matmul(pQ[:, g % 2, j * 128:(j + 1) * 128], lhsT=wqt[:, k, hp * 128:(hp + 1) * 128], rhs=hnT[:, k * 128:(k + 1) * 128],
                                                  start=(k == 0), stop=(k == 7)), reads=['wq%d' % k, 'hnT%d' % (k // 4)], writes=['pQ%d' % (g % 2)])
            eng = 'act' if g % 2 == 0 else 'dve'
            if eng == 'act':
                S.op('act', lambda e: e.activation(out=qTs[:, g * 4:(g + 1) * 4, :], in_=pQ[:, g % 2, :].rearrange("p (j n) -> p j n", n=128), func=AF.Copy), reads=['pQ%d' % (g % 2)], writes=['qTs%d' % g])
            else:
                S.op('dve', lambda e: e.tensor_copy(out=qTs[:, g * 4:(g + 1) * 4, :], in_=pQ[:, g % 2, :].rearrange("p (j n) -> p j n", n=128)), reads=['pQ%d' % (g % 2)], writes=['qTs%d' % g])
        for hp in range(16):
            S.op('pe', lambda e: e.matmul(pSc[:, hp * 128:(hp + 1) * 128], lhsT=qTs[:, hp, :], rhs=kyt[:, hp, :], start=True, stop=True),
                 reads=['qTs%d' % (hp // 4), 'kyt'], writes=['pSc%d' % (hp // 4)])
        for g in range(4):
            if g % 2 == 0:
                S.op('act', lambda e: e.activation(out=sc[:, g * 512:(g + 1) * 512], in_=pSc[:, g * 512:(g + 1) * 512], func=AF.Copy), reads=['pSc%d' % g], writes=['sc%d' % g, 'stg0'])
            else:
                S.op('dve', lambda e: e.tensor_copy(out=sc[:, g * 512:(g + 1) * 512], in_=pSc[:, g * 512:(g + 1) * 512]), reads=['pSc%d' % g], writes=['sc%d' % g, 'stg0'])
        for hp in range(16):
            blk = slice(hp * 128, (hp + 1) * 128)
            sk = 'sc%d' % (hp // 4)
            S.op('dve', lambda e: e.max(out=m16[:, hp, 0:8], in_=sc[:, blk]), reads=[sk], writes=['m16a'])
            S.op('dve', lambda e: e.match_replace(out=wk[:, blk], in_to_replace=m16[:, hp, 0:8], in_values=sc[:, blk], imm_value=-1e30), reads=[sk, 'm16a'], writes=['wk', 'stg1'])
            S.op('dve', lambda e: e.max(out=m16[:, hp, 8:16], in_=wk[:, blk]), reads=['wk'], writes=['m16b'])
            S.op('dve', lambda e: e.max_index(out=i16[:, hp, 0:8], in_max=m16[:, hp, 0:8], in_values=sc[:, blk]), reads=[sk, 'm16a'], writes=['i16'])
            S.op('dve', lambda e: e.max_index(out=i16[:, hp, 8:16], in_max=m16[:, hp, 8:16], in_values=sc[:, blk]), reads=[sk, 'm16b'], writes=['i16'])
        S.op('dve', lambda e: e.tensor_copy(out=i16f[:], in_=i16[:]), reads=['i16'], writes=['i16f'])
        m16v = m16[:].rearrange("p (h two) k -> p h two k", two=2)
        i16v = i16f[:].rearrange("p (h two) k -> p h two k", two=2)
        S.op('dve', lambda e: e.tensor_tensor(out=v4(cand[:]), in0=bc(m16v[:, :, 0, :], 3, 16), in1=bc(m16v[:, :, 1, :], 2, 16), op=ALU.add),
             reads=['m16a', 'm16b', 'wk'], writes=['cand'])
        for h in range(8):
            blk = slice(h * 256, (h + 1) * 256)
            S.op('dve', lambda e: e.max(out=c16[:, h, 0:8], in_=cand[:, blk]), reads=['cand'], writes=['c16a'])
            S.op('dve', lambda e: e.match_replace(out=junk[:, blk], in_to_replace=c16[:, h, 0:8], in_values=cand[:, blk], imm_value=-1e30), reads=['cand', 'c16a'] + ['sc%d' % i for i in range(4)], writes=['junk'])
            S.op('dve', lambda e: e.max(out=c16[:, h, 8:16], in_=junk[:, blk]), reads=['junk'], writes=['c16b'])
            S.op('dve', lambda e: e.max_index(out=p16[:, h, 0:8], in_max=c16[:, h, 0:8], in_values=cand[:, blk]), reads=['cand', 'c16a'], writes=['p16'])
            S.op('dve', lambda e: e.max_index(out=p16[:, h, 8:16], in_max=c16[:, h, 8:16], in_values=cand[:, blk]), reads=['cand', 'c16b'], writes=['p16'])
        S.op('dve', lambda e: e.tensor_single_scalar(out=pa[:], in_=p16[:], scalar=4, op=ALU.logical_shift_right), reads=['p16'], writes=['pa'])
        S.op('dve', lambda e: e.tensor_single_scalar(out=pb[:], in_=p16[:], scalar=15, op=ALU.bitwise_and), reads=['p16'], writes=['pb'])
        S.op('dve', lambda e: e.tensor_copy(out=paf[:], in_=pa[:]), reads=['pa'], writes=['paf'])
        S.op('dve', lambda e: e.tensor_copy(out=pbf[:], in_=pb[:]), reads=['pb'], writes=['pbf'])
        iob = bc(bc(iot[:], 1, 16), 1, 8)
        for w_, (pf, pk) in enumerate(((paf, 'paf'), (pbf, 'pbf'))):
            S.op('dve', lambda e: e.tensor_tensor(out=v4(junk[:]), in0=bc(pf[:], 3, 16), in1=iob, op=ALU.is_equal), reads=[pk, 'iot', 'junk'], writes=['junk'])
            S.op('dve', lambda e: e.tensor_tensor(out=v4(junk[:]), in0=v4(junk[:]), in1=bc(i16v[:, :, w_, :], 2, 16), op=ALU.mult), reads=['junk', 'i16f'], writes=['junk'])
            S.op('dve', lambda e: e.tensor_reduce(out=sel[:, w_, :], in_=junk[:].rearrange("p (x a) -> p x a", a=16), axis=AX.X, op=ALU.add), reads=['junk'], writes=['sel%d' % w_])
        S.op('dve', lambda e: e.scalar_tensor_tensor(out=idxf[:], in0=sel[:, 0, :], scalar=128.0, in1=sel[:, 1, :], op0=ALU.mult, op1=ALU.add), reads=['sel0', 'sel1'], writes=['idxf'])
        c16f = c16[:]
        S.op('dve', lambda e: e.tensor_tensor(out=gat[:].rearrange("p (h k) -> p h k", k=16), in0=c16f, in1=bc(c16[:, :, 0], 2, 16), op=ALU.subtract), reads=['c16a', 'c16b'], writes=['gat'])
        S.op('dve', lambda e: e.tensor_scalar(out=gat[:], in0=gat[:], scalar1=-80.0, scalar2=None, op0=ALU.max), reads=['gat'], writes=['gat'])
        S.op('act', lambda e: e.activation(out=gat[:], in_=gat[:], func=AF.Exp), reads=['gat'], writes=['gat'])
        S.op('dve', lambda e: e.tensor_reduce(out=gs[:, 0, :], in_=gat[:].rearrange("p (h k) -> p h k", k=16), axis=AX.X, op=ALU.add), reads=['gat'], writes=['gs0'])
        S.op('dve', lambda e: e.reciprocal(out=gs[:, 1, :], in_=gs[:, 0, :]), reads=['gs0'], writes=['gs1'])
        S.op('dve', lambda e: e.tensor_tensor(out=gat[:].rearrange("p (h k) -> p h k", k=16), in0=gat[:].rearrange("p (h k) -> p h k", k=16), in1=bc(gs[:, 1, :], 2, 16), op=ALU.mult), reads=['gat', 'gs1'], writes=['gat'])
        S.op('pe', lambda e: e.transpose(pQ[:, 0, 0:128], idxf[:], P.idf[:]), reads=['idxf', 'ident'], writes=['pQ0'])
        S.op('pe', lambda e: e.transpose(pQ[:, 1, 0:128], gat[:], P.idf[:]), reads=['gat', 'ident'], writes=['pQ1'])
        S.op('dve', lambda e: e.tensor_copy(out=idxTi[:], in_=pQ[:, 0, 0:128]), reads=['pQ0'], writes=['idxTi'])
        S.op('act', lambda e: e.activation(out=gateT[:], in_=pQ[:, 1, 0:128], func=AF.Copy), reads=['pQ1'], writes=['gateT'])
        for tk in range(128):
            ug = Ug[tk % NG]; uk = 'Ug%d' % (tk % NG)
            S.dmaf('pool', lambda g: g.indirect_dma_start(out=ug[:], out_offset=None, in_=Ub[:, :], in_offset=bass.IndirectOffsetOnAxis(ap=idxTi[:, tk:tk + 1], axis=0)),
                   reads=['idxTi', 'Ub16'], writes=[uk])
            pb_ = pB[tk % 2]; pbk = pBk[tk % 2]
            for cb in range(2):
                S.op('pe', lambda e: e.matmul(pb_[:, cb * 512:(cb + 1) * 512], lhsT=bc(P.idf[:, tk], 1, 128), rhs=hn[:, cb * 512:(cb + 1) * 512], start=True, stop=True),
                     reads=['ident', 'hn'], writes=[pbk[cb]])
            S.op('dve', lambda e: e.scalar_tensor_tensor(out=ujunk[:], in0=ug[:], scalar=1.0, in1=pb_, op0=ALU.mult, op1=ALU.mult, accum_out=actA[:, tk:tk + 1]),
                 reads=[uk] + pbk, writes=['ujunk', 'actA'])
        S.op('act', lambda e: e.activation(out=Wm[:], in_=actA[:], func=AF.Gelu), reads=['actA'], writes=['Wm'])
        S.op('dve', lambda e: e.tensor_tensor(out=Wm[:], in0=Wm[:], in1=gateT[:], op=ALU.mult), reads=['Wm', 'gateT'], writes=['Wm'])
        for tk in range(128):
            vg = Ug[tk % NG]; vk = 'Ug%d' % (tk % NG)
            S.dmaf('pool', lambda g: g.indirect_dma_start(out=vg[:], out_offset=None, in_=Vb[:, :], in_offset=bass.IndirectOffsetOnAxis(ap=idxTi[:, tk:tk + 1], axis=0)),
                   reads=['idxTi', 'Vb16'], writes=[vk])
            wz = Wz[tk % 3]; wzk = 'Wz%d' % (tk % 3)
            S.op('act', lambda e: e.activation(out=wz[:], in_=zr[:, 127 - tk:255 - tk], func=AF.Copy, scale=Wm[:, tk:tk + 1]), reads=['zr', 'Wm'], writes=[wzk])
            for cb in range(2):
                S.op('pe', lambda e: e.matmul(pOut[:, cb * 512:(cb + 1) * 512], lhsT=wz[:], rhs=vg[:, cb * 512:(cb + 1) * 512], start=(tk == 0), stop=(tk == 127)),
                     reads=[wzk, vk], writes=[pOk[cb]])
        S.op('dve', lambda e: e.tensor_tensor(out=hn[:], in0=pOut, in1=modb[:, 2048:3072], op=ALU.mult), reads=pOk + ['modb', 'hn'], writes=['hn'])
        S.op('pool', lambda e: e.tensor_tensor(out=xt[:], in0=xt[:], in1=hn[:], op=ALU.add), reads=['xt', 'hn'], writes=['xt'])
        if tail == 'hnext':
            S.dma('sp', Xo[rs, :], xt[:], reads=['xt'], writes=['Xo'])
            emit_rmsnorm_mod(P, xt, 'xt', wmodn, modn[:, 0:1024], ['wmodn', 'modn'], hn, 'hn', (hn, ss))
            S.dma('sp', Hn[rs, :], hn[:], reads=['hn'], writes=['Hn'])
        else:
            emit_rmsnorm_mod(P, xt, 'xt', nw, None, ['nw'], hn, 'hn', (hn, ss))
            S.dma('sp', Xo[rs, :], hn[:], reads=['hn'], writes=['Xo'])
    return P


def run_l5b(tail, x1, c, ada_w_i, ada_b_i, norm2_w_i, wq, keys, utab, vtab, ntt=NTT, ncores=NCORES, **kw):
    P = build_l5b(tail, ntt)
    tok = ntt * 128
    keysT = np.ascontiguousarray(keys.reshape(16, 128, 128).transpose(2, 0, 1))
    in_maps = []
    for core in range(ncores):
        b, hf = core // 2, core % 2
        sl = slice(hf * TOK, hf * TOK + tok)
        m = dict(X1=np.ascontiguousarray(x1[b, sl]), cT=cT_layout(c[b][None]), adaw=np.ascontiguousarray(ada_w_i[:, 3072:6144]), adab=rep128(ada_b_i[3072:6144]),
                 n2w=rep128(norm2_w_i), wq=np.ascontiguousarray(wq), keysT=keysT, Utab=np.ascontiguousarray(utab), Vtab=np.ascontiguousarray(vtab))
        if tail == 'hnext':
            m.update(adawn=np.ascontiguousarray(kw['ada_w_n'][:, :2048]), adabn=rep128(kw['ada_b_n'][:2048]), n1wn=rep128(kw['norm1_w_n']))
        else:
            m.update(fnw=rep128(kw['final_norm_w']))
        in_maps.append(m)
    res = P.run(in_maps)
    xo = np.zeros((4, 8192, D), np.float32)
    hn = np.zeros((4, 8192, D), np.float32) if tail == 'hnext' else None
    for core in range(ncores):
        b, hf = core // 2, core % 2
        xo[b, hf * TOK:hf * TOK + tok] = res[core]["Xo"]
        if hn is not None:
            hn[b, hf * TOK:hf * TOK + tok] = res[core]["Hn"]
    return xo, hn


def build_l6():
    P = Prog()
    S = P.S
    nc = P.nc
    HT = P.din("HT", [4, 128, 8192])
    CS = P.din("CS", [2, 128, 512])
    W64 = P.din("W64", [128, 128])
    WB = P.din("WB", [128, 64, 2, 128])
    FM = P.dout("FM", [8192, 512])
    Zs = [nc.dram_tensor("Zs%d" % g, [8192, 512], F32, kind="Internal").ap() for g in range(2)]
    Us = [nc.dram_tensor("Us%d" % g, [128, 128, 256], F32, kind="Internal").ap() for g in range(2)]
    cs = P.sb("cs", [128, 2, 512]); S.dma('sp', cs[:], CS.rearrange("h p n -> p h n"), writes=['cs'])
    w64 = P.sb("w64", [128, 128]); S.dma('sp', w64[:], W64, writes=['w64'])
    pz = [P.ps("pz%d" % i, [128, 512]) for i in range(2)]
    pu = P.ps("pu", [128, 2048])
    py = [P.ps("py%d" % i, [128, 256]) for i in range(2)]
    TB = 2048
    hts = [P.sb("ht%d" % i, [128, 4, TB]) for i in range(2)]
    zts = [P.sb("zt%d" % i, [128, 512]) for i in range(2)]
    it = 0
    for tb in range(8192 // TB):
        ht = hts[tb % 2]; hk = 'ht%d' % (tb % 2)
        S.dma('sp', ht[:], HT[:, :, tb * TB:(tb + 1) * TB].rearrange("c p t -> p c t"), writes=[hk])
        for tt in range(TB // 128):
            for g in range(2):
                p_ = pz[it % 2]; pk = 'pz%d' % (it % 2); zt = zts[it % 2]; zk = 'zt%d' % (it % 2)
                it += 1
                for hf in range(2):
                    S.op('pe', lambda e: e.matmul(p_[:], lhsT=ht[:, 2 * g + hf, tt * 128:(tt + 1) * 128], rhs=cs[:, hf, :], start=(hf == 0), stop=(hf == 1)),
                         reads=[hk, 'cs'], writes=[pk])
                S.op('act' if g == 0 else 'dve', (lambda e: e.activation(out=zt[:], in_=p_[:], func=AF.Copy)) if g == 0 else (lambda e: e.tensor_copy(out=zt[:], in_=p_[:])),
                     reads=[pk], writes=[zk])
                r0 = tb * TB + tt * 128
                S.dma('pool', Zs[g][r0:r0 + 128, :], zt[:], reads=[zk], writes=['Zs%d' % g])
    LB = 8
    zin = [P.sb("zin%d" % i, [128, LB, 256]) for i in range(2)]
    uts = [P.sb("ut%d" % i, [128, LB, 256]) for i in range(2)]
    it = 0
    for g in range(2):
        zv = Zs[g].rearrange("(l1 l2) (ri kc) -> ri l1 l2 kc", l2=128, ri=2)
        uv = Us[g].rearrange("l2 m kc -> m l2 kc")
        for lb in range(128 // LB):
            zi = zin[it % 2]; zk = 'zin%d' % (it % 2); ut = uts[it % 2]; uk = 'ut%d' % (it % 2)
            it += 1
            for ri in range(2):
                S.dma('sp', zi[ri * 64:(ri + 1) * 64, :, :], zv[ri][:, lb * LB:(lb + 1) * LB, :], reads=['Zs%d' % g], writes=[zk + '_%d' % ri])
            for j in range(LB // 2):
                S.op('pe', lambda e: e.matmul(pu[:, j * 512:(j + 1) * 512], lhsT=w64[:], rhs=zi[:, 2 * j:2 * j + 2, :].rearrange("p a b -> p (a b)"), start=True, stop=True),
                     reads=['w64', zk + '_0', zk + '_1'], writes=['pu'])
            S.op('act' if lb % 2 == 0 else 'dve', (lambda e: e.activation(out=ut[:].rearrange("p a b -> p (a b)"), in_=pu[:], func=AF.Copy)) if lb % 2 == 0 else
                 (lambda e: e.tensor_copy(out=ut[:].rearrange("p a b -> p (a b)"), in_=pu[:])), reads=['pu'], writes=[uk])
            S.dma('pool', uv[:, lb * LB:(lb + 1) * LB, :], ut[:], reads=[uk], writes=['Us%d' % g])
    KB = 16
    wbs = [P.sb("wb%d" % i, [128, KB, 2, 128]) for i in range(2)]
    urs = [P.sb("ur%d" % i, [128, 2, KB, 256]) for i in range(2)]
    yts = [P.sb("yt%d" % i, [128, 256]) for i in range(2)]
    fv = FM.rearrange("(k2 k1) c -> k1 k2 c", k1=64)
    it = 0; ib = 0
    for g in range(2):
        for kb in range(64 // KB):
            wb = wbs[ib % 2]; wk_ = 'wb%d' % (ib % 2); ur = urs[ib % 2]; urk = 'ur%d' % (ib % 2)
            ib += 1
            S.dma('sp', wb[:], WB[:, kb * KB:(kb + 1) * KB, :, :], writes=[wk_])
            for ri in range(2):
                S.dma('sp', ur[:, ri, :, :], Us[g][:, ri * 64 + kb * KB:ri * 64 + (kb + 1) * KB, :], reads=['Us%d' % g], writes=[urk + '_%d' % ri])
            for kk in range(KB):
                k1 = kb * KB + kk
                p_ = py[it % 2]; pk = 'py%d' % (it % 2); yt = yts[it % 2]; yk = 'yt%d' % (it % 2)
                it += 1
                for ri in range(2):
                    S.op('pe', lambda e: e.matmul(p_[:], lhsT=wb[:, kk, ri, :], rhs=ur[:, ri, kk, :], start=(ri == 0), stop=(ri == 1)),
                         reads=[wk_, urk + '_0', urk + '_1'], writes=[pk])
                S.op('act' if it % 2 == 0 else 'dve', (lambda e: e.activation(out=yt[:], in_=p_[:], func=AF.Copy)) if it % 2 == 0 else (lambda e: e.tensor_copy(out=yt[:], in_=p_[:])),
                     reads=[pk], writes=[yk])
                S.dma('pool', fv[k1][:, g * 256:(g + 1) * 256], yt[:], reads=[yk], writes=['FM'])
    return P


def l6_consts():
    sc = 1.0 / math.sqrt(8192.0 * 256.0)
    ch = np.arange(256, dtype=np.float64)
    th = 2 * np.pi * np.outer(ch, ch) / 256.0
    CS = np.concatenate([np.cos(th), np.sin(th)], axis=1) * sc
    CS = CS.reshape(2, 128, 512).astype(np.float32)
    l1 = np.arange(64, dtype=np.float64)
    t64 = 2 * np.pi * np.outer(l1, l1) / 64.0
    c, s = np.cos(t64), np.sin(t64)
    W64 = np.block([[c, -s], [-s, -c]]).astype(np.float32)
    l2 = np.arange(128, dtype=np.float64)[:, None, None]
    k1 = np.arange(64, dtype=np.float64)[None, :, None]
    k2 = np.arange(128, dtype=np.float64)[None, None, :]
    thb = 2 * np.pi * (l2 * k2 / 128.0 + l2 * k1 / 8192.0)
    WB = np.stack([np.cos(thb), np.sin(thb)], axis=2).astype(np.float32)
    return CS, W64, np.ascontiguousarray(WB)


def run_l6(h1):
    P = build_l6()
    CS, W64, WB = l6_consts()
    in_maps = []
    for core in range(NCORES):
        b, gp = core // 2, core % 2
        ht = h1[b, :, gp * 512:(gp + 1) * 512].T.reshape(4, 128, 8192)
        in_maps.append(dict(HT=np.ascontiguousarray(ht), CS=CS, W64=W64, WB=WB))
    res = P.run(in_maps)
    out = np.empty((4, 8192, D), np.float32)
    for core in range(NCORES):
        b, gp = core // 2, core % 2
        out[b, :, gp * 512:(gp + 1) * 512] = res[core]["FM"]
    return out


def kernel(x, c, ctx, c_ctx, ada_w, ada_b, norm1_w, norm2_w, w_in, conv_w, a_log, dt_bias, gdn_norm_w,
           lam_q1, lam_k1, lam_q2, lam_k2, subln_w, w_out_ab, w_out_f, peer_wq, peer_keys, peer_u, peer_v, final_norm_w):
    f = lambda a: np.asarray(a, dtype=np.float32)
    x, c, ctx, c_ctx, ada_w, ada_b, norm1_w, norm2_w, w_in, conv_w, a_log, dt_bias, gdn_norm_w = map(
        f, (x, c, ctx, c_ctx, ada_w, ada_b, norm1_w, norm2_w, w_in, conv_w, a_log, dt_bias, gdn_norm_w))
    lam_q1, lam_k1, lam_q2, lam_k2, subln_w, w_out_ab, w_out_f, peer_wq, peer_keys, peer_u, peer_v, final_norm_w = map(
        f, (lam_q1, lam_k1, lam_q2, lam_k2, subln_w, w_out_ab, w_out_f, peer_wq, peer_keys, peer_u, peer_v, final_norm_w))
    Plat, Pctx = run_l1(x, c, ctx, c_ctx, ada_w[0], ada_b[0], norm1_w[0], w_in[0])
    o2 = run_l2(Plat, Pctx, conv_w[0], a_log[0], dt_bias[0])
    O = run_l3(o2['QKV'], o2['BG'])
    dlat = run_l4(o2['QKr'], Plat, Pctx, lam_q1[0], lam_k1[0], lam_q2[0], lam_k2[0])
    del o2
    x1 = run_l5a('ab', x, c, ada_w[0], ada_b[0], w_out_ab[0], O=O, Plat=Plat, dlat=dlat, gdn_norm_w=gdn_norm_w[0], subln_w=subln_w[0])
    del O, dlat, Plat, Pctx
    x2, h1 = run_l5b('hnext', x1, c, ada_w[0], ada_b[0], norm2_w[0], peer_wq[0], peer_keys[0], peer_u[0], peer_v[0],
                     ada_w_n=ada_w[1], ada_b_n=ada_b[1], norm1_w_n=norm1_w[1])
    del x1
    fm = run_l6(h1)
    x3 = run_l5a('f', x2, c, ada_w[1], ada_b[1], w_out_f[0], fm=fm)
    del x2, fm, h1
    zw = np.zeros((D, 2048), np.float32)
    zb = np.zeros((2048,), np.float32)
    _, out = run_l5b('hnext', x3, c, ada_w[1], ada_b[1], norm2_w[1], peer_wq[1], peer_keys[1], peer_u[1], peer_v[1],
                     ada_w_n=zw, ada_b_n=zb, norm1_w_n=final_norm_w)
    return out.astype(np.float32)
```

```python
import math
import numpy as np
import concourse.bass as bass
import concourse.mybir as mybir
from concourse.bass_utils import run_bass_kernel_spmd

F32 = mybir.dt.float32
I32 = mybir.dt.int32
U32 = mybir.dt.uint32
AF = mybir.ActivationFunctionType
ALU = mybir.AluOpType
AX = mybir.AxisListType

NCORES = 8
D = 1024
EPS = 1e-6


class Sched:
    LIMIT = 20000

    def __init__(self, nc):
        self.nc = nc
        self.eng = {'pe': nc.tensor, 'act': nc.scalar, 'dve': nc.vector, 'pool': nc.gpsimd, 'sp': nc.sync}
        self.epoch = {k: 0 for k in self.eng}
        self.sem = {(k, 0): nc.alloc_semaphore('s_%s_0' % k) for k in self.eng}
        self.cnt = {k: 0 for k in self.eng}
        self.seen = {k: {} for k in self.eng}
        self.ndsem = 24
        self.dsem = [nc.alloc_semaphore('d_%d' % i) for i in range(self.ndsem)]
        self.dcnt = [0] * self.ndsem
        self.dnext = 0
        self.lastw = {}
        self.readers = {}
        self.dwr = {}
        self.ninst = 0

    def _wait(self, e, tok, kindw):
        kind, key, val = tok
        if kind == 'e':
            src = key[0]
            if src == e and (e == 'pe' or kindw != 'raw'):
                return
        seen = self.seen[e]
        k = (kind, key)
        if seen.get(k, 0) >= val:
            return
        sem = self.sem[key] if kind == 'e' else self.dsem[key]
        self.eng[e].wait_ge(sem, val)
        seen[k] = val

    def _deps(self, e, reads, writes):
        for b in reads:
            t = self.lastw.get(b)
            if t is not None:
                self._wait(e, t, 'raw')
            for i, v in self.dwr.get(b, {}).items():
                self._wait(e, ('d', i, v), 'raw')
        for b in writes:
            t = self.lastw.get(b)
            if t is not None:
                self._wait(e, t, 'waw')
            for t in self.readers.get(b, ()):
                self._wait(e, t, 'war')

    def _commit(self, tok, reads, writes):
        for b in writes:
            self.lastw[b] = tok
            self.readers[b] = []
            if tok[0] == 'd':
                self.dwr.setdefault(b, {})[tok[1]] = tok[2]
            else:
                self.dwr.pop(b, None)
        for b in reads:
            if b in writes:
                continue
            self.readers.setdefault(b, []).append(tok)

    def op(self, e, fn, reads=(), writes=()):
        self._deps(e, reads, writes)
        ins = fn(self.eng[e])
        if self.cnt[e] >= self.LIMIT:
            self.epoch[e] += 1
            self.cnt[e] = 0
            self.sem[(e, self.epoch[e])] = self.nc.alloc_semaphore('s_%s_%d' % (e, self.epoch[e]))
        self.cnt[e] += 1
        key = (e, self.epoch[e])
        ins.then_inc(self.sem[key], 1)
        tok = ('e', key, self.cnt[e])
        self._commit(tok, reads, writes)
        self.ninst += 1
        return tok

    def dma(self, e, out, in_, reads=(), writes=(), **kw):
        return self.dmaf(e, lambda g: g.dma_start(out=out, in_=in_, **kw), reads, writes)

    def dmaf(self, e, fn, reads=(), writes=()):
        i = self.dnext
        self.dnext = (self.dnext + 1) % self.ndsem
        if self.dcnt[i] > 0:
            self._wait(e, ('d', i, self.dcnt[i]), 'raw')
        self._deps(e, reads, writes)
        ins = fn(self.eng[e])
        self.dcnt[i] += 16
        ins.then_inc(self.dsem[i], 16)
        tok = ('d', i, self.dcnt[i])
        self._commit(tok, reads, writes)
        self.ninst += 1
        return tok

    def finish(self, bufs, e='sp'):
        for b in bufs:
            t = self.lastw.get(b)
            if t is not None:
                self._wait(e, t, 'raw')
        for i in range(self.ndsem):
            if self.dcnt[i] > 0:
                self._wait(e, ('d', i, self.dcnt[i]), 'raw')


class Prog:
    def __init__(self):
        self.nc = bass.Bass("TRN2", target_bir_lowering=False)
        self.S = Sched(self.nc)
        self.outs = []
        self._n = 0

    def din(self, name, shape, dt=F32):
        return self.nc.dram_tensor(name, list(shape), dt, kind="ExternalInput").ap()

    def dout(self, name, shape, dt=F32):
        self.outs.append(name)
        return self.nc.dram_tensor(name, list(shape), dt, kind="ExternalOutput").ap()

    def sb(self, name, shape, dt=F32):
        return self.nc.alloc_sbuf_tensor(name, list(shape), dt)

    def ps(self, name, shape):
        return self.nc.alloc_psum_tensor(name, list(shape), F32)

    def ident(self):
        idf = self.sb("ident", [128, 128])
        S = self.S
        S.op('pool', lambda g: g.memset(idf[:], 1.0), writes=['ident'])
        S.op('pool', lambda g: g.affine_select(out=idf[:], in_=idf[:], pattern=[[-1, 128]], compare_op=ALU.is_equal,
                                               fill=0.0, base=0, channel_multiplier=1), reads=['ident'], writes=['ident'])
        return idf

    def run(self, in_maps):
        self.S.finish(self.outs)
        res = run_bass_kernel_spmd(self.nc, in_maps, core_ids=list(range(len(in_maps))))
        return res.results


def emit_mod(P, csil, v, adaw, adab, col0, ncols, outt, key, ones, tmpname, BW=256, pmx=None):
    S = P.S
    if not hasattr(P, '_modtmp'):
        P._modtmp = {}
    if tmpname not in P._modtmp:
        P._modtmp[tmpname] = (P.sb(tmpname + "_lt", [128, 8, 128]), [P.sb(tmpname + "_w%d" % i, [128, 8, BW]) for i in range(2)],
                              [P.ps(tmpname + "_pm%d" % i, [128, BW]) for i in range(2)] if pmx is None else None, P.sb(tmpname + "_b", [128, getattr(P, "mod_bw", 2048)]))
    lt, wts, pm, bt = P._modtmp[tmpname]
    pmk = [tmpname + '_pm0', tmpname + '_pm1']
    if pmx is not None:
        pm, pmk = pmx
    for k in range(8):
        S.op('dve', lambda e: e.tensor_scalar(out=lt[:, k, :], in0=ones[:, 0:128], scalar1=csil[:, v, k:k + 1], scalar2=None,
                                              op0=ALU.mult), reads=['csil', 'ones'], writes=[tmpname + '_lt%d' % k])
    wv = adaw.rearrange("(k p) n -> p k n", p=128)
    nb = (ncols + BW - 1) // BW
    S.dma('sp', bt[:, 0:ncols], adab[:, col0:col0 + ncols], writes=[tmpname + '_b'])
    for j in range(nb):
        c0 = col0 + j * BW
        w = min(BW, col0 + ncols - c0)
        wt = wts[j % 2]
        wk = tmpname + '_w%d' % (j % 2)
        S.dma('sp', wt[:, :, 0:w], wv[:, :, c0:c0 + w], writes=[wk])
        pk = pmk[j % 2]
        for k in range(8):
            S.op('pe', lambda e: e.matmul(pm[j % 2][:, 0:w], lhsT=lt[:, k, :], rhs=wt[:, k, 0:w], start=(k == 0), stop=(k == 7)),
                 reads=[wk, tmpname + '_lt%d' % k], writes=[pk])
        S.op('dve', lambda e: e.tensor_tensor(out=outt[:, j * BW:j * BW + w], in0=pm[j % 2][:, 0:w], in1=bt[:, j * BW:j * BW + w],
                                              op=ALU.add), reads=[pk, tmpname + '_b'], writes=[key])


def emit_rmsnorm_mod(P, xt, xkey, wmod, shb, modkeys, outt, okey, tmp):
    S = P.S
    junk, ss = tmp
    S.op('act', lambda e: e.activation(out=junk[:], in_=xt[:], func=AF.Square, accum_out=ss[:, 0:1]), reads=[xkey], writes=[okey, 'ss'])
    S.op('act', lambda e: e.activation(out=ss[:, 1:2], in_=ss[:, 0:1], func=AF.Sqrt, bias=P.epsb[:, 0:1], scale=1.0 / D), reads=['ss', 'epsb'], writes=['ss1'])
    S.op('dve', lambda e: e.reciprocal(out=ss[:, 2:3], in_=ss[:, 1:2]), reads=['ss1'], writes=['ss2'])
    S.op('dve', lambda e: e.scalar_tensor_tensor(out=outt[:], in0=xt[:], scalar=ss[:, 2:3], in1=wmod[:], op0=ALU.mult, op1=ALU.mult),
         reads=[xkey, 'ss2'] + modkeys, writes=[okey])
    if shb is not None:
        S.op('pool', lambda e: e.tensor_tensor(out=outt[:], in0=outt[:], in1=shb[:], op=ALU.add), reads=[okey] + modkeys, writes=[okey])


def consts(P):
    S = P.S
    P.ones = P.sb("ones", [128, 512])
    S.op('pool', lambda g: g.memset(P.ones[:], 1.0), writes=['ones'])
    P.epsb = P.sb("epsb", [128, 1])
    S.op('pool', lambda g: g.memset(P.epsb[:], EPS), writes=['epsb'])
    P.idf = P.ident()


L1_TILES = 33
IN_W = 3616


def build_l1():
    P = Prog()
    S = P.S
    X = P.din("X", [L1_TILES * 128, D])
    cT = P.din("cT", [128, 2, 8])
    adaw = P.din("adaw", [D, 2048])
    adab = P.din("adab", [128, 2048])
    n1w = P.din("n1w", [128, D])
    win = P.din("win", [D, IN_W])
    Pout = P.dout("P", [L1_TILES * 128, IN_W])
    consts(P)
    csil = P.sb("csil", [128, 2, 8])
    S.dma('sp', csil[:], cT, writes=['csil'])
    S.op('act', lambda e: e.activation(out=csil[:], in_=csil[:], func=AF.Silu), reads=['csil'], writes=['csil'])
    n1wt = P.sb("n1wt", [128, D])
    S.dma('sp', n1wt[:], n1w, writes=['n1w'])
    wint = P.sb("wint", [128, 8, IN_W])
    winv = win.rearrange("(k p) n -> p k n", p=128)
    for k in range(8):
        S.dma('sp', wint[:, k, :], winv[:, k, :], writes=['win%d' % k])
    mods = []
    for v in range(2):
        mb = P.sb("modb%d" % v, [128, 2048])
        emit_mod(P, csil, v, adaw, adab, 0, 2048, mb, 'modb%d' % v, P.ones, "m")
        wm = P.sb("wmod%d" % v, [128, D])
        S.op('dve', lambda e: e.scalar_tensor_tensor(out=wm[:], in0=mb[:, 1024:2048], scalar=1.0, in1=n1wt[:], op0=ALU.add, op1=ALU.mult),
             reads=['modb%d' % v, 'n1w'], writes=['wmod%d' % v])
        mods.append((wm, mb))
    xts = [P.sb("xt%d" % i, [128, D]) for i in range(2)]
    ss = P.sb("ss", [128, 4])
    ht = P.sb("ht", [128, D])
    junk = ht
    hT = P.sb("hT", [128, D])
    pT = P.ps("pT", [128, D])
    pp = [P.ps("pp%d" % i, [128, 512]) for i in range(4)]
    pts = [P.sb("pt0", [128, IN_W])] * 2
    for t in range(L1_TILES):
        v = 1 if t == L1_TILES - 1 else 0
        xt = xts[t % 2]
        xk = 'xt%d' % (t % 2)
        S.dma('sp', xt[:], X[t * 128:(t + 1) * 128, :], writes=[xk])
        wm, mb = mods[v]
        emit_rmsnorm_mod(P, xt, xk, wm, mb[:, 0:1024], ['wmod%d' % v, 'modb%d' % v], ht, 'ht', (junk, ss))
        for k in range(8):
            S.op('pe', lambda e: e.transpose(pT[:, k * 128:(k + 1) * 128], ht[:, k * 128:(k + 1) * 128], P.idf[:]),
                 reads=['ht', 'ident'], writes=['pT%d' % k])
        S.op('act', lambda e: e.activation(out=hT[:, 0:512], in_=pT[:, 0:512], func=AF.Copy), reads=['pT%d' % k for k in range(4)], writes=['hT0'])
        S.op('dve', lambda e: e.tensor_copy(out=hT[:, 512:1024], in_=pT[:, 512:1024]), reads=['pT%d' % k for k in range(4, 8)], writes=['hT1'])
        pt = pts[t % 2]
        ptk = 'pt0'
        ncb = (IN_W + 511) // 512
        for cb in range(ncb):
            c0 = cb * 512
            w = min(512, IN_W - c0)
            pq = pp[cb % 4]
            for k in range(8):
                S.op('pe', lambda e: e.matmul(pq[:, 0:w], lhsT=hT[:, k * 128:(k + 1) * 128], rhs=wint[:, k, c0:c0 + w], start=(k == 0), stop=(k == 7)),
                     reads=['hT%d' % (k // 4), 'win%d' % k], writes=['pp%d' % (cb % 4)])
            if cb % 2 == 0:
                S.op('act', lambda e: e.activation(out=pt[:, c0:c0 + w], in_=pq[:, 0:w], func=AF.Copy), reads=['pp%d' % (cb % 4)], writes=[ptk + '_%d' % cb])
            else:
                S.op('dve', lambda e: e.tensor_copy(out=pt[:, c0:c0 + w], in_=pq[:, 0:w]), reads=['pp%d' % (cb % 4)], writes=[ptk + '_%d' % cb])
        S.dma('pool', Pout[t * 128:(t + 1) * 128, :], pt[:], reads=[ptk + '_%d' % cb for cb in range(ncb)], writes=['P'])
    return P


def rep128(v):
    v = np.asarray(v, np.float32).reshape(1, -1)
    return np.ascontiguousarray(np.broadcast_to(v, (128, v.shape[1])))


def cT_layout(vecs):
    vecs = np.asarray(vecs, np.float32)
    return np.ascontiguousarray(vecs.reshape(vecs.shape[0], 8, 128).transpose(2, 0, 1))


def run_l1(x, c, ctx, c_ctx, ada_w0, ada_b0, norm1_w0, w_in0):
    P = build_l1()
    in_maps = []
    for core in range(NCORES):
        b, hf = core // 2, core % 2
        X = np.concatenate([x[b, hf * 4096:(hf + 1) * 4096], ctx[b, hf * 128:(hf + 1) * 128]], axis=0)
        in_maps.append(dict(X=np.ascontiguousarray(X), cT=cT_layout(np.stack([c[b], c_ctx])),
                            adaw=np.ascontiguousarray(ada_w0[:, :2048]), adab=rep128(ada_b0[:2048]),
                            n1w=rep128(norm1_w0), win=np.ascontiguousarray(w_in0)))
    res = P.run(in_maps)
    Plat = np.empty((4, 8192, IN_W), np.float32)
    Pctx = np.empty((4, 256, IN_W), np.float32)
    for core in range(NCORES):
        b, hf = core // 2, core % 2
        r = res[core]["P"]
        Plat[b, hf * 4096:(hf + 1) * 4096] = r[:4096]
        Pctx[b, hf * 128:(hf + 1) * 128] = r[4096:]
    return Plat, Pctx


def bc(ap, axis, n):
    a = ap.unsqueeze(axis)
    shp = list(a.shape)
    shp[axis] = n
    return a.broadcast_to(shp)


def build_l2():
    P = Prog()
    S = P.S
    NT = L1_TILES
    R = NT * 128
    Pp = P.din("Pp", [R, 1536]); Pc = P.din("Pc", [R, 1536]); Pn = P.din("Pn", [R, 1536])
    Pab = P.din("Pab", [R, 32]); Pqk = P.din("Pqk", [R, 1024])
    cosT = P.din("cosT", [R, 32]); sinT = P.din("sinT", [R, 32])
    convw = P.din("convw", [128, 3, 1536]); alog = P.din("alog", [128, 16]); dtb = P.din("dtb", [128, 16])
    QKV = P.dout("QKV", [R, 1536]); BG = P.dout("BG", [R, 32]); QKr = P.dout("QKr", [R, 1024])
    consts(P)
    cw = P.sb("cw", [128, 3, 1536]); S.dma('sp', cw[:], convw, writes=['cw'])
    negA = P.sb("negA", [128, 16]); S.dma('sp', negA[:], alog, writes=['negA'])
    S.op('act', lambda e: e.activation(out=negA[:], in_=negA[:], func=AF.Exp), reads=['negA'], writes=['negA'])
    S.op('dve', lambda e: e.tensor_scalar(out=negA[:], in0=negA[:], scalar1=-1.0, scalar2=None, op0=ALU.mult), reads=['negA'], writes=['negA'])
    dtbt = P.sb("dtbt", [128, 16]); S.dma('sp', dtbt[:], dtb, writes=['dtbt'])
    a0 = P.sb("a0", [128, 1536]); a1 = P.sb("a1", [128, 1536]); a2 = P.sb("a2", [128, 1536])
    qkv = P.sb("qkv", [128, 1536]); sq = P.sb("sq", [128, 1024]); st = P.sb("st", [128, 3, 16])
    ab = P.sb("ab", [128, 32]); bg = P.sb("bg", [128, 32]); tt = P.sb("tt", [128, 16])
    qk = P.sb("qk", [128, 1024]); qo = P.sb("qo", [128, 1024]); cs = P.sb("cs", [128, 2, 32])
    r0 = P.sb("r0", [128, 512]); r1 = P.sb("r1", [128, 512])
    for t in range(NT):
        rs = slice(t * 128, (t + 1) * 128)
        S.dma('sp', a0[:], Pp[rs, :], writes=['a0']); S.dma('sp', a1[:], Pc[rs, :], writes=['a1']); S.dma('sp', a2[:], Pn[rs, :], writes=['a2'])
        S.dma('sp', ab[:], Pab[rs, :], writes=['ab']); S.dma('sp', qk[:], Pqk[rs, :], writes=['qk'])
        S.dma('sp', cs[:, 0, :], cosT[rs, :], writes=['cs0']); S.dma('sp', cs[:, 1, :], sinT[rs, :], writes=['cs1'])
        S.op('dve', lambda e: e.tensor_tensor(out=a0[:], in0=a0[:], in1=cw[:, 0, :], op=ALU.mult), reads=['a0', 'cw'], writes=['a0'])
        S.op('pool', lambda e: e.tensor_tensor(out=a1[:], in0=a1[:], in1=cw[:, 1, :], op=ALU.mult), reads=['a1', 'cw'], writes=['a1'])
        S.op('dve', lambda e: e.tensor_tensor(out=a2[:], in0=a2[:], in1=cw[:, 2, :], op=ALU.mult), reads=['a2', 'cw'], writes=['a2'])
        S.op('pool', lambda e: e.tensor_tensor(out=a1[:], in0=a1[:], in1=a0[:], op=ALU.add), reads=['a1', 'a0'], writes=['a1'])
        S.op('dve', lambda e: e.tensor_tensor(out=a1[:], in0=a1[:], in1=a2[:], op=ALU.add), reads=['a1', 'a2'], writes=['a1'])
        S.op('act', lambda e: e.activation(out=qkv[:], in_=a1[:], func=AF.Silu), reads=['a1'], writes=['qkv'])
        S.op('act', lambda e: e.activation(out=sq[:], in_=qkv[:, 0:1024], func=AF.Square), reads=['qkv'], writes=['sq'])
        S.op('dve', lambda e: e.tensor_reduce(out=st[:, 0, :], in_=sq[:].rearrange("p (g d) -> p g d", d=64), axis=AX.X, op=ALU.add), reads=['sq'], writes=['st0'])
        S.op('act', lambda e: e.activation(out=st[:, 1, :], in_=st[:, 0, :], func=AF.Sqrt, bias=P.epsb[:, 0:1], scale=1.0), reads=['st0', 'epsb'], writes=['st1'])
        S.op('dve', lambda e: e.reciprocal(out=st[:, 2, :], in_=st[:, 1, :]), reads=['st1'], writes=['st2'])
        S.op('dve', lambda e: e.tensor_scalar(out=st[:, 2, 0:8], in0=st[:, 2, 0:8], scalar1=0.125, scalar2=None, op0=ALU.mult), reads=['st2'], writes=['st2'])
        S.op('dve', lambda e: e.tensor_tensor(out=qkv[:, 0:1024].rearrange("p (g d) -> p g d", d=64), in0=qkv[:, 0:1024].rearrange("p (g d) -> p g d", d=64),
                                              in1=bc(st[:, 2, :], 2, 64), op=ALU.mult), reads=['qkv', 'st2'], writes=['qkv'])
        S.dma('pool', QKV[rs, :], qkv[:], reads=['qkv'], writes=['QKV'])
        S.op('act', lambda e: e.activation(out=bg[:, 0:16], in_=ab[:, 0:16], func=AF.Sigmoid), reads=['ab'], writes=['bg0'])
        S.op('dve', lambda e: e.tensor_tensor(out=tt[:], in0=ab[:, 16:32], in1=dtbt[:], op=ALU.add), reads=['ab', 'dtbt'], writes=['tt'])
        S.op('act', lambda e: e.activation(out=tt[:], in_=tt[:], func=AF.Exp), reads=['tt'], writes=['tt'])
        S.op('act', lambda e: e.activation(out=tt[:], in_=tt[:], func=AF.Ln, bias=P.ones[:, 0:1], scale=1.0), reads=['tt', 'ones'], writes=['tt'])
        S.op('dve', lambda e: e.tensor_tensor(out=bg[:, 16:32], in0=tt[:], in1=negA[:], op=ALU.mult), reads=['tt', 'negA'], writes=['bg1'])
        S.dma('pool', BG[rs, :], bg[:], reads=['bg0', 'bg1'], writes=['BG'])
        v = qk[:].rearrange("p (g h d) -> p g h d", h=2, d=32)
        o = qo[:].rearrange("p (g h d) -> p g h d", h=2, d=32)
        cb = bc(cs[:, 0, :], 1, 16); sb_ = bc(cs[:, 1, :], 1, 16)
        r0v = r0[:].rearrange("p (g d) -> p g d", d=32); r1v = r1[:].rearrange("p (g d) -> p g d", d=32)
        S.op('dve', lambda e: e.tensor_tensor(out=r0v, in0=v[:, :, 0, :], in1=cb, op=ALU.mult), reads=['qk', 'cs0'], writes=['r0'])
        S.op('pool', lambda e: e.tensor_tensor(out=r1v, in0=v[:, :, 1, :], in1=sb_, op=ALU.mult), reads=['qk', 'cs1'], writes=['r1'])
        S.op('dve', lambda e: e.tensor_tensor(out=o[:, :, 0, :], in0=r0v, in1=r1v, op=ALU.subtract), reads=['r0', 'r1'], writes=['qo0'])
        S.op('pool', lambda e: e.tensor_tensor(out=r0v, in0=v[:, :, 0, :], in1=sb_, op=ALU.mult), reads=['qk', 'cs1', 'r0'], writes=['r0'])
        S.op('dve', lambda e: e.tensor_tensor(out=r1v, in0=v[:, :, 1, :], in1=cb, op=ALU.mult), reads=['qk', 'cs0', 'r1'], writes=['r1'])
        S.op('pool', lambda e: e.tensor_tensor(out=o[:, :, 1, :], in0=r0v, in1=r1v, op=ALU.add), reads=['r0', 'r1'], writes=['qo1'])
        S.dma('pool', QKr[rs, :], qo[:], reads=['qo0', 'qo1'], writes=['QKr'])
    return P


def rope_tables():
    rows = 8192 // 64
    row = np.repeat(np.arange(rows, dtype=np.float32), 64)
    col = np.tile(np.arange(64, dtype=np.float32), rows)
    inv = (10000.0 ** (-np.arange(0, 32, 2, dtype=np.float32) / 32)).astype(np.float32)
    ang = np.concatenate([row[:, None] * inv, col[:, None] * inv], axis=-1).astype(np.float32)
    return np.cos(ang).astype(np.float32), np.sin(ang).astype(np.float32)


def shift_rows(a, s):
    b = np.zeros_like(a)
    if s == -1:
        b[..., 1:, :] = a[..., :-1, :]
    else:
        b[..., :-1, :] = a[..., 1:, :]
    return b


def run_l2(Plat, Pctx, conv_w0, a_log0, dt_bias0):
    P = build_l2()
    cos, sin = rope_tables()
    in_maps = []
    for core in range(NCORES):
        b, hf = core // 2, core % 2
        sl, sc = slice(hf * 4096, (hf + 1) * 4096), slice(hf * 128, (hf + 1) * 128)
        cat = lambda A, B: np.ascontiguousarray(np.concatenate([A, B], axis=0))
        ql, qc = Plat[b, :, :1536], Pctx[b, :, :1536]
        in_maps.append(dict(
            Pp=cat(shift_rows(ql, -1)[sl], shift_rows(qc, -1)[sc]), Pc=cat(ql[sl], qc[sc]), Pn=cat(shift_rows(ql, 1)[sl], shift_rows(qc, 1)[sc]),
            Pab=cat(Plat[b, sl, 2048:2080], Pctx[b, sc, 2048:2080]), Pqk=cat(Plat[b, sl, 2080:3104], Pctx[b, sc, 2080:3104]),
            cosT=cat(cos[sl], np.ones((128, 32), np.float32)), sinT=cat(sin[sl], np.zeros((128, 32), np.float32)),
            convw=np.ascontiguousarray(np.broadcast_to(conv_w0[None], (128, 3, 1536))), alog=rep128(a_log0.reshape(-1)), dtb=rep128(dt_bias0.reshape(-1))))
    res = P.run(in_maps)
    out = {}
    for name, w in (("QKV", 1536), ("BG", 32), ("QKr", 1024)):
        lat = np.empty((4, 8192, w), np.float32); cx = np.empty((4, 256, w), np.float32)
        for core in range(NCORES):
            b, hf = core // 2, core % 2
            r = res[core][name]
            lat[b, hf * 4096:(hf + 1) * 4096] = r[:4096]
            cx[b, hf * 128:(hf + 1) * 128] = r[4096:]
        out[name] = (lat, cx)
    return out


GD_CH = 132
GD_CTX = 4


import os
LIM = int(os.environ.get('LIM', '99'))


def build_l3(nch=GD_CH, nctx=GD_CTX, stage=9):
    P = Prog()
    S = P.S
    T = nch * 64
    Kt = P.din("Kt", [T, 512]); Vt = P.din("Vt", [T, 512])
    KT = P.din("KT", [nch, 64, 512]); QT = P.din("QT", [nch, 64, 512])
    Bt = P.din("Bt", [T, 8]); Gt = P.din("Gt", [T, 8])
    O = P.dout("O", [(nch - nctx) * 64, 512])
    N = 64

    def mask(name, op, sgn=1):
        m = P.sb(name, [N, N])
        S.op('pool', lambda g: g.memset(m[:], 1.0), writes=[name])
        S.op('pool', lambda g: g.affine_select(out=m[:], in_=m[:], pattern=[[-sgn, N]], compare_op=op, fill=0.0, base=0, channel_multiplier=sgn),
             reads=[name], writes=[name])
        return m
    mL = mask("mL", ALU.is_ge); mLs = mask("mLs", ALU.is_gt); mU = mask("mU", ALU.is_ge, -1); mUs = mask("mUs", ALU.is_gt, -1); I64 = mask("I64", ALU.is_equal)
    ones = P.sb("ones64", [N, N]); S.op('pool', lambda g: g.memset(ones[:], 1.0), writes=['ones64'])
    B = [P.ps("b%d" % i, [N, 512]) for i in range(8)]
    w3 = lambda t: t[:].rearrange("p (h j) -> p h j", j=64)
    sbw = lambda name: P.sb(name, [N, 512])
    kt = sbw("kt"); vt = sbw("vt"); kT = sbw("kT"); qT = sbw("qT")
    b8 = P.sb("b8", [N, 8]); g8 = P.sb("g8", [N, 8]); gc = P.sb("gc", [N, 8]); egc = P.sb("egc", [N, 8]); nb8 = P.sb("nb8", [N, 8]); bge = P.sb("bge", [N, 8])
    R = sbw("R"); arg = sbw("arg"); expgB = sbw("expgB"); t1 = sbw("t1"); t2 = sbw("t2")
    Dls = sbw("Dls"); Dus = sbw("Dus"); Du = sbw("Du"); E2 = sbw("E2")
    Q = [sbw("Q0"), sbw("Q1")]; QTt = [sbw("QT0"), sbw("QT1")]; PT = [sbw("PT0"), sbw("PT1")]
    AT = sbw("AT"); vb = sbw("vb"); kbg = sbw("kbg"); U = sbw("U"); WT = sbw("WT"); ktl = sbw("ktl"); qdT = sbw("qdT")
    vnew = sbw("vnew"); Sst = sbw("Sst"); ot = sbw("ot"); stmp = sbw("stmp")
    S.op('pool', lambda g: g.memset(Sst[:], 0.0), writes=['Sst'])
    hs = lambda t, h: t[:, h * 64:(h + 1) * 64]

    for c in range(nch):
        rs = slice(c * 64, (c + 1) * 64)
        S.dma('sp', kt[:], Kt[rs, :], writes=['kt']); S.dma('sp', vt[:], Vt[rs, :], writes=['vt'])
        S.dma('sp', kT[:], KT[c], writes=['kT']); S.dma('sp', qT[:], QT[c], writes=['qT'])
        S.dma('sp', b8[:], Bt[rs, :], writes=['b8']); S.dma('sp', g8[:], Gt[rs, :], writes=['g8'])
        if stage < -3: continue
        S.op('dve', lambda e: e.tensor_tensor(out=w3(R), in0=bc(g8[:], 2, 64), in1=bc(mU[:], 1, 8), op=ALU.mult), reads=['g8', 'mU'], writes=['R'])
        S.op('pe', lambda e: e.matmul(B[0][:], lhsT=ones[:], rhs=R[:], start=True, stop=True), reads=['ones64', 'R'], writes=['b0'])
        if stage < -2: continue
        S.op('pe', lambda e: e.matmul(B[4][:, 0:8], lhsT=mU[:], rhs=g8[:], start=True, stop=True), reads=['mU', 'g8'], writes=['b4'])
        S.op('act', lambda e: e.activation(out=gc[:], in_=B[4][:, 0:8], func=AF.Copy), reads=['b4'], writes=['gc'])
        if stage < -1: continue
        if LIM > 0:
            S.op('dve', lambda e: e.tensor_tensor(out=w3(arg), in0=bc(gc[:], 2, 64), in1=w3(B[0]), op=ALU.subtract), reads=['gc', 'b0'], writes=['arg'])
        if LIM > 1:
            S.op('dve', lambda e: e.tensor_scalar(out=expgB[:], in0=B[0][:], scalar1=-80.0, scalar2=None, op0=ALU.max), reads=['b0'], writes=['expgB'])
            S.op('act', lambda e: e.activation(out=expgB[:], in_=expgB[:], func=AF.Exp), reads=['expgB'], writes=['expgB'])
        if LIM > 2:
            S.op('dve', lambda e: e.tensor_scalar(out=egc[:], in0=gc[:], scalar1=-80.0, scalar2=None, op0=ALU.max), reads=['gc'], writes=['egc'])
            S.op('act', lambda e: e.activation(out=egc[:], in_=egc[:], func=AF.Exp), reads=['egc'], writes=['egc'])
        if LIM > 3:
            S.op('dve', lambda e: e.tensor_scalar(out=t1[:], in0=arg[:], scalar1=0.0, scalar2=-80.0, op0=ALU.min, op1=ALU.max), reads=['arg'], writes=['t1'])
        if LIM > 4:
            S.op('act', lambda e: e.activation(out=t1[:], in_=t1[:], func=AF.Exp), reads=['t1'], writes=['t1'])
        if LIM > 5:
            S.op('dve', lambda e: e.tensor_tensor(out=w3(Dls), in0=w3(t1), in1=bc(mLs[:], 1, 8), op=ALU.mult), reads=['t1', 'mLs'], writes=['Dls'])
        if LIM > 6:
            S.op('dve', lambda e: e.tensor_scalar(out=t2[:], in0=arg[:], scalar1=-1.0, scalar2=0.0, op0=ALU.mult, op1=ALU.min), reads=['arg'], writes=['t2'])
            S.op('dve', lambda e: e.tensor_scalar(out=t2[:], in0=t2[:], scalar1=-80.0, scalar2=None, op0=ALU.max), reads=['t2'], writes=['t2'])
        if LIM > 7:
            S.op('act', lambda e: e.activation(out=E2[:], in_=t2[:], func=AF.Exp), reads=['t2'], writes=['E2'])
        if LIM > 8:
            S.op('dve', lambda e: e.tensor_tensor(out=w3(Dus), in0=w3(E2), in1=bc(mUs[:], 1, 8), op=ALU.mult), reads=['E2', 'mUs'], writes=['Dus'])
        if LIM > 9:
            S.op('pool', lambda e: e.tensor_tensor(out=w3(Du), in0=w3(E2), in1=bc(mU[:], 1, 8), op=ALU.mult), reads=['E2', 'mU'], writes=['Du'])
        if stage < 1: continue
        S.op('dve', lambda e: e.tensor_tensor(out=w3(R), in0=bc(b8[:], 2, 64), in1=bc(I64[:], 1, 8), op=ALU.mult), reads=['b8', 'I64', 'R'], writes=['R'])
        S.op('pe', lambda e: e.matmul(B[1][:], lhsT=ones[:], rhs=R[:], start=True, stop=True), reads=['ones64', 'R'], writes=['b1'])
        S.op('dve', lambda e: e.tensor_scalar(out=nb8[:], in0=b8[:], scalar1=-1.0, scalar2=None, op0=ALU.mult), reads=['b8'], writes=['nb8'])
        if stage < 2: continue
        for h in range(8):
            S.op('pe', lambda e: e.matmul(hs(B[2], h), lhsT=hs(kT, h), rhs=hs(kT, h), start=True, stop=True), reads=['kT'], writes=['b2'])
        for h in range(8):
            S.op('pe', lambda e: e.matmul(hs(B[3], h), lhsT=hs(kT, h), rhs=hs(qT, h), start=True, stop=True), reads=['kT', 'qT'], writes=['b3'])
        if stage < 3: continue
        S.op('dve', lambda e: e.tensor_tensor(out=t1[:], in0=B[2][:], in1=Dls[:], op=ALU.mult), reads=['b2', 'Dls', 't1'], writes=['t1'])
        S.op('dve', lambda e: e.tensor_tensor(out=w3(Q[0]), in0=w3(t1), in1=bc(nb8[:], 2, 64), op=ALU.mult), reads=['t1', 'nb8'], writes=['Q0'])
        S.op('dve', lambda e: e.tensor_tensor(out=t2[:], in0=B[2][:], in1=Dus[:], op=ALU.mult), reads=['b2', 'Dus', 't2'], writes=['t2'])
        S.op('dve', lambda e: e.scalar_tensor_tensor(out=QTt[0][:], in0=B[1][:], scalar=-1.0, in1=t2[:], op0=ALU.mult, op1=ALU.mult), reads=['b1', 't2'], writes=['QT0'])
        S.op('dve', lambda e: e.tensor_tensor(out=AT[:], in0=B[3][:], in1=Du[:], op=ALU.mult), reads=['b3', 'Du'], writes=['AT'])
        S.op('pool', lambda e: e.tensor_tensor(out=w3(PT[0]), in0=w3(QTt[0]), in1=bc(I64[:], 1, 8), op=ALU.add), reads=['QT0', 'I64'], writes=['PT0'])
        if stage < 4: continue
        cur = 0
        for l in range(5):
            nx = 1 - cur
            qk_, qtk, qn, qtn = 'Q%d' % cur, 'QT%d' % cur, 'Q%d' % nx, 'QT%d' % nx
            for h in range(8):
                S.op('pe', lambda e: e.matmul(hs(B[4], h), lhsT=hs(QTt[cur], h), rhs=hs(Q[cur], h), start=True, stop=True), reads=[qk_, qtk], writes=['b4'])
            if l < 4:
                for h in range(8):
                    S.op('pe', lambda e: e.matmul(hs(B[5], h), lhsT=hs(Q[cur], h), rhs=hs(QTt[cur], h), start=True, stop=True), reads=[qk_, qtk], writes=['b5'])
            S.op('act', lambda e: e.activation(out=Q[nx][:], in_=B[4][:], func=AF.Copy), reads=['b4'], writes=[qn])
            if l < 4:
                S.op('dve', lambda e: e.tensor_copy(out=QTt[nx][:], in_=B[5][:]), reads=['b5'], writes=[qtn])
            pk, pn = 'PT%d' % (l % 2), 'PT%d' % ((l + 1) % 2)
            for h in range(8):
                S.op('pe', lambda e: e.matmul(hs(B[6], h), lhsT=hs(Q[nx], h), rhs=hs(PT[l % 2], h), start=True, stop=True), reads=[qn, pk], writes=['b6'])
            S.op('dve', lambda e: e.tensor_tensor(out=PT[(l + 1) % 2][:], in0=B[6][:], in1=PT[l % 2][:], op=ALU.add), reads=['b6', pk], writes=[pn])
            cur = nx
        TT = PT[1]; ttk = 'PT1'
        if stage < 5: continue
        S.op('pool', lambda e: e.tensor_tensor(out=w3(vb), in0=w3(vt), in1=bc(b8[:], 2, 64), op=ALU.mult), reads=['vt', 'b8'], writes=['vb'])
        S.op('dve', lambda e: e.tensor_tensor(out=bge[:], in0=b8[:], in1=egc[:], op=ALU.mult), reads=['b8', 'egc'], writes=['bge'])
        S.op('dve', lambda e: e.tensor_tensor(out=w3(kbg), in0=w3(kt), in1=bc(bge[:], 2, 64), op=ALU.mult), reads=['kt', 'bge'], writes=['kbg'])
        S.op('pool', lambda e: e.tensor_tensor(out=w3(ktl), in0=w3(kt), in1=bc(w3(E2)[:, :, 63], 2, 64), op=ALU.mult), reads=['kt', 'E2'], writes=['ktl'])
        S.op('dve', lambda e: e.tensor_tensor(out=qdT[:], in0=qT[:], in1=expgB[:], op=ALU.mult), reads=['qT', 'expgB'], writes=['qdT'])
        for h in range(8):
            S.op('pe', lambda e: e.matmul(hs(B[0], h), lhsT=hs(TT, h), rhs=hs(vb, h), start=True, stop=True), reads=[ttk, 'vb'], writes=['b0'])
        for h in range(8):
            S.op('pe', lambda e: e.matmul(hs(B[1], h), lhsT=hs(kbg, h), rhs=hs(TT, h), start=True, stop=True), reads=[ttk, 'kbg'], writes=['b1'])
        S.op('act', lambda e: e.activation(out=U[:], in_=B[0][:], func=AF.Copy), reads=['b0'], writes=['U'])
        S.op('dve', lambda e: e.tensor_copy(out=WT[:], in_=B[1][:]), reads=['b1'], writes=['WT'])
        if stage < 6: continue
        for h in range(8):
            S.op('pe', lambda e: e.matmul(hs(B[2], h), lhsT=hs(WT, h), rhs=hs(Sst, h), start=True, stop=True), reads=['WT', 'Sst'], writes=['b2'])
        S.op('dve', lambda e: e.tensor_tensor(out=vnew[:], in0=U[:], in1=B[2][:], op=ALU.subtract), reads=['U', 'b2'], writes=['vnew'])
        if c >= nctx:
            for h in range(8):
                S.op('pe', lambda e: e.matmul(hs(B[3], h), lhsT=hs(qdT, h), rhs=hs(Sst, h), start=True, stop=False), reads=['qdT', 'Sst'], writes=['b3'])
                S.op('pe', lambda e: e.matmul(hs(B[3], h), lhsT=hs(AT, h), rhs=hs(vnew, h), start=False, stop=True), reads=['AT', 'vnew'], writes=['b3'])
            S.op('act', lambda e: e.activation(out=ot[:], in_=B[3][:], func=AF.Copy), reads=['b3'], writes=['ot'])
            S.dma('pool', O[(c - nctx) * 64:(c - nctx + 1) * 64, :], ot[:], reads=['ot'], writes=['O'])
        for h in range(8):
            S.op('pe', lambda e: e.matmul(hs(B[7], h), lhsT=hs(ktl, h), rhs=hs(vnew, h), start=True, stop=True), reads=['ktl', 'vnew'], writes=['b7'])
        S.op('dve', lambda e: e.tensor_tensor(out=w3(stmp), in0=w3(Sst), in1=bc(w3(expgB)[:, :, 63], 2, 64), op=ALU.mult), reads=['Sst', 'expgB'], writes=['stmp'])
        S.op('dve', lambda e: e.tensor_tensor(out=Sst[:], in0=stmp[:], in1=B[7][:], op=ALU.add), reads=['stmp', 'b7'], writes=['Sst'])
    if stage < 9:
        S.dma('pool', O[0:64, :], Sst[:], reads=['Sst'], writes=['O'])
    return P


def run_l3(QKV, BG, nch=GD_CH, nctx=GD_CTX, stage=9):
    P = build_l3(nch, nctx, stage)
    L = (nch - nctx) * 64
    C = nctx * 64
    in_maps = []
    for core in range(NCORES):
        b, dr = core // 2, core % 2
        def seq(lat, cx):
            a, c_ = lat[b, :L], cx[b, :C]
            if dr == 1:
                a, c_ = a[::-1], c_[::-1]
            return np.concatenate([c_, a], axis=0)
        qkv = seq(QKV[0], QKV[1]); bg = seq(BG[0], BG[1])
        q, k, v = qkv[:, :512], qkv[:, 512:1024], qkv[:, 1024:1536]
        fm = lambda a: np.ascontiguousarray(a.reshape(nch, 64, 8, 64).transpose(0, 3, 2, 1).reshape(nch, 64, 512))
        in_maps.append(dict(Kt=np.ascontiguousarray(k), Vt=np.ascontiguousarray(v), KT=fm(k), QT=fm(q),
                            Bt=np.ascontiguousarray(bg[:, dr * 8:dr * 8 + 8]), Gt=np.ascontiguousarray(bg[:, 16 + dr * 8:16 + dr * 8 + 8])))
    res = P.run(in_maps)
    O = np.empty((4, 2, L, 512), np.float32)
    for core in range(NCORES):
        b, dr = core // 2, core % 2
        o = res[core]["O"]
        O[b, dr] = o[::-1] if dr == 1 else o
    return O


BF16 = mybir.dt.bfloat16
NKEY = 8448
NKT = NKEY // 128


def build_l4(nq=8192, lam_init=0.2):
    P = Prog()
    S = P.S
    qT = P.din("qT", [2, 2, 64, nq]); kT = P.din("kT", [2, 2, 64, NKEY]); V = P.din("V", [2, NKEY, 128])
    lam = P.din("lam", [128, 4, 64])
    DT = P.dout("DT", [2, 128, nq])
    consts(P)
    onesb = P.sb("onesb", [128, 128], BF16)
    S.op('dve', lambda e: e.tensor_copy(out=onesb[:], in_=P.ones[:, 0:128]), reads=['ones'], writes=['onesb'])
    lt = P.sb("lamt", [128, 4, 64]); S.dma('sp', lt[:], lam, writes=['lamt'])
    lp = P.sb("lamp", [128, 2, 64]); ls = P.sb("lams", [128, 4])
    S.op('dve', lambda e: e.tensor_tensor(out=lp[:, 0, :], in0=lt[:, 0, :], in1=lt[:, 1, :], op=ALU.mult), reads=['lamt'], writes=['lamp0'])
    S.op('dve', lambda e: e.tensor_tensor(out=lp[:, 1, :], in0=lt[:, 2, :], in1=lt[:, 3, :], op=ALU.mult), reads=['lamt'], writes=['lamp1'])
    S.op('dve', lambda e: e.tensor_reduce(out=ls[:, 0:2], in_=lp[:], axis=AX.X, op=ALU.add), reads=['lamp0', 'lamp1'], writes=['lams'])
    S.op('act', lambda e: e.activation(out=ls[:, 0:2], in_=ls[:, 0:2], func=AF.Exp), reads=['lams'], writes=['lams'])
    S.op('dve', lambda e: e.tensor_tensor(out=ls[:, 2:3], in0=ls[:, 1:2], in1=ls[:, 0:1], op=ALU.subtract), reads=['lams'], writes=['lams2'])
    S.op('dve', lambda e: e.tensor_scalar(out=ls[:, 3:4], in0=ls[:, 2:3], scalar1=-lam_init, scalar2=None, op0=ALU.add), reads=['lams2'], writes=['neglam'])
    kTt = P.sb("kTt", [64, 2, NKEY]); Vf = P.sb("Vf", [128, NKT, 128]); Vb = P.sb("Vb", [128, NKT, 128], BF16)
    kTb = P.sb("kTb", [64, 2, NKEY], BF16)
    qTt = P.sb("qTt", [64, 2, 512])
    qTb = [P.sb("qTb%d" % i, [64, 2, 512], BF16) for i in range(2)]
    Pt = [P.sb("Pt%d" % i, [128, 512], BF16) for i in range(3)]
    pS = [P.ps("pS%d" % i, [128, 512]) for i in range(2)]
    pO = [P.ps("pO%d" % i, [128, 512]) for i in range(2)]
    pZ = [P.ps("pZ%d" % i, [128, 512]) for i in range(2)]
    rz = P.sb("rz", [128, 512]); Om = [P.sb("Om%d" % i, [128, 512]) for i in range(2)]
    Dt = [P.sb("Dt%d" % i, [128, 512]) for i in range(2)]
    items = [(h, qc, m, kt) for h in range(2) for qc in range(nq // 512) for m in range(2) for kt in range(NKT)]

    def emit_s(j):
        h, qc, m, kt = items[j]
        if qc == 0 and m == 0 and kt == 0:
            S.dma('sp', kTt[:], kT[h].rearrange("m d k -> d m k"), writes=['kTt'])
            S.dma('sp', Vf[:], V[h].rearrange("(t p) e -> p t e", p=128), writes=['Vf'])
            S.op('dve', lambda e: e.tensor_copy(out=Vb[:], in_=Vf[:]), reads=['Vf'], writes=['Vb'])
            S.op('dve', lambda e: e.tensor_copy(out=kTb[:], in_=kTt[:]), reads=['kTt'], writes=['kTb'])
        if m == 0 and kt == 0:
            S.dma('sp', qTt[:], qT[h, :, :, qc * 512:(qc + 1) * 512].rearrange("m d k -> d m k"), writes=['qTt'])
            S.op('dve', lambda e: e.tensor_copy(out=qTb[qc % 2][:], in_=qTt[:]), reads=['qTt'], writes=['qTb%d' % (qc % 2)])
        S.op('pe', lambda e: e.matmul(pS[j % 2][:], lhsT=kTb[:, m, kt * 128:(kt + 1) * 128], rhs=qTb[qc % 2][:, m, :], start=True, stop=True),
             reads=['kTb', 'qTb%d' % (qc % 2)], writes=['pS%d' % (j % 2)])

    emit_s(0)
    for j, (h, qc, m, kt) in enumerate(items):
        if j + 1 < len(items):
            emit_s(j + 1)
        ps = pS[j % 2]; psk = 'pS%d' % (j % 2)
        pt = Pt[j % 3]; ptk = 'Pt%d' % (j % 3)
        S.op('act', lambda e: e.activation(out=pt[:], in_=ps[:], func=AF.Exp, scale=0.125), reads=[psk], writes=[ptk])
        S.op('pe', lambda e: e.matmul(pO[m][:], lhsT=Vb[:, kt, :], rhs=pt[:], start=(kt == 0), stop=(kt == NKT - 1)),
             reads=['Vb', ptk], writes=['pO%d' % m])
        S.op('pe', lambda e: e.matmul(pZ[m][:], lhsT=onesb[:], rhs=pt[:], start=(kt == 0), stop=(kt == NKT - 1)),
             reads=['onesb', ptk], writes=['pZ%d' % m])
        if kt == NKT - 1:
            S.op('dve', lambda e: e.reciprocal(out=rz[:], in_=pZ[m][:]), reads=['pZ%d' % m], writes=['rz'])
            S.op('dve', lambda e: e.tensor_tensor(out=Om[m][:], in0=pO[m][:], in1=rz[:], op=ALU.mult), reads=['pO%d' % m, 'rz'], writes=['Om%d' % m])
            if m == 1:
                dt_ = Dt[qc % 2]; dk = 'Dt%d' % (qc % 2)
                S.op('dve', lambda e: e.scalar_tensor_tensor(out=dt_[:], in0=Om[1][:], scalar=ls[:, 3:4], in1=Om[0][:], op0=ALU.mult, op1=ALU.add),
                     reads=['Om0', 'Om1', 'neglam'], writes=[dk])
                S.dma('pool', DT[h, :, qc * 512:(qc + 1) * 512], dt_[:], reads=[dk], writes=['DT'])
    return P


def run_l4(QKr, Plat, Pctx, lam_q1, lam_k1, lam_q2, lam_k2, nq=8192):
    P = build_l4(nq)
    lamin = np.ascontiguousarray(np.broadcast_to(np.stack([lam_q1, lam_k1, lam_q2, lam_k2])[None], (128, 4, 64))).astype(np.float32)
    in_maps = []
    for core in range(NCORES):
        b, hp = core // 2, core % 2
        q = QKr[0][b, :nq, 0:512].reshape(nq, 4, 2, 64)[:, 2 * hp:2 * hp + 2]
        k = np.concatenate([QKr[0][b, :, 512:1024], QKr[1][b, :, 512:1024]], axis=0).reshape(NKEY, 4, 2, 64)[:, 2 * hp:2 * hp + 2]
        v = np.concatenate([Plat[b, :, 3104:3616], Pctx[b, :, 3104:3616]], axis=0).reshape(NKEY, 4, 128)[:, 2 * hp:2 * hp + 2]
        in_maps.append(dict(qT=np.ascontiguousarray(q.transpose(1, 2, 3, 0)), kT=np.ascontiguousarray(k.transpose(1, 2, 3, 0)),
                            V=np.ascontiguousarray(v.transpose(1, 0, 2)), lam=lamin))
    res = P.run(in_maps)
    out = np.empty((4, nq, 512), np.float32)
    for core in range(NCORES):
        b, hp = core // 2, core % 2
        dt = res[core]["DT"]
        out[b, :, hp * 256:(hp + 1) * 256] = dt.transpose(2, 0, 1).reshape(nq, 256)
    return out


TOK = 4096
NTT = TOK // 128


def build_l5a(kind):
    P = Prog()
    S = P.S
    X = P.din("X", [TOK, D])
    if kind == 'ab':
        O0 = P.din("O0", [TOK, 512]); O1 = P.din("O1", [TOK, 512]); GATE = P.din("GATE", [TOK, 512]); DL = P.din("DL", [TOK, 512])
        gdnw = P.din("gdnw", [128, 64]); sublnw = P.din("sublnw", [128, 128])
    else:
        FM = P.din("FM", [TOK, D])
    wout = P.din("wout", [D, D])
    cT = P.din("cT", [128, 1, 8]); adaw = P.din("adaw", [D, 1024]); adab = P.din("adab", [128, 1024])
    X1 = P.dout("X1", [TOK, D])
    consts(P)
    csil = P.sb("csil", [128, 1, 8]); S.dma('sp', csil[:], cT, writes=['csil'])
    S.op('act', lambda e: e.activation(out=csil[:], in_=csil[:], func=AF.Silu), reads=['csil'], writes=['csil'])
    g1b = P.sb("g1b", [128, D])
    emit_mod(P, csil, 0, adaw, adab, 0, 1024, g1b, 'g1b', P.ones, "m")
    wt = P.sb("wt", [128, 8, D])
    wv = wout.rearrange("(k p) n -> p k n", p=128)
    for k in range(8):
        S.dma('sp', wt[:, k, :], wv[:, k, :], writes=['wt%d' % k])
    if kind == 'ab':
        gw = P.sb("gw", [128, 64]); S.dma('sp', gw[:], gdnw, writes=['gw'])
        sw = P.sb("sw", [128, 128]); S.dma('sp', sw[:], sublnw, writes=['sw'])
        o0 = P.sb("o0", [128, 512]); o1 = P.sb("o1", [128, 512]); gt = P.sb("gt", [128, 512]); dl = P.sb("dl", [128, 512])
        sq = P.sb("sq", [128, 512]); st = P.sb("st", [128, 3, 12])
    xt = P.sb("xt", [128, D]); mix = P.sb("mix", [128, D]); mixT = P.sb("mixT", [128, D]); x1 = P.sb("x1", [128, D])
    pT = P.ps("pT", [128, D]); pY = P.ps("pY", [128, D])
    for t in range(NTT):
        rs = slice(t * 128, (t + 1) * 128)
        S.dma('sp', xt[:], X[rs, :], writes=['xt'])
        if kind == 'ab':
            S.dma('sp', o0[:], O0[rs, :], writes=['o0']); S.dma('sp', o1[:], O1[rs, :], writes=['o1'])
            S.dma('sp', gt[:], GATE[rs, :], writes=['gt']); S.dma('sp', dl[:], DL[rs, :], writes=['dl'])
            S.op('dve', lambda e: e.tensor_tensor(out=o0[:], in0=o0[:], in1=o1[:], op=ALU.add), reads=['o0', 'o1'], writes=['o0'])
            S.op('act', lambda e: e.activation(out=sq[:], in_=o0[:], func=AF.Square), reads=['o0'], writes=['sq'])
            S.op('dve', lambda e: e.tensor_reduce(out=st[:, 0, 0:8], in_=sq[:].rearrange("p (g d) -> p g d", d=64), axis=AX.X, op=ALU.add), reads=['sq'], writes=['st0a'])
            S.op('act', lambda e: e.activation(out=sq[:], in_=dl[:], func=AF.Square), reads=['dl', 'st0a'], writes=['sq'])
            S.op('dve', lambda e: e.tensor_reduce(out=st[:, 0, 8:12], in_=sq[:].rearrange("p (g d) -> p g d", d=128), axis=AX.X, op=ALU.add), reads=['sq'], writes=['st0b'])
            S.op('act', lambda e: e.activation(out=st[:, 1, 0:8], in_=st[:, 0, 0:8], func=AF.Sqrt, bias=P.epsb[:, 0:1], scale=1.0 / 64), reads=['st0a', 'epsb'], writes=['st1a'])
            S.op('act', lambda e: e.activation(out=st[:, 1, 8:12], in_=st[:, 0, 8:12], func=AF.Sqrt, bias=P.epsb[:, 0:1], scale=1.0 / 128), reads=['st0b', 'epsb'], writes=['st1b'])
            S.op('dve', lambda e: e.reciprocal(out=st[:, 2, :], in_=st[:, 1, :]), reads=['st1a', 'st1b'], writes=['st2'])
            S.op('dve', lambda e: e.tensor_scalar(out=st[:, 2, 8:12], in0=st[:, 2, 8:12], scalar1=0.8, scalar2=None, op0=ALU.mult), reads=['st2'], writes=['st2'])
            v8 = lambda a: a.rearrange("p (g d) -> p g d", d=64)
            v4 = lambda a: a.rearrange("p (g d) -> p g d", d=128)
            S.op('dve', lambda e: e.tensor_tensor(out=v8(o0[:]), in0=v8(o0[:]), in1=bc(st[:, 2, 0:8], 2, 64), op=ALU.mult), reads=['o0', 'st2'], writes=['o0'])
            S.op('pool', lambda e: e.tensor_tensor(out=v8(o0[:]), in0=v8(o0[:]), in1=bc(gw[:], 1, 8), op=ALU.mult), reads=['o0', 'gw'], writes=['o0'])
            S.op('act', lambda e: e.activation(out=gt[:], in_=gt[:], func=AF.Silu), reads=['gt'], writes=['gt'])
            S.op('dve', lambda e: e.tensor_tensor(out=mix[:, 0:512], in0=o0[:], in1=gt[:], op=ALU.mult), reads=['o0', 'gt'], writes=['mix0'])
            S.op('dve', lambda e: e.tensor_tensor(out=v4(dl[:]), in0=v4(dl[:]), in1=bc(st[:, 2, 8:12], 2, 128), op=ALU.mult), reads=['dl', 'st2'], writes=['dl'])
            S.op('pool', lambda e: e.tensor_tensor(out=v4(mix[:, 512:1024]), in0=v4(dl[:]), in1=bc(sw[:], 1, 4), op=ALU.mult), reads=['dl', 'sw'], writes=['mix1'])
        else:
            S.dma('sp', mix[:], FM[rs, :], writes=['mix0', 'mix1'])
        for k in range(8):
            S.op('pe', lambda e: e.transpose(pT[:, k * 128:(k + 1) * 128], mix[:, k * 128:(k + 1) * 128], P.idf[:]),
                 reads=['mix%d' % (k // 4), 'ident'], writes=['pT%d' % (k // 4)])
        S.op('act', lambda e: e.activation(out=mixT[:, 0:512], in_=pT[:, 0:512], func=AF.Copy), reads=['pT0'], writes=['mixT0'])
        S.op('dve', lambda e: e.tensor_copy(out=mixT[:, 512:1024], in_=pT[:, 512:1024]), reads=['pT1'], writes=['mixT1'])
        for cb in range(2):
            for k in range(8):
                S.op('pe', lambda e: e.matmul(pY[:, cb * 512:(cb + 1) * 512], lhsT=mixT[:, k * 128:(k + 1) * 128], rhs=wt[:, k, cb * 512:(cb + 1) * 512],
                                              start=(k == 0), stop=(k == 7)), reads=['mixT%d' % (k // 4), 'wt%d' % k], writes=['pY%d' % cb])
        S.op('dve', lambda e: e.tensor_tensor(out=x1[:], in0=pY[:], in1=g1b[:], op=ALU.mult), reads=['pY0', 'pY1', 'g1b'], writes=['x1'])
        S.op('pool', lambda e: e.tensor_tensor(out=x1[:], in0=x1[:], in1=xt[:], op=ALU.add), reads=['x1', 'xt'], writes=['x1'])
        S.dma('pool', X1[rs, :], x1[:], reads=['x1'], writes=['X1'])
    return P


def run_l5a(kind, x, c, ada_w_i, ada_b_i, wout, **kw):
    P = build_l5a(kind)
    in_maps = []
    for core in range(NCORES):
        b, hf = core // 2, core % 2
        sl = slice(hf * TOK, (hf + 1) * TOK)
        m = dict(X=np.ascontiguousarray(x[b, sl]), wout=np.ascontiguousarray(wout), cT=cT_layout(c[b][None]),
                 adaw=np.ascontiguousarray(ada_w_i[:, 2048:3072]), adab=rep128(ada_b_i[2048:3072]))
        if kind == 'ab':
            m.update(O0=np.ascontiguousarray(kw['O'][b, 0, sl]), O1=np.ascontiguousarray(kw['O'][b, 1, sl]),
                     GATE=np.ascontiguousarray(kw['Plat'][b, sl, 1536:2048]), DL=np.ascontiguousarray(kw['dlat'][b, sl]),
                     gdnw=rep128(kw['gdn_norm_w']), sublnw=rep128(kw['subln_w']))
        else:
            m.update(FM=np.ascontiguousarray(kw['fm'][b, sl]))
        in_maps.append(m)
    res = P.run(in_maps)
    out = np.empty((4, 8192, D), np.float32)
    for core in range(NCORES):
        b, hf = core // 2, core % 2
        out[b, hf * TOK:(hf + 1) * TOK] = res[core]["X1"]
    return out


def build_l5b(tail, ntt=NTT):
    P = Prog()
    S = P.S
    tok = ntt * 128
    X1 = P.din("X1", [tok, D])
    cT = P.din("cT", [128, 1, 8]); adaw = P.din("adaw", [D, 3072]); adab = P.din("adab", [128, 3072]); n2w = P.din("n2w", [128, D])
    wq = P.din("wq", [D, 2048]); keysT = P.din("keysT", [128, 16, 128])
    Utab = P.din("Utab", [16384, D]); Vtab = P.din("Vtab", [16384, D])
    if tail == 'hnext':
        adawn = P.din("adawn", [D, 2048]); adabn = P.din("adabn", [128, 2048]); n1wn = P.din("n1wn", [128, D])
        Hn = P.dout("Hn", [tok, D])
    else:
        fnw = P.din("fnw", [128, D])
    Xo = P.dout("Xo", [tok, D])
    consts(P)
    csil = P.sb("csil", [128, 1, 8]); S.dma('sp', csil[:], cT, writes=['csil'])
    S.op('act', lambda e: e.activation(out=csil[:], in_=csil[:], func=AF.Silu), reads=['csil'], writes=['csil'])
    modb = P.sb("modb", [128, 3072])
    P.mod_bw = 3072
    pA = P.ps("pA", [128, D]); pQ = P.ps("pQ", [128, 2, 512]); pSc = P.ps("pSc", [128, 2048])
    pmx = ([pQ[:, 0, :], pQ[:, 1, :]], ['pQ0', 'pQ1'])
    emit_mod(P, csil, 0, adaw, adab, 0, 3072, modb, 'modb', P.ones, "m", pmx=pmx)
    nw = P.sb("nw", [128, D]); S.dma('sp', nw[:], n2w, writes=['nw'])
    wmod2 = P.sb("wmod2", [128, D])
    S.op('dve', lambda e: e.scalar_tensor_tensor(out=wmod2[:], in0=modb[:, 1024:2048], scalar=1.0, in1=nw[:], op0=ALU.add, op1=ALU.mult),
         reads=['modb', 'nw'], writes=['wmod2'])
    if tail == 'hnext':
        modn = P.sb("modn", [128, 2048])
        emit_mod(P, csil, 0, adawn, adabn, 0, 2048, modn, 'modn', P.ones, "m", pmx=pmx)
        S.dma('sp', nw[:], n1wn, reads=['wmod2'], writes=['nw'])
        wmodn = P.sb("wmodn", [128, D])
        S.op('dve', lambda e: e.scalar_tensor_tensor(out=wmodn[:], in0=modn[:, 1024:2048], scalar=1.0, in1=nw[:], op0=ALU.add, op1=ALU.mult),
             reads=['modn', 'nw'], writes=['wmodn'])
    else:
        S.dma('sp', nw[:], fnw, reads=['wmod2'], writes=['nw'])
    wqt = P.sb("wqt", [128, 8, 2048])
    wqv = wq.rearrange("(k p) n -> p k n", p=128)
    for k in range(8):
        S.dma('sp', wqt[:, k, :], wqv[:, k, :], writes=['wq%d' % k])
    kyt = P.sb("kyt", [128, 16, 128]); S.dma('sp', kyt[:], keysT, writes=['kyt'])
    zr = P.sb("zr", [128, 255]); S.op('pool', lambda g: g.memset(zr[:], 0.0), writes=['zr']); S.op('pool', lambda g: g.memset(zr[:, 127:128], 1.0), reads=['zr'], writes=['zr'])
    iot = P.sb("iot", [128, 16]); S.op('pool', lambda g: g.iota(iot[:], pattern=[[1, 16]], base=0, channel_multiplier=0, allow_small_or_imprecise_dtypes=True), writes=['iot'])
    xt = P.sb("xt", [128, D]); hn = P.sb("hn", [128, D]); hnT = P.sb("hnT", [128, D]); ss = P.sb("ss", [128, 4])
    qTs = P.sb("qTs", [128, 16, 128]); sc = P.sb("sc", [128, 2048]); wk = P.sb("wk", [128, 2048])
    m16 = P.sb("m16", [128, 16, 16]); i16 = P.sb("i16", [128, 16, 16], U32); i16f = P.sb("i16f", [128, 16, 16])
    c16 = P.sb("c16", [128, 8, 16]); p16 = P.sb("p16", [128, 8, 16], U32); pa = P.sb("pa", [128, 8, 16], U32); pb = P.sb("pb", [128, 8, 16], U32)
    paf = P.sb("paf", [128, 8, 16]); pbf = P.sb("pbf", [128, 8, 16]); sel = P.sb("sel", [128, 2, 128]); idxf = P.sb("idxf", [128, 128])
    gat = P.sb("gat", [128, 128]); gs = P.sb("gs", [128, 2, 8])
    idxTi = P.sb("idxTi", [128, 128], I32); gateT = P.sb("gateT", [128, 128]); actA = P.sb("actA", [128, 128]); Wm = P.sb("Wm", [128, 128])
    NG = 4
    Ug = [P.sb("Ug%d" % i, [128, D], BF16) for i in range(NG)]
    Wz = [P.sb("Wz%d" % i, [128, 128], BF16) for i in range(3)]
    ujunk = P.sb("ujunk", [128, D], BF16)
    hnb = P.sb("hnb", [128, D], BF16)
    idb = P.sb("idb", [128, 128], BF16)
    S.op('dve', lambda e: e.tensor_copy(out=idb[:], in_=P.idf[:]), reads=['ident'], writes=['idb'])
    Ub = P.nc.dram_tensor("Ub16", [16384, D], BF16, kind="Internal").ap()
    Vb = P.nc.dram_tensor("Vb16", [16384, D], BF16, kind="Internal").ap()
    cvb = [P.sb("cvb%d" % i, [128, D], BF16) for i in range(2)]
    stg = [sc, wk]
    ci = 0
    for src, dst, dk in ((Utab, Ub, 'Ub16'), (Vtab, Vb, 'Vb16')):
        for r in range(128):
            st_ = stg[ci % 2]; sk_ = 'stg%d' % (ci % 2); cb_ = cvb[ci % 2]; ck_ = 'cvb%d' % (ci % 2)
            S.dma('sp', st_[:, 0:D], src[r * 128:(r + 1) * 128, :], writes=[sk_])
            if ci % 2 == 0:
                S.op('act', lambda e: e.activation(out=cb_[:], in_=st_[:, 0:D], func=AF.Copy), reads=[sk_], writes=[ck_])
            else:
                S.op('dve', lambda e: e.tensor_copy(out=cb_[:], in_=st_[:, 0:D]), reads=[sk_], writes=[ck_])
            S.dma('pool', dst[r * 128:(r + 1) * 128, :], cb_[:], reads=[ck_], writes=[dk])
            ci += 1
    pB = [pSc[:, 0:1024], pA[:, :]]
    pBk = [['pSc0', 'pSc1'], ['pA0', 'pA1']]
    pOut = pSc[:, 1024:2048]; pOk = ['pSc2', 'pSc3']
    cand = wk; junk = sc
    v4 = lambda a: a.rearrange("p (h a b) -> p h a b", a=16, b=16)
    for t in range(ntt):
        rs = slice(t * 128, (t + 1) * 128)
        S.dma('sp', xt[:], X1[rs, :], writes=['xt'])
        emit_rmsnorm_mod(P, xt, 'xt', wmod2, modb[:, 0:1024], ['wmod2', 'modb'], hn, 'hn', (hn, ss))
        for k in range(8):
            S.op('pe', lambda e: e.transpose(pA[:, k * 128:(k + 1) * 128], hn[:, k * 128:(k + 1) * 128], P.idf[:]), reads=['hn', 'ident'], writes=['pA%d' % (k // 4)])
        S.op('act', lambda e: e.activation(out=hnT[:, 0:512], in_=pA[:, 0:512], func=AF.Copy), reads=['pA0'], writes=['hnT0'])
        S.op('dve', lambda e: e.tensor_copy(out=hnT[:, 512:1024], in_=pA[:, 512:1024]), reads=['pA1'], writes=['hnT1'])
        for g in range(4):
            for j in range(4):
                hp = g * 4 + j
                for k in range(8):
                    S.op('pe', lambda e: e.matmul(pQ[:, g % 2, j * 128:(j + 1) * 128], lhsT=wqt[:, k, hp * 128:(hp + 1) * 128], rhs=hnT[:, k * 128:(k + 1) * 128],
                                                  start=(k == 0), stop=(k == 7)), reads=['wq%d' % k, 'hnT%d' % (k // 4)], writes=['pQ%d' % (g % 2)])
            eng = 'act' if g % 2 == 0 else 'dve'
            if eng == 'act':
                S.op('act', lambda e: e.activation(out=qTs[:, g * 4:(g + 1) * 4, :], in_=pQ[:, g % 2, :].rearrange("p (j n) -> p j n", n=128), func=AF.Copy), reads=['pQ%d' % (g % 2)], writes=['qTs%d' % g])
            else:
                S.op('dve', lambda e: e.tensor_copy(out=qTs[:, g * 4:(g + 1) * 4, :], in_=pQ[:, g % 2, :].rearrange("p (j n) -> p j n", n=128)), reads=['pQ%d' % (g % 2)], writes=['qTs%d' % g])
        for hp in range(16):
            S.op('pe', lambda e: e.matmul(pSc[:, hp * 128:(hp + 1) * 128], lhsT=qTs[:, hp, :], rhs=kyt[:, hp, :], start=True, stop=True),
                 reads=['qTs%d' % (hp // 4), 'kyt'], writes=['pSc%d' % (hp // 4)])
        for g in range(4):
            if g % 2 == 0:
                S.op('act', lambda e: e.activation(out=sc[:, g * 512:(g + 1) * 512], in_=pSc[:, g * 512:(g + 1) * 512], func=AF.Copy), reads=['pSc%d' % g], writes=['sc%d' % g, 'stg0'])
            else:
                S.op('dve', lambda e: e.tensor_copy(out=sc[:, g * 512:(g + 1) * 512], in_=pSc[:, g * 512:(g + 1) * 512]), reads=['pSc%d' % g], writes=['sc%d' % g, 'stg0'])
        for hp in range(16):
            blk = slice(hp * 128, (hp + 1) * 128)
            sk = 'sc%d' % (hp // 4)
            S.op('dve', lambda e: e.max(out=m16[:, hp, 0:8], in_=sc[:, blk]), reads=[sk], writes=['m16a'])
            S.op('dve', lambda e: e.match_replace(out=wk[:, blk], in_to_replace=m16[:, hp, 0:8], in_values=sc[:, blk], imm_value=-1e30), reads=[sk, 'm16a'], writes=['wk', 'stg1'])
            S.op('dve', lambda e: e.max(out=m16[:, hp, 8:16], in_=wk[:, blk]), reads=['wk'], writes=['m16b'])
            S.op('dve', lambda e: e.max_index(out=i16[:, hp, 0:8], in_max=m16[:, hp, 0:8], in_values=sc[:, blk]), reads=[sk, 'm16a'], writes=['i16'])
            S.op('dve', lambda e: e.max_index(out=i16[:, hp, 8:16], in_max=m16[:, hp, 8:16], in_values=sc[:, blk]), reads=[sk, 'm16b'], writes=['i16'])
        S.op('dve', lambda e: e.tensor_copy(out=i16f[:], in_=i16[:]), reads=['i16'], writes=['i16f'])
        m16v = m16[:].rearrange("p (h two) k -> p h two k", two=2)
        i16v = i16f[:].rearrange("p (h two) k -> p h two k", two=2)
        S.op('dve', lambda e: e.tensor_tensor(out=v4(cand[:]), in0=bc(m16v[:, :, 0, :], 3, 16), in1=bc(m16v[:, :, 1, :], 2, 16), op=ALU.add),
             reads=['m16a', 'm16b', 'wk'], writes=['cand'])
        for h in range(8):
            blk = slice(h * 256, (h + 1) * 256)
            S.op('dve', lambda e: e.max(out=c16[:, h, 0:8], in_=cand[:, blk]), reads=['cand'], writes=['c16a'])
            S.op('dve', lambda e: e.match_replace(out=junk[:, blk], in_to_replace=c16[:, h, 0:8], in_values=cand[:, blk], imm_value=-1e30), reads=['cand', 'c16a'] + ['sc%d' % i for i in range(4)], writes=['junk'])
            S.op('dve', lambda e: e.max(out=c16[:, h, 8:16], in_=junk[:, blk]), reads=['junk'], writes=['c16b'])
            S.op('dve', lambda e: e.max_index(out=p16[:, h, 0:8], in_max=c16[:, h, 0:8], in_values=cand[:, blk]), reads=['cand', 'c16a'], writes=['p16'])
            S.op('dve', lambda e: e.max_index(out=p16[:, h, 8:16], in_max=c16[:, h, 8:16], in_values=cand[:, blk]), reads=['cand', 'c16b'], writes=['p16'])
        S.op('dve', lambda e: e.tensor_single_scalar(out=pa[:], in_=p16[:], scalar=4, op=ALU.logical_shift_right), reads=['p16'], writes=['pa'])
        S.op('dve', lambda e: e.tensor_single_scalar(out=pb[:], in_=p16[:], scalar=15, op=ALU.bitwise_and), reads=['p16'], writes=['pb'])
        S.op('dve', lambda e: e.tensor_copy(out=paf[:], in_=pa[:]), reads=['pa'], writes=['paf'])
        S.op('dve', lambda e: e.tensor_copy(out=pbf[:], in_=pb[:]), reads=['pb'], writes=['pbf'])
        iob = bc(bc(iot[:], 1, 16), 1, 8)
        for w_, (pf, pk) in enumerate(((paf, 'paf'), (pbf, 'pbf'))):
            S.op('dve', lambda e: e.tensor_tensor(out=v4(junk[:]), in0=bc(pf[:], 3, 16), in1=iob, op=ALU.is_equal), reads=[pk, 'iot', 'junk'], writes=['junk'])
            S.op('dve', lambda e: e.tensor_tensor(out=v4(junk[:]), in0=v4(junk[:]), in1=bc(i16v[:, :, w_, :], 2, 16), op=ALU.mult), reads=['junk', 'i16f'], writes=['junk'])
            S.op('dve', lambda e: e.tensor_reduce(out=sel[:, w_, :], in_=junk[:].rearrange("p (x a) -> p x a", a=16), axis=AX.X, op=ALU.add), reads=['junk'], writes=['sel%d' % w_])
        S.op('dve', lambda e: e.scalar_tensor_tensor(out=idxf[:], in0=sel[:, 0, :], scalar=128.0, in1=sel[:, 1, :], op0=ALU.mult, op1=ALU.add), reads=['sel0', 'sel1'], writes=['idxf'])
        c16f = c16[:]
        S.op('dve', lambda e: e.tensor_tensor(out=gat[:].rearrange("p (h k) -> p h k", k=16), in0=c16f, in1=bc(c16[:, :, 0], 2, 16), op=ALU.subtract), reads=['c16a', 'c16b'], writes=['gat'])
        S.op('dve', lambda e: e.tensor_scalar(out=gat[:], in0=gat[:], scalar1=-80.0, scalar2=None, op0=ALU.max), reads=['gat'], writes=['gat'])
        S.op('act', lambda e: e.activation(out=gat[:], in_=gat[:], func=AF.Exp), reads=['gat'], writes=['gat'])
        S.op('dve', lambda e: e.tensor_reduce(out=gs[:, 0, :], in_=gat[:].rearrange("p (h k) -> p h k", k=16), axis=AX.X, op=ALU.add), reads=['gat'], writes=['gs0'])
        S.op('dve', lambda e: e.reciprocal(out=gs[:, 1, :], in_=gs[:, 0, :]), reads=['gs0'], writes=['gs1'])
        S.op('dve', lambda e: e.tensor_tensor(out=gat[:].rearrange("p (h k) -> p h k", k=16), in0=gat[:].rearrange("p (h k) -> p h k", k=16), in1=bc(gs[:, 1, :], 2, 16), op=ALU.mult), reads=['gat', 'gs1'], writes=['gat'])
        S.op('pe', lambda e: e.transpose(pQ[:, 0, 0:128], idxf[:], P.idf[:]), reads=['idxf', 'ident'], writes=['pQ0'])
        S.op('pe', lambda e: e.transpose(pQ[:, 1, 0:128], gat[:], P.idf[:]), reads=['gat', 'ident'], writes=['pQ1'])
        S.op('dve', lambda e: e.tensor_copy(out=idxTi[:], in_=pQ[:, 0, 0:128]), reads=['pQ0'], writes=['idxTi'])
        S.op('act', lambda e: e.activation(out=gateT[:], in_=pQ[:, 1, 0:128], func=AF.Copy), reads=['pQ1'], writes=['gateT'])
        S.op('dve', lambda e: e.tensor_copy(out=hnb[:], in_=hn[:]), reads=['hn'], writes=['hnb'])
        for tk in range(128):
            ug = Ug[tk % NG]; uk = 'Ug%d' % (tk % NG)
            S.dmaf('pool', lambda g: g.indirect_dma_start(out=ug[:], out_offset=None, in_=Ub[:, :], in_offset=bass.IndirectOffsetOnAxis(ap=idxTi[:, tk:tk + 1], axis=0)),
                   reads=['idxTi', 'Ub16'], writes=[uk])
            pb_ = pB[tk % 2]; pbk = pBk[tk % 2]
            for cb in range(2):
                S.op('pe', lambda e: e.matmul(pb_[:, cb * 512:(cb + 1) * 512], lhsT=bc(idb[:, tk], 1, 128), rhs=hnb[:, cb * 512:(cb + 1) * 512], start=True, stop=True),
                     reads=['idb', 'hnb'], writes=[pbk[cb]])
            S.op('dve', lambda e: e.scalar_tensor_tensor(out=ujunk[:], in0=ug[:], scalar=1.0, in1=pb_, op0=ALU.mult, op1=ALU.mult, accum_out=actA[:, tk:tk + 1]),
                 reads=[uk] + pbk, writes=['ujunk', 'actA'])
        S.op('act', lambda e: e.activation(out=Wm[:], in_=actA[:], func=AF.Gelu), reads=['actA'], writes=['Wm'])
        S.op('dve', lambda e: e.tensor_tensor(out=Wm[:], in0=Wm[:], in1=gateT[:], op=ALU.mult), reads=['Wm', 'gateT'], writes=['Wm'])
        for tk in range(128):
            vg = Ug[tk % NG]; vk = 'Ug%d' % (tk % NG)
            S.dmaf('pool', lambda g: g.indirect_dma_start(out=vg[:], out_offset=None, in_=Vb[:, :], in_offset=bass.IndirectOffsetOnAxis(ap=idxTi[:, tk:tk + 1], axis=0)),
                   reads=['idxTi', 'Vb16'], writes=[vk])
            wz = Wz[tk % 3]; wzk = 'Wz%d' % (tk % 3)
            S.op('act', lambda e: e.activation(out=wz[:], in_=zr[:, 127 - tk:255 - tk], func=AF.Copy, scale=Wm[:, tk:tk + 1]), reads=['zr', 'Wm'], writes=[wzk])
            for cb in range(2):
                S.op('pe', lambda e: e.matmul(pOut[:, cb * 512:(cb + 1) * 512], lhsT=wz[:], rhs=vg[:, cb * 512:(cb + 1) * 512], start=(tk == 0), stop=(tk == 127)),
                     reads=[wzk, vk], writes=[pOk[cb]])
        S.op('dve', lambda e: e.tensor_tensor(out=hn[:], in0=pOut, in1=modb[:, 2048:3072], op=ALU.mult), reads=pOk + ['modb', 'hn'], writes=['hn'])
        S.op('pool', lambda e: e.tensor_tensor(out=xt[:], in0=xt[:], in1=hn[:], op=ALU.add), reads=['xt', 'hn'], writes=['xt'])
        if tail == 'hnext':
            S.dma('sp', Xo[rs, :], xt[:], reads=['xt'], writes=['Xo'])
            emit_rmsnorm_mod(P, xt, 'xt', wmodn, modn[:, 0:1024], ['wmodn', 'modn'], hn, 'hn', (hn, ss))
            S.dma('sp', Hn[rs, :], hn[:], reads=['hn'], writes=['Hn'])
        else:
            emit_rmsnorm_mod(P, xt, 'xt', nw, None, ['nw'], hn, 'hn', (hn, ss))
            S.dma('sp', Xo[rs, :], hn[:], reads=['hn'], writes=['Xo'])
    return P


def run_l5b(tail, x1, c, ada_w_i, ada_b_i, norm2_w_i, wq, keys, utab, vtab, ntt=NTT, ncores=NCORES, **kw):
    P = build_l5b(tail, ntt)
    tok = ntt * 128
    keysT = np.ascontiguousarray(keys.reshape(16, 128, 128).transpose(2, 0, 1))
    in_maps = []
    for core in range(ncores):
        b, hf = core // 2, core % 2
        sl = slice(hf * TOK, hf * TOK + tok)
        m = dict(X1=np.ascontiguousarray(x1[b, sl]), cT=cT_layout(c[b][None]), adaw=np.ascontiguousarray(ada_w_i[:, 3072:6144]), adab=rep128(ada_b_i[3072:6144]),
                 n2w=rep128(norm2_w_i), wq=np.ascontiguousarray(wq), keysT=keysT, Utab=np.ascontiguousarray(utab), Vtab=np.ascontiguousarray(vtab))
        if tail == 'hnext':
            m.update(adawn=np.ascontiguousarray(kw['ada_w_n'][:, :2048]), adabn=rep128(kw['ada_b_n'][:2048]), n1wn=rep128(kw['norm1_w_n']))
        else:
            m.update(fnw=rep128(kw['final_norm_w']))
        in_maps.append(m)
    res = P.run(in_maps)
    xo = np.zeros((4, 8192, D), np.float32)
    hn = np.zeros((4, 8192, D), np.float32) if tail == 'hnext' else None
    for core in range(ncores):
        b, hf = core // 2, core % 2
        xo[b, hf * TOK:hf * TOK + tok] = res[core]["Xo"]
        if hn is not None:
            hn[b, hf * TOK:hf * TOK + tok] = res[core]["Hn"]
    return xo, hn


def build_l6():
    P = Prog()
    S = P.S
    nc = P.nc
    HT = P.din("HT", [4, 128, 8192])
    CS = P.din("CS", [2, 128, 512])
    W64 = P.din("W64", [128, 128])
    WB = P.din("WB", [128, 64, 2, 128])
    FM = P.dout("FM", [8192, 512])
    Zs = [nc.dram_tensor("Zs%d" % g, [8192, 512], F32, kind="Internal").ap() for g in range(2)]
    Us = [nc.dram_tensor("Us%d" % g, [128, 128, 256], F32, kind="Internal").ap() for g in range(2)]
    cs = P.sb("cs", [128, 2, 512]); S.dma('sp', cs[:], CS.rearrange("h p n -> p h n"), writes=['cs'])
    w64 = P.sb("w64", [128, 128]); S.dma('sp', w64[:], W64, writes=['w64'])
    pz = [P.ps("pz%d" % i, [128, 512]) for i in range(2)]
    pu = P.ps("pu", [128, 2048])
    py = [P.ps("py%d" % i, [128, 256]) for i in range(2)]
    TB = 2048
    hts = [P.sb("ht%d" % i, [128, 4, TB]) for i in range(2)]
    zts = [P.sb("zt%d" % i, [128, 512]) for i in range(2)]
    it = 0
    for tb in range(8192 // TB):
        ht = hts[tb % 2]; hk = 'ht%d' % (tb % 2)
        S.dma('sp', ht[:], HT[:, :, tb * TB:(tb + 1) * TB].rearrange("c p t -> p c t"), writes=[hk])
        for tt in range(TB // 128):
            for g in range(2):
                p_ = pz[it % 2]; pk = 'pz%d' % (it % 2); zt = zts[it % 2]; zk = 'zt%d' % (it % 2)
                it += 1
                for hf in range(2):
                    S.op('pe', lambda e: e.matmul(p_[:], lhsT=ht[:, 2 * g + hf, tt * 128:(tt + 1) * 128], rhs=cs[:, hf, :], start=(hf == 0), stop=(hf == 1)),
                         reads=[hk, 'cs'], writes=[pk])
                S.op('act' if g == 0 else 'dve', (lambda e: e.activation(out=zt[:], in_=p_[:], func=AF.Copy)) if g == 0 else (lambda e: e.tensor_copy(out=zt[:], in_=p_[:])),
                     reads=[pk], writes=[zk])
                r0 = tb * TB + tt * 128
                S.dma('pool', Zs[g][r0:r0 + 128, :], zt[:], reads=[zk], writes=['Zs%d' % g])
    LB = 8
    zin = [P.sb("zin%d" % i, [128, LB, 256]) for i in range(2)]
    uts = [P.sb("ut%d" % i, [128, LB, 256]) for i in range(2)]
    it = 0
    for g in range(2):
        zv = Zs[g].rearrange("(l1 l2) (ri kc) -> ri l1 l2 kc", l2=128, ri=2)
        uv = Us[g].rearrange("l2 m kc -> m l2 kc")
        for lb in range(128 // LB):
            zi = zin[it % 2]; zk = 'zin%d' % (it % 2); ut = uts[it % 2]; uk = 'ut%d' % (it % 2)
            it += 1
            for ri in range(2):
                S.dma('sp', zi[ri * 64:(ri + 1) * 64, :, :], zv[ri][:, lb * LB:(lb + 1) * LB, :], reads=['Zs%d' % g], writes=[zk + '_%d' % ri])
            for j in range(LB // 2):
                S.op('pe', lambda e: e.matmul(pu[:, j * 512:(j + 1) * 512], lhsT=w64[:], rhs=zi[:, 2 * j:2 * j + 2, :].rearrange("p a b -> p (a b)"), start=True, stop=True),
                     reads=['w64', zk + '_0', zk + '_1'], writes=['pu'])
            S.op('act' if lb % 2 == 0 else 'dve', (lambda e: e.activation(out=ut[:].rearrange("p a b -> p (a b)"), in_=pu[:], func=AF.Copy)) if lb % 2 == 0 else
                 (lambda e: e.tensor_copy(out=ut[:].rearrange("p a b -> p (a b)"), in_=pu[:])), reads=['pu'], writes=[uk])
            S.dma('pool', uv[:, lb * LB:(lb + 1) * LB, :], ut[:], reads=[uk], writes=['Us%d' % g])
    KB = 16
    wbs = [P.sb("wb%d" % i, [128, KB, 2, 128]) for i in range(2)]
    urs = [P.sb("ur%d" % i, [128, 2, KB, 256]) for i in range(2)]
    yts = [P.sb("yt%d" % i, [128, 256]) for i in range(2)]
    fv = FM.rearrange("(k2 k1) c -> k1 k2 c", k1=64)
    it = 0; ib = 0
    for g in range(2):
        for kb in range(64 // KB):
            wb = wbs[ib % 2]; wk_ = 'wb%d' % (ib % 2); ur = urs[ib % 2]; urk = 'ur%d' % (ib % 2)
            ib += 1
            S.dma('sp', wb[:], WB[:, kb * KB:(kb + 1) * KB, :, :], writes=[wk_])
            for ri in range(2):
                S.dma('sp', ur[:, ri, :, :], Us[g][:, ri * 64 + kb * KB:ri * 64 + (kb + 1) * KB, :], reads=['Us%d' % g], writes=[urk + '_%d' % ri])
            for kk in range(KB):
                k1 = kb * KB + kk
                p_ = py[it % 2]; pk = 'py%d' % (it % 2); yt = yts[it % 2]; yk = 'yt%d' % (it % 2)
                it += 1
                for ri in range(2):
                    S.op('pe', lambda e: e.matmul(p_[:], lhsT=wb[:, kk, ri, :], rhs=ur[:, ri, kk, :], start=(ri == 0), stop=(ri == 1)),
                         reads=[wk_, urk + '_0', urk + '_1'], writes=[pk])
                S.op('act' if it % 2 == 0 else 'dve', (lambda e: e.activation(out=yt[:], in_=p_[:], func=AF.Copy)) if it % 2 == 0 else (lambda e: e.tensor_copy(out=yt[:], in_=p_[:])),
                     reads=[pk], writes=[yk])
                S.dma('pool', fv[k1][:, g * 256:(g + 1) * 256], yt[:], reads=[yk], writes=['FM'])
    return P


def l6_consts():
    sc = 1.0 / math.sqrt(8192.0 * 256.0)
    ch = np.arange(256, dtype=np.float64)
    th = 2 * np.pi * np.outer(ch, ch) / 256.0
    CS = np.concatenate([np.cos(th), np.sin(th)], axis=1) * sc
    CS = CS.reshape(2, 128, 512).astype(np.float32)
    l1 = np.arange(64, dtype=np.float64)
    t64 = 2 * np.pi * np.outer(l1, l1) / 64.0
    c, s = np.cos(t64), np.sin(t64)
    W64 = np.block([[c, -s], [-s, -c]]).astype(np.float32)
    l2 = np.arange(128, dtype=np.float64)[:, None, None]
    k1 = np.arange(64, dtype=np.float64)[None, :, None]
    k2 = np.arange(128, dtype=np.float64)[None, None, :]
    thb = 2 * np.pi * (l2 * k2 / 128.0 + l2 * k1 / 8192.0)
    WB = np.stack([np.cos(thb), np.sin(thb)], axis=2).astype(np.float32)
    return CS, W64, np.ascontiguousarray(WB)


def run_l6(h1):
    P = build_l6()
    CS, W64, WB = l6_consts()
    in_maps = []
    for core in range(NCORES):
        b, gp = core // 2, core % 2
        ht = h1[b, :, gp * 512:(gp + 1) * 512].T.reshape(4, 128, 8192)
        in_maps.append(dict(HT=np.ascontiguousarray(ht), CS=CS, W64=W64, WB=WB))
    res = P.run(in_maps)
    out = np.empty((4, 8192, D), np.float32)
    for core in range(NCORES):
        b, gp = core // 2, core % 2
        out[b, :, gp * 512:(gp + 1) * 512] = res[core]["FM"]
    return out


def kernel(x, c, ctx, c_ctx, ada_w, ada_b, norm1_w, norm2_w, w_in, conv_w, a_log, dt_bias, gdn_norm_w,
           lam_q1, lam_k1, lam_q2, lam_k2, subln_w, w_out_ab, w_out_f, peer_wq, peer_keys, peer_u, peer_v, final_norm_w):
    f = lambda a: np.asarray(a, dtype=np.float32)
    x, c, ctx, c_ctx, ada_w, ada_b, norm1_w, norm2_w, w_in, conv_w, a_log, dt_bias, gdn_norm_w = map(
        f, (x, c, ctx, c_ctx, ada_w, ada_b, norm1_w, norm2_w, w_in, conv_w, a_log, dt_bias, gdn_norm_w))
    lam_q1, lam_k1, lam_q2, lam_k2, subln_w, w_out_ab, w_out_f, peer_wq, peer_keys, peer_u, peer_v, final_norm_w = map(
        f, (lam_q1, lam_k1, lam_q2, lam_k2, subln_w, w_out_ab, w_out_f, peer_wq, peer_keys, peer_u, peer_v, final_norm_w))
    Plat, Pctx = run_l1(x, c, ctx, c_ctx, ada_w[0], ada_b[0], norm1_w[0], w_in[0])
    o2 = run_l2(Plat, Pctx, conv_w[0], a_log[0], dt_bias[0])
    O = run_l3(o2['QKV'], o2['BG'])
    dlat = run_l4(o2['QKr'], Plat, Pctx, lam_q1[0], lam_k1[0], lam_q2[0], lam_k2[0])
    del o2
    x1 = run_l5a('ab', x, c, ada_w[0], ada_b[0], w_out_ab[0], O=O, Plat=Plat, dlat=dlat, gdn_norm_w=gdn_norm_w[0], subln_w=subln_w[0])
    del O, dlat, Plat, Pctx
    x2, h1 = run_l5b('hnext', x1, c, ada_w[0], ada_b[0], norm2_w[0], peer_wq[0], peer_keys[0], peer_u[0], peer_v[0],
                     ada_w_n=ada_w[1], ada_b_n=ada_b[1], norm1_w_n=norm1_w[1])
    del x1
    fm = run_l6(h1)
    x3 = run_l5a('f', x2, c, ada_w[1], ada_b[1], w_out_f[0], fm=fm)
    del x2, fm, h1
    zw = np.zeros((D, 2048), np.float32)
    zb = np.zeros((2048,), np.float32)
    _, out = run_l5b('hnext', x3, c, ada_w[1], ada_b[1], norm2_w[1], peer_wq[1], peer_keys[1], peer_u[1], peer_v[1],
                     ada_w_n=zw, ada_b_n=zb, norm1_w_n=final_norm_w)
    return out.astype(np.float32)
```
